# Optimizing a Trainium2 kernel written in Bass

```python
import math
import jax, jax.numpy as jnp
from jax import lax
import numpy as np

D_MODEL = 1024
BATCH = 8
SEQ = 4096
DEPTH = 4

H_A = 4
DK_A = 64
DV_A = 128
CONV_A = 4
MLSTM_CHUNK = 64
F_BIAS_INIT = 3.0
H_B = 8
DH_B = 64
R_KV = 128
H_IDX = 4
D_IDX = 64
TOPK_MAX = 256
TOPK_DIV = 4
Q_BLOCK = 128
H_C = 4
DK_C = 64
DV_C = 128
GLA_RANK = 16
GLA_TAU = 16.0
GLA_CHUNK = 64
H_D = 8
DH_D = 64
DIL_PATTERNS = ((128, 1), (512, 4), (2048, 16))
N_BUCKETS = 32
MAX_DIST = 2048
N_REL_HEADS = 8
N_GROUPS = 4
E_PER_GROUP = 4
TOP_K_FINE = 2
EXPERT_FF = 512

EPS = 1e-6
NEG_INF = -1e30
N_AB = (DEPTH + 1) // 2
N_CD = DEPTH // 2
AB_SIZES = (H_A * DK_A, H_A * DK_A, H_A * DV_A, H_A * DV_A, H_A, H_A, H_B * DH_B, R_KV, H_IDX * D_IDX, D_IDX, H_IDX)
CD_SIZES = (H_C * DK_C, H_C * DK_C, H_C * DV_C, GLA_RANK, H_C * DV_C, H_D * DH_D, H_D * DH_D, H_D * DH_D)
W_AB = sum(AB_SIZES)
W_CD = sum(CD_SIZES)
MIX_AB = H_A * DV_A + H_B * DH_B
MIX_CD = H_C * DV_C + H_D * DH_D

kernel_name = "hybrid_mlstm_dsa_gla_dilated_hmoe"


def rmsnorm(x, g):
    xf = x.astype(jnp.float32)
    y = xf * lax.rsqrt(jnp.mean(xf * xf, axis=-1, keepdims=True) + EPS)
    return (y * g.astype(jnp.float32)).astype(x.dtype)


def split_cols(p, sizes):
    offs = np.cumsum((0,) + tuple(sizes))
    return [p[..., int(a):int(b)] for a, b in zip(offs[:-1], offs[1:])]


def heads(a, n):
    return a.reshape(a.shape[0], a.shape[1], n, -1)


def rel_bucket(dist):
    n = jnp.maximum(dist, 0)
    max_exact = N_BUCKETS // 2
    nf = jnp.maximum(n, 1).astype(jnp.float32)
    large = max_exact + (jnp.log(nf / max_exact) / math.log(MAX_DIST / max_exact)
                         * (N_BUCKETS - max_exact)).astype(jnp.int32)
    large = jnp.minimum(large, N_BUCKETS - 1)
    return jnp.where(n < max_exact, n, large)


def causal_dwconv(x, w, b):
    K = w.shape[0]
    L = x.shape[1]
    xp = jnp.pad(x, ((0, 0), (K - 1, 0), (0, 0)))
    return sum(xp[:, j:j + L] * w[j] for j in range(K)) + b


def mlstm_chunkwise(q, k, v, i_pre, f_pre):
    Bn, L, H, DK = q.shape
    DV = v.shape[-1]
    T = MLSTM_CHUNK
    NC = L // T

    def chunks(a):
        return jnp.moveaxis(a.reshape((Bn, NC, T) + a.shape[2:]), 1, 0)

    causal = jnp.tril(jnp.ones((T, T), bool))
    log_f = jax.nn.log_sigmoid(f_pre)

    def step(carry, xs):
        Cm, n, m = carry
        qc, kc, vc, li, lf = xs
        b = jnp.cumsum(lf, axis=1)
        Dm = jnp.where(causal[None, :, :, None],
                       b[:, :, None, :] - b[:, None, :, :] + li[:, None, :, :], NEG_INF)
        g = b + m[:, None, :]
        mt = jnp.maximum(g, Dm.max(axis=2))
        w_intra = jnp.exp(Dm - mt[:, :, None, :])
        w_state = jnp.exp(g - mt)
        s = jnp.einsum('bthk,bshk->btsh', qc, kc) * w_intra
        num = (w_state[..., None] * jnp.einsum('bthk,bhkv->bthv', qc, Cm)
               + jnp.einsum('btsh,bshv->bthv', s, vc))
        den = w_state * jnp.einsum('bthk,bhk->bth', qc, n) + s.sum(axis=2)
        h = num / jnp.maximum(jnp.abs(den), jnp.exp(-mt))[..., None]
        bL = b[:, -1]
        d_end = bL[:, None, :] - b + li
        m_new = jnp.maximum(bL + m, d_end.max(axis=1))
        w_e = jnp.exp(d_end - m_new[:, None, :])
        decay = jnp.exp(bL + m - m_new)
        Cm = decay[..., None, None] * Cm + jnp.einsum('bsh,bshk,bshv->bhkv', w_e, kc, vc)
        n = decay[..., None] * n + jnp.einsum('bsh,bshk->bhk', w_e, kc)
        return (Cm, n, m_new), h

    init = (jnp.zeros((Bn, H, DK, DV), jnp.float32), jnp.zeros((Bn, H, DK), jnp.float32),
            jnp.full((Bn, H), NEG_INF, jnp.float32))
    _, hs = lax.scan(step, init, (chunks(q), chunks(k), chunks(v), chunks(i_pre), chunks(log_f)))
    return jnp.moveaxis(hs, 0, 1).reshape(Bn, L, H, DV)


def gla_chunkwise(q, k, v, log_a):
    Bn, L, H, DK = q.shape
    DV = v.shape[-1]
    T = GLA_CHUNK
    NC = L // T

    def chunks(a):
        return jnp.moveaxis(a.reshape((Bn, NC, T) + a.shape[2:]), 1, 0)

    causal = jnp.tril(jnp.ones((T, T), bool))

    def step(S, xs):
        qc, kc, vc, la = xs
        b = jnp.cumsum(la, axis=1)
        inter = jnp.einsum('bthk,bhkv->bthv', qc * jnp.exp(b), S)
        decay = jnp.exp(jnp.where(causal[None, :, :, None, None], b[:, :, None] - b[:, None, :], NEG_INF))
        A = jnp.einsum('bthk,btshk,bshk->btsh', qc, decay, kc)
        o = inter + jnp.einsum('btsh,bshv->bthv', A, vc)
        bL = b[:, -1]
        S = jnp.exp(bL)[..., None] * S + jnp.einsum('bshk,bshv->bhkv', kc * jnp.exp(bL[:, None] - b), vc)
        return S, o

    _, os_ = lax.scan(step, jnp.zeros((Bn, H, DK, DV), jnp.float32),
                      (chunks(q), chunks(k), chunks(v), chunks(log_a)))
    return jnp.moveaxis(os_, 0, 1).reshape(Bn, L, H, DV)


def dsa_attention(q_abs, c_kv, q_idx, k_idx, w_idx, w_uv, rel_bias):
    Bn, L, H, R = q_abs.shape
    topk = min(TOPK_MAX, L // TOPK_DIV)
    NB = L // Q_BLOCK
    key_pos = jnp.arange(L)
    gather = jax.vmap(lambda tab, idx: tab[idx])

    def block(blk):
        t0 = blk * Q_BLOCK
        qa = lax.dynamic_slice_in_dim(q_abs, t0, Q_BLOCK, axis=1)
        qi = lax.dynamic_slice_in_dim(q_idx, t0, Q_BLOCK, axis=1)
        wi = lax.dynamic_slice_in_dim(w_idx, t0, Q_BLOCK, axis=1)
        pos = t0 + jnp.arange(Q_BLOCK)
        rel = jax.nn.relu(jnp.einsum('bthd,bsd->bths', qi, k_idx))
        score = jnp.einsum('bths,bth->bts', rel, wi).astype(jnp.float32)
        score = jnp.where(key_pos[None, None, :] <= pos[None, :, None], score, -jnp.inf)
        _, idx = lax.top_k(score, topk)
        c_sel = gather(c_kv, idx)
        dist = pos[None, :, None] - idx
        bias = rel_bias[rel_bucket(dist)].transpose(0, 1, 3, 2)
        logits = jnp.einsum('bthr,btkr->bthk', qa, c_sel).astype(jnp.float32) + bias.astype(jnp.float32)
        logits = jnp.where((dist >= 0)[:, :, None, :], logits, NEG_INF)
        p = jax.nn.softmax(logits, axis=-1).astype(c_kv.dtype)
        o_lat = jnp.einsum('bthk,btkr->bthr', p, c_sel)
        return jnp.einsum('bthr,rhd->bthd', o_lat, w_uv)

    out = lax.map(block, jnp.arange(NB))
    return jnp.moveaxis(out, 0, 1).reshape(Bn, L, H, -1)


def dilated_partial(q, k, v, rel_bias, window, dil):
    Bn, L, H, Dh = q.shape
    W = window // dil
    M = L // dil
    nb = -(-M // W)
    Mp = nb * W

    def to_sub(a):
        a = a.reshape(Bn, M, dil, H, Dh).transpose(0, 2, 1, 3, 4)
        a = jnp.pad(a, ((0, 0), (0, 0), (0, Mp - M), (0, 0), (0, 0)))
        return a.reshape(Bn, dil, nb, W, H, Dh)

    def with_prev(a):
        prev = jnp.pad(a[:, :, :-1], ((0, 0), (0, 0), (1, 0), (0, 0), (0, 0), (0, 0)))
        return jnp.concatenate([prev, a], axis=3)

    qs = to_sub(q)
    kc = with_prev(to_sub(k))
    vc = with_prev(to_sub(v))
    tap = jnp.arange(W)[:, None] + W - jnp.arange(2 * W)[None, :]
    tap_ok = (tap >= 0) & (tap <= W)
    blk_ok = (jnp.arange(nb)[:, None] > 0) | (jnp.arange(2 * W)[None, :] >= W)
    mask = tap_ok[None] & blk_ok[:, None, :]
    bias = rel_bias[rel_bucket(jnp.clip(tap, 0, W) * dil)].transpose(2, 0, 1).astype(jnp.float32)
    logits = jnp.einsum('brnqhd,brnkhd->brnhqk', qs, kc).astype(jnp.float32) + bias
    logits = jnp.where(mask[:, None], logits, NEG_INF)
    m = logits.max(axis=-1)
    p = jnp.exp(logits - m[..., None])
    den = p.sum(axis=-1)
    num = jnp.einsum('brnhqk,brnkhd->brnqhd', p, vc.astype(jnp.float32))

    def from_sub(a):
        a = a.reshape((Bn, dil, Mp) + a.shape[4:])[:, :, :M]
        a = jnp.swapaxes(a, 1, 2)
        return a.reshape((Bn, L) + a.shape[3:])

    return from_sub(num), from_sub(jnp.swapaxes(m, 3, 4)), from_sub(jnp.swapaxes(den, 3, 4))


def dilated_mixture(q, k, v, rel_bias):
    parts = [dilated_partial(q, k, v, rel_bias, w, d) for (w, d) in DIL_PATTERNS]
    m_all = jnp.max(jnp.stack([pt[1] for pt in parts]), axis=0)
    num = sum(pt[0] * jnp.exp(pt[1] - m_all)[..., None] for pt in parts)
    den = sum(pt[2] * jnp.exp(pt[1] - m_all) for pt in parts)
    return num / den[..., None]


def mixer_ab(h, w_in, conv_w, conv_b, gate_b, hnorm_g, w_uk, w_uv, rel_bias, w_out):
    Bn, L, _ = h.shape
    p = h @ w_in
    qa, ka, va, oa, ia, fa, qb, ckv, qi, ki, wi = split_cols(p, AB_SIZES)
    qk = jax.nn.silu(causal_dwconv(jnp.concatenate([qa, ka], axis=-1), conv_w, conv_b))
    qa, ka = qk[..., :H_A * DK_A], qk[..., H_A * DK_A:]
    ha = mlstm_chunkwise(heads(qa, H_A).astype(jnp.float32) * DK_A ** -0.5,
                         heads(ka, H_A).astype(jnp.float32),
                         heads(va, H_A).astype(jnp.float32),
                         (ia + gate_b[0]).astype(jnp.float32),
                         (fa + gate_b[1]).astype(jnp.float32))
    ha = rmsnorm(ha.astype(h.dtype), hnorm_g.reshape(H_A, DV_A)).reshape(Bn, L, -1)
    out_a = ha * jax.nn.sigmoid(oa)
    q_abs = jnp.einsum('blhd,rhd->blhr', heads(qb, H_B), w_uk) * DH_B ** -0.5
    out_b = dsa_attention(q_abs, ckv, heads(qi, H_IDX), ki, wi * H_IDX ** -0.5, w_uv, rel_bias)
    y = jnp.concatenate([out_a, out_b.reshape(Bn, L, -1).astype(h.dtype)], axis=-1)
    return y @ w_out


def mixer_cd(h, w_in, w_alpha, b_alpha, hnorm_g, rel_bias, w_out):
    Bn, L, _ = h.shape
    p = h @ w_in
    qc, kc, vc, gc, rc, qd, kd, vd = split_cols(p, CD_SIZES)
    log_a = jax.nn.log_sigmoid((gc @ w_alpha + b_alpha).astype(jnp.float32)) / GLA_TAU
    oc = gla_chunkwise(heads(qc, H_C).astype(jnp.float32) * DK_C ** -0.5,
                       heads(kc, H_C).astype(jnp.float32),
                       heads(vc, H_C).astype(jnp.float32),
                       heads(log_a, H_C))
    oc = rmsnorm(oc.astype(h.dtype), hnorm_g.reshape(H_C, DV_C)).reshape(Bn, L, -1) * jax.nn.silu(rc)
    od = dilated_mixture(heads(qd, H_D) * DH_D ** -0.5, heads(kd, H_D), heads(vd, H_D), rel_bias)
    y = jnp.concatenate([oc, od.reshape(Bn, L, -1).astype(h.dtype)], axis=-1)
    return y @ w_out


def hier_moe(h, w_coarse, b_coarse, w_fine, b_fine, w_gate, w_up, w_down):
    pc = jax.nn.softmax((h @ w_coarse + b_coarse).astype(jnp.float32), axis=-1)
    g_idx = jnp.argmax(pc, axis=-1)
    p_g = jnp.max(pc, axis=-1)
    fine = (jnp.einsum('bld,gde->blge', h, w_fine) + b_fine).astype(jnp.float32)
    fine_sel = jnp.take_along_axis(fine, g_idx[..., None, None], axis=2)[..., 0, :]
    top_v, top_i = lax.top_k(fine_sel, TOP_K_FINE)
    p_e = jax.nn.softmax(top_v, axis=-1)
    w_e = jnp.sum(jax.nn.one_hot(top_i, E_PER_GROUP, dtype=jnp.float32) * p_e[..., None], axis=-2)
    gates = (p_g[..., None, None] * jax.nn.one_hot(g_idx, N_GROUPS, dtype=jnp.float32)[..., None]
             * w_e[..., None, :]).astype(h.dtype)
    y = jnp.zeros_like(h)
    for g in range(N_GROUPS):
        a = jax.nn.silu(jnp.einsum('bld,edf->blef', h, w_gate[g])) * jnp.einsum('bld,edf->blef', h, w_up[g])
        y = y + jnp.einsum('blef,efd->bld', a * gates[:, :, g, :, None], w_down[g])
    return y


def setup_inputs(seed: int = 0) -> dict:
    key = jax.random.key(seed)
    ks = jax.random.split(key, 32)
    f32 = jnp.float32

    def nrm(k, shape, scale):
        return jax.random.normal(k, shape, f32) * scale

    D = D_MODEL
    gate_base = jnp.array([0.0, F_BIAS_INIT], f32)[None, :, None]
    return {
        "x": nrm(ks[0], (BATCH, SEQ, D), 1.0),
        "c": nrm(ks[1], (BATCH, D), 1.0),
        "w_ada": nrm(ks[2], (DEPTH, D, 6 * D), 0.5 * D ** -0.5),
        "b_ada": nrm(ks[3], (DEPTH, 6 * D), 0.02),
        "g_mix": 1.0 + nrm(ks[4], (DEPTH, D), 0.05),
        "g_ffn": 1.0 + nrm(ks[5], (DEPTH, D), 0.05),
        "g_final": 1.0 + nrm(ks[6], (D,), 0.05),
        "rel_bias": nrm(ks[7], (N_BUCKETS, N_REL_HEADS), 0.5),
        "ab_w_in": nrm(ks[8], (N_AB, D, W_AB), D ** -0.5),
        "ab_conv_w": nrm(ks[9], (N_AB, CONV_A, 2 * H_A * DK_A), CONV_A ** -0.5),
        "ab_conv_b": nrm(ks[10], (N_AB, 2 * H_A * DK_A), 0.02),
        "ab_gate_b": gate_base + nrm(ks[11], (N_AB, 2, H_A), 0.1),
        "ab_hnorm_g": 1.0 + nrm(ks[12], (N_AB, H_A * DV_A), 0.05),
        "ab_w_uk": nrm(ks[13], (N_AB, R_KV, H_B, DH_B), R_KV ** -0.5),
        "ab_w_uv": nrm(ks[14], (N_AB, R_KV, H_B, DH_B), R_KV ** -0.5),
        "ab_w_out": nrm(ks[15], (N_AB, MIX_AB, D), MIX_AB ** -0.5),
        "cd_w_in": nrm(ks[16], (N_CD, D, W_CD), D ** -0.5),
        "cd_w_alpha": nrm(ks[17], (N_CD, GLA_RANK, H_C * DK_C), GLA_RANK ** -0.5),
        "cd_b_alpha": nrm(ks[18], (N_CD, H_C * DK_C), 0.1),
        "cd_hnorm_g": 1.0 + nrm(ks[19], (N_CD, H_C * DV_C), 0.05),
        "cd_w_out": nrm(ks[20], (N_CD, MIX_CD, D), MIX_CD ** -0.5),
        "moe_w_coarse": nrm(ks[21], (DEPTH, D, N_GROUPS), D ** -0.5),
        "moe_b_coarse": nrm(ks[22], (DEPTH, N_GROUPS), 0.01),
        "moe_w_fine": nrm(ks[23], (DEPTH, N_GROUPS, D, E_PER_GROUP), D ** -0.5),
        "moe_b_fine": nrm(ks[24], (DEPTH, N_GROUPS, E_PER_GROUP), 0.01),
        "moe_w_gate": nrm(ks[25], (DEPTH, N_GROUPS, E_PER_GROUP, D, EXPERT_FF), D ** -0.5),
        "moe_w_up": nrm(ks[26], (DEPTH, N_GROUPS, E_PER_GROUP, D, EXPERT_FF), D ** -0.5),
        "moe_w_down": nrm(ks[27], (DEPTH, N_GROUPS, E_PER_GROUP, EXPERT_FF, D), EXPERT_FF ** -0.5),
    }


def reference(x, c, w_ada, b_ada, g_mix, g_ffn, g_final, rel_bias,
              ab_w_in, ab_conv_w, ab_conv_b, ab_gate_b, ab_hnorm_g, ab_w_uk, ab_w_uv, ab_w_out,
              cd_w_in, cd_w_alpha, cd_b_alpha, cd_hnorm_g, cd_w_out,
              moe_w_coarse, moe_b_coarse, moe_w_fine, moe_b_fine, moe_w_gate, moe_w_up, moe_w_down):
    mod = jnp.einsum('bd,lde->lbe', jax.nn.silu(c), w_ada) + b_ada[:, None, :]
    for l in range(DEPTH):
        sh_m, sc_m, gt_m, sh_f, sc_f, gt_f = jnp.split(mod[l][:, None, :], 6, axis=-1)
        hm = rmsnorm(x, g_mix[l]) * (1.0 + sc_m) + sh_m
        j = l // 2
        if l % 2 == 0:
            y = mixer_ab(hm, ab_w_in[j], ab_conv_w[j], ab_conv_b[j], ab_gate_b[j], ab_hnorm_g[j],
                         ab_w_uk[j], ab_w_uv[j], rel_bias, ab_w_out[j])
        else:
            y = mixer_cd(hm, cd_w_in[j], cd_w_alpha[j], cd_b_alpha[j], cd_hnorm_g[j], rel_bias, cd_w_out[j])
        x = x + gt_m * y
        hf = rmsnorm(x, g_ffn[l]) * (1.0 + sc_f) + sh_f
        x = x + gt_f * hier_moe(hf, moe_w_coarse[l], moe_b_coarse[l], moe_w_fine[l], moe_b_fine[l],
                                moe_w_gate[l], moe_w_up[l], moe_w_down[l])
    return rmsnorm(x, g_final)
```

```python
import math
import os
from contextlib import ExitStack
import numpy as np
import concourse.bass as bass
import concourse.mybir as mybir
from concourse.bass_utils import run_bass_kernel_spmd

F32 = mybir.dt.float32
BF16 = mybir.dt.bfloat16
ALU = mybir.AluOpType
AF = mybir.ActivationFunctionType
AX = mybir.AxisListType


class _Op:
    __slots__ = ("eng", "fn", "deps", "signal", "sidx", "is_dma", "dslot", "dval", "event")

    def __init__(self, eng, fn, is_dma):
        self.eng = eng
        self.fn = fn
        self.is_dma = is_dma
        self.deps = []
        self.signal = False
        self.sidx = 0
        self.dslot = 0
        self.dval = 0
        self.event = None


def _key(k):
    if isinstance(k, str):
        return k
    if isinstance(k, tuple):
        return _key(k[0]) + "#" + "#".join(str(i) for i in k[1:])
    return k.name


class Prog:
    ENGS = ("tensor", "vector", "scalar", "gpsimd", "sync")
    EPOCH = 16000
    NDSEM = 8

    def __init__(self, nc):
        self.nc = nc
        self.es = ExitStack()
        self.ops = {e: [] for e in self.ENGS}
        self.lastw = {}
        self.readers = {}
        self.ndma = {e: 0 for e in self.ENGS}
        self.lastop = {}
        self.lastdma = {}

    def sb(self, name, shape, dt):
        return self.es.enter_context(self.nc.sbuf_tensor(name, list(shape), dt))

    def ps(self, name, shape, dt):
        return self.es.enter_context(self.nc.psum_tensor(name, list(shape), dt))

    def dram(self, name, shape, dt):
        return self.nc.dram_tensor(name, list(shape), dt, kind="Internal").ap()

    def add(self, eng, fn, reads=(), writes=(), is_dma=False):
        op = _Op(eng, fn, is_dma)
        deps = {}
        rk = [_key(k) for k in reads]
        wk = [_key(k) for k in writes]
        for k in rk:
            w = self.lastw.get(k)
            if w is not None:
                deps[id(w)] = w
        for k in wk:
            w = self.lastw.get(k)
            if w is not None:
                deps[id(w)] = w
            for r in self.readers.get(k, {}).values():
                deps[id(r)] = r
        for d in deps.values():
            if d is op:
                continue
            if (not is_dma) and (not d.is_dma) and d.eng == eng == "tensor":
                continue
            op.deps.append(d)
            d.signal = True
        if is_dma:
            n = self.ndma[eng]
            self.ndma[eng] = n + 1
            op.dslot = n % self.NDSEM
            op.dval = 16 * (n // self.NDSEM + 1)
            self.lastdma[(eng, op.dslot)] = op
        else:
            self.lastop[eng] = op
        rkey = (eng, op.dslot) if is_dma else eng
        for k in rk:
            self.readers.setdefault(k, {})[rkey] = op
        for k in wk:
            self.lastw[k] = op
            self.readers[k] = {}
        self.ops[eng].append(op)
        return op

    def fence(self):
        allops = list(self.lastop.values()) + list(self.lastdma.values())
        for e in self.ENGS:
            op = _Op(e, None, False)
            for d in allops:
                if d.eng == e and not d.is_dma:
                    continue
                op.deps.append(d)
                d.signal = True
            self.ops[e].append(op)
        self.lastw = {}
        self.readers = {}

    def dma(self, q, out, in_, reads, writes, **kw):
        return self.add(q, lambda e: e.dma_start(out=out, in_=in_, **kw), reads, writes, is_dma=True)

    def act(self, out, in_, func, reads, writes, **kw):
        return self.add("scalar", lambda e: e.activation(out=out, in_=in_, func=func, **kw), reads, writes)

    def mm(self, out, lhsT, rhs, start, stop, reads, writes):
        return self.add("tensor", lambda e: e.matmul(out, lhsT=lhsT, rhs=rhs, start=start, stop=stop),
                        reads, writes)

    def tr(self, out, in_, ident, reads, writes):
        return self.add("tensor", lambda e: e.transpose(out, in_, ident), reads, writes)

    def v(self, fn, reads, writes):
        return self.add("vector", fn, reads, writes)

    def g(self, fn, reads, writes):
        return self.add("gpsimd", fn, reads, writes)

    def finish(self):
        nc = self.nc
        es = self.es
        esem = {}
        for e in self.ENGS:
            c = 0
            for op in self.ops[e]:
                if op.is_dma:
                    continue
                if op.signal:
                    c += 1
                    op.sidx = c
            nep = c // self.EPOCH + 1
            esem[e] = [es.enter_context(nc.semaphore(f"s_{e}_{i}")) for i in range(nep)]
        dsem = {}
        for e in self.ENGS:
            if self.ndma[e]:
                dsem[e] = [es.enter_context(nc.semaphore(f"d_{e}_{i}")) for i in range(self.NDSEM)]
        for e in self.ENGS:
            for op in self.ops[e]:
                if op.is_dma:
                    op.event = (dsem[e][op.dslot], op.dval)
                elif op.signal:
                    i = op.sidx - 1
                    op.event = (esem[e][i // self.EPOCH], i % self.EPOCH + 1)

        def emit(eng, e):
            waited = {}

            def wait(ev):
                sem, val = ev
                k = sem.name
                if waited.get(k, 0) < val:
                    eng.wait_ge(sem, val)
                    waited[k] = val

            for op in self.ops[e]:
                mx = {}
                for d in op.deps:
                    sem, val = d.event
                    k = sem.name
                    if k not in mx or mx[k][1] < val:
                        mx[k] = (sem, val)
                for ev in mx.values():
                    wait(ev)
                if op.fn is None:
                    continue
                if op.is_dma:
                    if op.dval > 16:
                        wait((dsem[e][op.dslot], op.dval - 16))
                    op.fn(eng).then_inc(dsem[e][op.dslot], 16)
                else:
                    ins = op.fn(eng)
                    if op.signal:
                        ins.then_inc(*((op.event[0], 1)))
            if e == "sync":
                for q in self.ENGS:
                    n = self.ndma[q]
                    for s in range(min(n, self.NDSEM)):
                        last = ((n - 1 - s) // self.NDSEM) * self.NDSEM + s
                        wait((dsem[q][s], 16 * (last // self.NDSEM + 1)))

        with nc.Block() as block:
            @block.tensor
            def _(eng):
                emit(eng, "tensor")

            @block.vector
            def _(eng):
                emit(eng, "vector")

            @block.scalar
            def _(eng):
                emit(eng, "scalar")

            @block.gpsimd
            def _(eng):
                emit(eng, "gpsimd")

            @block.sync
            def _(eng):
                emit(eng, "sync")
        es.close()


L = 4096
D = 1024
NB = 32
NG = 8
DEPTH = 4
W_AB = 2508
W_CD = 3088
NEG = -30000.0
NFR = 3072
SU = 2944
NIT = 24
TOPK = 256


def _rel_bucket_np(d):
    n = np.maximum(d, 0)
    nf = np.maximum(n, 1).astype(np.float32)
    large = 16 + (np.log(nf / np.float32(16)) / np.float32(math.log(2048 / 16)) * np.float32(16)).astype(np.int32)
    large = np.minimum(large, 31)
    return np.where(n < 16, n, large)


def _host_consts():
    c = {}
    c["ident"] = np.eye(128, dtype=np.float32)
    c["exch"] = np.eye(128, dtype=np.float32)[::-1].copy()
    mk = np.zeros((128, 4, 512), np.float32)
    p = np.arange(128)[:, None]
    u = np.arange(512)[None, :]
    for j in range(4):
        mk[:, j, :] = np.where(u - 128 * j - p >= 0, 0.0, NEG)
    c["maskneg"] = mk
    c["cmT"] = np.where(np.arange(128)[None, :] > np.arange(128)[:, None], -1e30, 0.0).astype(np.float32)
    i = np.arange(NFR)
    d = i - 511
    bk = _rel_bucket_np(d)
    oh = np.zeros((32, NFR), np.float32)
    oh[bk, i] = 1.0
    oh[:, d < 0] = 0.0
    c["oh"] = oh
    add = np.zeros((2, 8, NFR), np.float32)
    add[0, :, d < 0] = NEG
    cnt = ((d <= 128).astype(np.float32) + ((d % 4 == 0) & (d <= 512)) + ((d % 16 == 0) & (d <= 2048)))
    dil = np.where((d >= 0) & (cnt > 0), np.log(np.maximum(cnt, 1.0)), NEG).astype(np.float32)
    add[1, :, :] = dil[None, :]
    c["addrow"] = add
    c["ck"] = np.broadcast_to((0.5 ** (np.arange(NIT) + 1)).astype(np.float32)[None, :], (128, NIT)).copy()
    return c


class _Rot:
    def __init__(self, tiles):
        self.t = tiles
        self.i = 0

    def next(self):
        t = self.t[self.i % len(self.t)]
        self.i += 1
        return t


class _Stop(Exception):
    pass


def build_nc(nlayers=DEPTH, dbg=False, stop=None, only=None):
    def chk(name):
        if stop == name:
            raise _Stop()

    nc = bass.Bass("TRN2", target_bir_lowering=False)
    P = Prog(nc)
    I = {}

    def inp(name, shape):
        I[name] = nc.dram_tensor(name, list(shape), F32, kind="ExternalInput").ap()

    WL = max(1, nlayers) if dbg else 4
    for name, shape in [
        ("x", (L, D)), ("c", (8, 128)), ("w_ada", (WL, D, 6 * D)), ("b_ada", (4, 6 * D)), ("g_mix", (4, D)),
        ("g_ffn", (4, D)), ("g_final", (1, D)), ("rel_bias", (32, 8)), ("ab_w_in", (2, D, W_AB)),
        ("ab_conv_w", (2, 4, 512)), ("ab_conv_b", (2, 512)), ("ab_gate_b", (2, 8)), ("ab_hnorm_g", (2, 512)),
        ("ab_w_uk", (2, 128, 512)), ("ab_w_uv", (2, 128, 512)), ("ab_w_out", (2, D, D)),
        ("cd_w_in", (2, D, W_CD)), ("cd_w_alpha", (2, 16, 256)), ("cd_b_alpha", (2, 256)),
        ("cd_hnorm_g", (2, 512)), ("cd_w_out", (2, D, D)), ("moe_w_coarse", (4, D, 4)), ("moe_b_coarse", (4, 4)),
        ("moe_w_fine", (4, 4, D, 4)), ("moe_b_fine", (4, 16)), ("moe_w_gate", (WL, 16, D, 512)),
        ("moe_w_up", (WL, 16, D, 512)), ("moe_w_down", (WL, 16, 512, D)),
        ("ident", (128, 128)), ("exch", (128, 128)), ("maskneg", (128, 4, 512)), ("cmT", (128, 128)),
        ("oh", (32, NFR)), ("addrow", (2, 8, NFR)), ("ck", (128, NIT)),
    ]:
        inp(name, shape)
    out = nc.dram_tensor("out", [L, D], F32, kind="ExternalOutput").ap()
    okind = "ExternalOutput" if dbg else "Internal"
    xs = nc.dram_tensor("xs", [L, D], F32, kind=okind).ap()
    Yd = nc.dram_tensor("Yd", [8, 128, L], BF16, kind=okind).ap()
    modd = nc.dram_tensor("modd", [4, 6 * D], F32, kind=okind).ap()
    Frow = nc.dram_tensor("Frow", [2, 8, NFR], F32, kind="Internal").ap()
    gated = nc.dram_tensor("gated", [2, 128, L], BF16, kind="Internal").ap()
    gated = nc.dram_tensor("gated4", [4, 128, L], BF16, kind="Internal").ap()
    rowsd = nc.dram_tensor("rowsd", [2, 4, L], F32, kind="Internal").ap()
    nmtd = nc.dram_tensor("nmtd", [NB, 128, L], BF16, kind="Internal").ap()

    R = [P.sb(f"R{i}", [128, 16384], BF16) for i in range(3)]
    W = [P.sb(f"W{i}", [128, 12416], BF16) for i in range(2)]
    ident_f = P.sb("ident_f", [128, 128], F32)
    ident_b = P.sb("ident_b", [128, 128], BF16)
    exch_b = P.sb("exch_b", [128, 128], BF16)
    ones_b = P.sb("ones_b", [128, 128], BF16)
    ones_f = P.sb("ones_f", [128, 128], F32)
    maskneg = P.sb("maskneg_t", [128, 4, 512], BF16)
    mask01 = maskneg
    cmT = P.sb("cmT_t", [128, 128], F32)
    eps_t = P.sb("eps_t", [128, 1], F32)
    ckt = P.sb("ckt", [128, NIT], F32)
    gsB = P.sb("gsB", [128, D], F32)
    shB = P.sb("shB", [128, D], F32)
    gtB = P.sb("gtB", [128, D], F32)
    XB = _Rot([P.sb(f"xb{i}", [128, D], F32) for i in range(1)])
    HM = _Rot([P.sb(f"hm{i}", [128, D], F32) for i in range(1)])
    HT = _Rot([P.sb(f"hT{i}", [128, 8, 512], BF16) for i in range(2)])
    sm = P.sb("sm", [128, 64], F32)
    PT = _Rot([P.sb(f"pt{i}", [128, 512], BF16) for i in range(3)])
    ET = _Rot([P.sb(f"et{i}", [128, 512], F32) for i in range(2)])
    junk = ET.t[0][:, :].bitcast(BF16)
    FT = _Rot([P.sb(f"ft{i}", [128, 512], F32) for i in range(4)])
    ST = _Rot([P.sb(f"st{i}", [128, 512], BF16) for i in range(2)])
    pT = P.ps("pT", [128, 1024], F32)
    PG = _Rot([P.ps(f"pG{i}", [128, 512], F32) for i in range(2)])
    PA = [P.ps(f"pA{i}", [128, 512], F32) for i in range(4)]

    def bc(row_ap, n=D):
        return row_ap.to_broadcast([128, n])

    P.dma("sync", ident_f[:], I["ident"], ["c_ident"], [ident_f])
    P.v(lambda e: e.tensor_copy(out=ident_b[:], in_=ident_f[:]), [ident_f], [ident_b])
    P.dma("gpsimd", exch_b[:], I["exch"], ["c_exch"], [exch_b])
    P.v(lambda e: e.memset(ones_b[:], 1.0), [], [ones_b])
    P.v(lambda e: e.memset(ones_f[:], 1.0), [], [ones_f])
    P.v(lambda e: e.memset(eps_t[:], 1e-6), [], [eps_t])
    P.dma("sync", cmT[:], I["cmT"], ["c_cm"], [cmT])
    P.dma("sync", ckt[:], I["ck"], ["c_ck"], [ckt])

    relt = P.sb("relt", [32, 8], F32)
    P.dma("sync", relt[:], I["rel_bias"], ["c_rel"], [relt])
    oht = R[0][0:32, 0:2 * NFR].bitcast(F32)
    P.dma("sync", oht, I["oh"], ["c_oh"], ["oht"])
    for ty in range(2):
        addt = R[1][0:8, 0:2 * NFR].bitcast(F32)
        P.dma("sync", addt, I["addrow"][ty], ["c_add"], ["addt"])
        for j in range(NFR // 512):
            pg = PG.next()
            P.mm(pg[0:8, :], relt[:, :], oht[:, j * 512:(j + 1) * 512], True, True, [relt, "oht"], [pg])
            P.v(lambda e, pg=pg, j=j, addt=addt: e.tensor_tensor(out=addt[:, j * 512:(j + 1) * 512], in0=pg[0:8, :],
                                                                  in1=addt[:, j * 512:(j + 1) * 512], op=ALU.add),
                [pg, "addt"], ["addt"])
        P.dma("sync", Frow[ty], addt, ["addt"], [("Frow", ty)])
    P.fence()

    c8 = P.sb("c8", [8, 128], F32)
    cs = P.sb("cs", [128, 8], F32)
    P.dma("sync", c8[:], I["c"], ["c_c"], [c8])
    pg = PG.next()
    P.tr(pg[:, 0:8], c8[:, :], ident_f[0:8, 0:8], [c8, ident_f], [pg])
    P.act(cs[:], pg[:, 0:8], AF.Silu, [pg], [cs])
    R2f = R[2][:, :].bitcast(F32)
    brow = _Rot([R2f[0:1, i * 512:(i + 1) * 512] for i in range(2)])
    mrow = _Rot([R2f[0:1, (2 + i) * 512:(3 + i) * 512] for i in range(2)])
    wi_ = 0
    for l in range(nlayers):
        for j in range(12):
            wt = W[wi_ % 2]
            wi_ += 1
            wv = wt[:, 0:8192].bitcast(F32).rearrange("p (k n) -> p k n", k=8)
            P.dma("sync", wv, I["w_ada"][l][:, j * 512:(j + 1) * 512].rearrange("(k p) n -> p k n", p=128),
                  ["w_ada"], [wt])
            br = brow.next()
            brk = f"brow{j % 2}"
            mrk = f"mrow{j % 2}"
            P.dma("sync", br, I["b_ada"][l:l + 1, j * 512:(j + 1) * 512], ["b_ada"], [brk])
            pg = PG.next()
            for k in range(8):
                P.mm(pg[0:1, :], cs[:, k:k + 1], wv[:, k, :], k == 0, k == 7, [cs, wt], [pg])
            mr = mrow.next()
            P.v(lambda e, mr=mr, pg=pg, br=br: e.tensor_tensor(out=mr, in0=pg[0:1, :], in1=br, op=ALU.add),
                [pg, brk], [mrk])
            P.dma("sync", modd[l:l + 1, j * 512:(j + 1) * 512], mr, [mrk], [("modd", l)])
    P.fence()

    def modrow(l, j):
        return modd[l:l + 1, j * D:(j + 1) * D]

    def prep_norm(l, ffn):
        o = 3 if ffn else 0
        grow = (I["g_ffn"] if ffn else I["g_mix"])[l:l + 1, :]
        gB = HM.next()
        P.dma("sync", gB[:], bc(grow), ["g"], [gB])
        P.dma("sync", gsB[:], bc(modrow(l, o + 1)), [("modd", l)], [gsB])
        P.dma("sync", shB[:], bc(modrow(l, o + 0)), [("modd", l)], [shB])
        P.dma("sync", gtB[:], bc(modrow(l, o + 2)), [("modd", l)], [gtB])
        P.v(lambda e: e.scalar_tensor_tensor(out=gsB[:], in0=gsB[:], scalar=1.0, in1=gB[:], op0=ALU.add, op1=ALU.mult),
            [gsB, gB], [gsB])

    def xsrc(l):
        return I["x"] if l == 0 else xs

    def norm_block(xsrc_ap, blk, ht, b, htf=None):
        xb = XB.next()
        P.dma("sync", xb[:], xsrc_ap[blk * 128:(blk + 1) * 128, :], [("xs", blk)], [xb])
        ss = sm[:, 0:1]
        P.act(junk, xb[:], AF.Square, [xb], [ET.t[0], "ss"], accum_out=ss)
        P.v(lambda e: e.tensor_scalar(out=sm[:, 1:2], in0=ss, scalar1=1.0 / D, scalar2=1e-6, op0=ALU.mult, op1=ALU.add),
            ["ss"], ["rs"])
        P.act(sm[:, 1:2], sm[:, 1:2], AF.Sqrt, ["rs"], ["rs"])
        P.v(lambda e: e.reciprocal(out=sm[:, 1:2], in_=sm[:, 1:2]), ["rs"], ["rs"])
        hm = HM.next()
        P.v(lambda e, hm=hm, xb=xb: e.scalar_tensor_tensor(out=hm[:], in0=xb[:], scalar=sm[:, 1:2], in1=gsB[:],
                                                           op0=ALU.mult, op1=ALU.mult), [xb, "rs", gsB], [hm])
        P.g(lambda e, hm=hm: e.tensor_tensor(out=hm[:], in0=hm[:], in1=shB[:], op=ALU.add), [hm, shB], [hm])
        for k in range(8):
            P.tr(pT[:, k * 128:(k + 1) * 128], hm[:, k * 128:(k + 1) * 128], ident_f[:], [hm, ident_f], [pT])
        pv = pT[:, :].rearrange("p (k t) -> p k t", k=8)
        P.act(ht[:, :, b * 128:(b + 1) * 128], pv, AF.Copy, [pT], [ht])
        if htf is not None:
            P.act(htf[:, :, b * 128:(b + 1) * 128], pv, AF.Copy, [pT], ["htf"])

    def wload(wt, ncols, src2d):
        wv = wt[:, 0:8 * ncols].rearrange("p (k n) -> p k n", k=8)
        for k in range(8):
            P.dma("gpsimd", wv[:, k, :], src2d[k * 128:(k + 1) * 128, :], ["wsrc"], [wt])
        return wv

    def gemm_fm(pg, wv, c0, m, ht, reads, n=512, t0=0):
        for k in range(8):
            P.mm(pg[0:m, 0:n], wv[:, k, c0:c0 + m], ht[:, k, t0:t0 + n], k == 0, k == 7, reads, [pg])

    def gemm_tm(pg, ht, b, wv, c0, n, reads):
        for k in range(8):
            P.mm(pg[:, 0:n], ht[:, k, b * 128:(b + 1) * 128], wv[:, k, c0:c0 + n], k == 0, k == 7, reads, [pg])

    def head_norm_store(l, h, g, raw, hg_col, gate_chunk, ychunk):
        sq = FT.next()
        P.act(sq[:], raw[:], AF.Square, [raw], [sq])
        pg = PG.next()
        P.mm(pg[:, :], ones_f[:, :], sq[:], True, True, [ones_f, sq], [pg])
        rsd = FT.next()
        P.act(rsd[:], pg[:, :], AF.Sqrt, [pg, eps_t], [rsd], scale=1.0 / 128, bias=eps_t[:, 0:1])
        P.v(lambda e: e.reciprocal(out=rsd[:], in_=rsd[:]), [rsd], [rsd])
        P.v(lambda e: e.scalar_tensor_tensor(out=raw[:], in0=raw[:], scalar=hg_col, in1=rsd[:], op0=ALU.mult,
                                             op1=ALU.mult), [raw, rsd, "hgt"], [raw])
        gt_ = PT.next()
        P.dma("sync", gt_[:], gated[gate_chunk][:, g * 512:(g + 1) * 512], [("gated", gate_chunk)], [gt_])
        st = ST.next()
        P.v(lambda e: e.tensor_tensor(out=st[:], in0=raw[:], in1=gt_[:], op=ALU.mult), [raw, gt_], [st])
        P.dma("sync", Yd[ychunk][:, g * 512:(g + 1) * 512], st[:], [st], [("Yd", ychunk)])

    def load_strip(ty, h, stf, stb):
        src = bass.AP(Frow.tensor, (ty * 8 + h) * NFR, [[1, 128], [1, SU]])
        P.dma("sync", stf, src, [("Frow", ty)], ["stf"])
        P.act(stb, stf, AF.Copy, ["stf"], ["stb"])

    def softmax_attn(ty, QTv, KTv, VTv, use_mask, ybase):
        stf = W[0][:, 0:2 * SU].bitcast(F32)
        stb = W[1][:, 0:SU]
        NMT = _Rot([W[1][:, 3072 + i * 512:3072 + (i + 1) * 512] for i in range(4)])
        nmi = 0
        for h in range(8):
            load_strip(ty, h, stf, stb)
            c, po = h // 2, (h % 2) * 64
            for g in range(NG):
                acc_o = PA[(h * NG + g) % 2 * 2]
                acc_d = PA[(h * NG + g) % 2 * 2 + 1]
                kb_lo = 0 if ty == 0 else max(0, 4 * g - 16)
                kbs = list(range(kb_lo, 4 * g + 4))
                for i, kb in enumerate(kbs):
                    delta = 512 * g - 128 * kb
                    col = min(delta, 1664 if ty == 0 else 2048) + 384
                    pl = PG.next()
                    P.mm(pl[:, :], KTv[po:po + 64, c, kb * 128:(kb + 1) * 128], QTv[po:po + 64, c, g * 512:(g + 1) * 512],
                         True, False, ["KT", "QT"], [pl])
                    P.mm(pl[:, :], exch_b[:, :], stb[:, col:col + 512], False, not use_mask, [exch_b, "stb"], [pl])
                    if use_mask:
                        nm = NMT.next()
                        nk = f"nmt{nmi % 4}"
                        nmi += 1
                        P.dma("sync", nm, nmtd[kb][:, g * 512:(g + 1) * 512], ["nmtd"], [nk])
                        P.mm(pl[:, :], ident_b[:, :], nm, False, True, [ident_b, nk], [pl])
                    pt = PT.next()
                    P.act(pt[:], pl[:, :], AF.Exp, [pl], [pt])
                    P.mm(acc_o[0:64, :], VTv[:, kb, h * 64:(h + 1) * 64], pt[:], i == 0, i == len(kbs) - 1, ["VT", pt], [acc_o])
                    P.mm(acc_d[0:64, :], ones_b[:, 0:64], pt[:], i == 0, i == len(kbs) - 1, [ones_b, pt], [acc_d])
                rec = FT.next()
                P.v(lambda e, rec=rec, acc_d=acc_d: e.reciprocal(out=rec[0:64, :], in_=acc_d[0:64, :]), [acc_d], [rec])
                st = ST.next()
                P.v(lambda e, rec=rec, acc_o=acc_o, st=st: e.tensor_tensor(out=st[0:64, :], in0=acc_o[0:64, :],
                                                                          in1=rec[0:64, :], op=ALU.mult),
                    [acc_o, rec], [st])
                P.dma("sync", Yd[ybase + c][po:po + 64, g * 512:(g + 1) * 512], st[0:64, :], [st], [("Yd", ybase + c)])

    def out_proj_residual(l, wsrc):
        wv = wload(W[0], 1024, wsrc)
        for g in range(NG):
            yg = HT.next()
            for k in range(8):
                P.dma("sync", yg[:, k, :], Yd[k][:, g * 512:(g + 1) * 512], [("Yd", k)], [yg])
            for b in range(4):
                blk = 4 * g + b
                xb = XB.next()
                P.dma("sync", xb[:], xsrc(l)[blk * 128:(blk + 1) * 128, :], [("xs", blk)], [xb])
                hm = HM.next()
                for half in range(2):
                    pg = PG.next()
                    gemm_tm(pg, yg, b, wv, half * 512, 512, [yg, W[0]])
                    P.v(lambda e, hm=hm, pg=pg, half=half: e.tensor_tensor(out=hm[:, half * 512:(half + 1) * 512], in0=pg[:, :],
                                                                            in1=gtB[:, half * 512:(half + 1) * 512], op=ALU.mult),
                        [pg, gtB], [hm])
                P.g(lambda e, hm=hm, xb=xb: e.tensor_tensor(out=xb[:], in0=xb[:], in1=hm[:], op=ALU.add), [xb, hm], [xb])
                P.dma("sync", xs[blk * 128:(blk + 1) * 128, :], xb[:], [xb], [("xs", blk)])

    aT_t = P.sb("aT_t", [128, 128], F32)
    hgt = P.sb("hgt", [128, 4], F32)
    cw = P.sb("cw", [128, 4, 4], F32)
    cb = P.sb("cb", [128, 4], F32)
    gb4 = P.sb("gb4", [4, 4], F32)
    one_c = P.sb("one_c", [128, 1], F32)
    wit = P.sb("wit", [128, NB, 4], F32)
    thr_c = P.sb("thr_c", [128, 1], F32)
    steps = P.sb("steps", [128, NIT], F32)
    P.v(lambda e: e.memset(one_c[:], 1.0), [], [one_c])
    P.v(lambda e: e.memset(thr_c[:], -1e29), [], [thr_c])
    NC = dict(allow_slow_non_contiguous=True)

    def store_gated(pg, func, chunk, g):
        st = ST.next()
        P.act(st[:], pg[:, :], func, [pg], [st])
        P.dma("sync", gated[chunk][:, g * 512:(g + 1) * 512], st[:], [st], [("gated", chunk)])

    def lin_attn(l, kind, QKv, qc0, kc0, VTv, hn_src, gla=None):
        for hh in range(4):
            P.dma("sync", hgt[:, hh:hh + 1], hn_src[:, hh * 128:(hh + 1) * 128].rearrange("o p -> p o"), ["hn"], [hgt], **NC)
        for g in range(int(os.environ.get("DBG_G", NG))):
            if kind == 1:
                QTg, KTg = gla(g)
            for h in range(int(os.environ.get("DBG_H", 4))):
                c, po = h // 2, (h % 2) * 64
                acc_n = PA[(h * NG + g) % 2 * 2]
                acc_d = PA[(h * NG + g) % 2 * 2 + 1]
                if kind == 0:
                    negMB = FT.next()
                    P.dma("sync", negMB[:], bc(rowsd[0][h:h + 1, g * 512:(g + 1) * 512], 512), ["rowsd"], [negMB])
                    emB = FT.next()
                    P.dma("sync", emB[:], bc(rowsd[1][h:h + 1, g * 512:(g + 1) * 512], 512), ["rowsd"], [emB])
                nkb = 4 * g + 4
                for kb in range(nkb):
                    pl = PG.next()
                    if kind == 0:
                        P.mm(pl[:, :], QKv[po:po + 64, kc0 + c, kb * 128:(kb + 1) * 128],
                             QKv[po:po + 64, qc0 + c, g * 512:(g + 1) * 512], True, True, ["QK"], [pl])
                    else:
                        P.mm(pl[:, :], KTg[po:po + 64, c, kb * 128:(kb + 1) * 128], QTg[po:po + 64, c, :], True, True,
                             ["KTg", "QTg"], [pl])
                    pt = PT.next()
                    j = kb - 4 * g
                    if kind == 0:
                        src_ = negMB
                        if j >= 0:
                            tmp = ET.next()
                            P.g(lambda e, tmp=tmp, negMB=negMB, j=j: e.tensor_tensor(out=tmp[:], in0=negMB[:], in1=maskneg[:, j, :],
                                                                                      op=ALU.add), [negMB, maskneg], [tmp])
                            src_ = tmp
                        E = ET.next()
                        P.act(E[:], src_[:], AF.Exp, [src_, aT_t], [E], bias=aT_t[:, kb * 4 + h:kb * 4 + h + 1])
                        P.v(lambda e, pt=pt, pl=pl, E=E: e.scalar_tensor_tensor(out=pt[:], in0=pl[:, :], scalar=0.125, in1=E[:],
                                                                                op0=ALU.mult, op1=ALU.mult), [pl, E], [pt])
                    else:
                        if j >= 0:
                            P.v(lambda e, pt=pt, pl=pl, j=j: e.tensor_tensor(out=pt[:], in0=pl[:, :], in1=mask01[:, j, :], op=ALU.mult),
                                [pl, mask01], [pt])
                        else:
                            P.act(pt[:], pl[:, :], AF.Copy, [pl], [pt])
                    P.mm(acc_n[:, :], VTv[:, kb, h * 128:(h + 1) * 128], pt[:], kb == 0, kb == nkb - 1, ["VT", pt], [acc_n])
                    if kind == 0:
                        P.mm(acc_d[:, :], ones_b[:, :], pt[:], kb == 0, kb == nkb - 1, [ones_b, pt], [acc_d])
                raw = FT.next()
                if kind == 0:
                    dn = FT.next()
                    P.act(dn[:], acc_d[:, :], AF.Abs, [acc_d], [dn])
                    P.v(lambda e, dn=dn, emB=emB: e.tensor_tensor(out=dn[:], in0=dn[:], in1=emB[:], op=ALU.max), [dn, emB], [dn])
                    P.v(lambda e, dn=dn: e.reciprocal(out=dn[:], in_=dn[:]), [dn], [dn])
                    P.v(lambda e, raw=raw, acc_n=acc_n, dn=dn: e.tensor_tensor(out=raw[:], in0=acc_n[:, :], in1=dn[:], op=ALU.mult),
                        [acc_n, dn], [raw])
                else:
                    P.act(raw[:], acc_n[:, :], AF.Copy, [acc_n], [raw])
                head_norm_store(l, h, g, raw, hgt[:, h:h + 1], h, h)

    def layer_ab(l):
        j = l // 2
        win = I["ab_w_in"][j]
        P.dma("gpsimd", maskneg[:], I["maskneg"], ["c_mk"], [maskneg])
        prep_norm(l, False)
        wv = wload(W[0], 1544, win[:, 0:1544])
        QKraw = R[0][:, :].rearrange("p (c t) -> p c t", c=4)
        R1f = R[1][:, :].bitcast(F32)
        IA = R1f[0:4, 0:L]
        FA = R1f[0:4, L:2 * L]
        VA = R[2][:, :].rearrange("p (b n) -> p b n", b=NB)
        for g in range(NG):
            ht = HT.next()
            for b in range(4):
                norm_block(xsrc(l), 4 * g + b, ht, b)
            for cch in range(4):
                pg = PG.next()
                gemm_fm(pg, wv, cch * 128, 128, ht, [W[0], ht])
                P.act(QKraw[:, cch, g * 512:(g + 1) * 512], pg[:, :], AF.Copy, [pg], ["QKraw"])
            for cch in range(4):
                pg = PG.next()
                gemm_fm(pg, wv, 1024 + cch * 128, 128, ht, [W[0], ht])
                store_gated(pg, AF.Sigmoid, cch, g)
            pg = PG.next()
            gemm_fm(pg, wv, 1536, 4, ht, [W[0], ht])
            P.v(lambda e, pg=pg, g=g: e.tensor_copy(out=IA[:, g * 512:(g + 1) * 512], in_=pg[0:4, :]), [pg], ["IA"])
            pg = PG.next()
            gemm_fm(pg, wv, 1540, 4, ht, [W[0], ht])
            P.v(lambda e, pg=pg, g=g: e.tensor_copy(out=FA[:, g * 512:(g + 1) * 512], in_=pg[0:4, :]), [pg], ["FA"])
            for b in range(4):
                pg = PG.next()
                gemm_tm(pg, ht, b, wv, 512, 512, [W[0], ht])
                P.v(lambda e, pg=pg, b=b, g=g: e.tensor_copy(out=VA[:, 4 * g + b, :], in_=pg[:, :]), [pg], ["VT"])
        P.fence()
        chk('A')
        W0f = W[0][:, :].bitcast(F32)
        W1f = W[1][:, :].bitcast(F32)
        NBr = W0f[0:4, 0:L]
        Mr = W1f[0:4, 0:L]
        for tt in range(2):
            P.dma("sync", gb4[:, tt:tt + 1], I["ab_gate_b"][j:j + 1, tt * 4:(tt + 1) * 4].rearrange("o p -> p o"), ["gb"], [gb4], **NC)
        P.v(lambda e: e.tensor_scalar(out=gb4[:, 2:3], in0=gb4[:, 1:2], scalar1=-1.0, scalar2=None, op0=ALU.mult), [gb4], [gb4])
        P.act(FA, FA, AF.Exp, ["FA", gb4], ["FA"], scale=-1.0, bias=gb4[:, 2:3])
        P.act(FA, FA, AF.Ln, ["FA", one_c], ["FA"], bias=one_c[0:4, 0:1])
        P.v(lambda e: e.tensor_tensor_scan(out=NBr, data0=one_c[0:4, 0:1].to_broadcast([4, L]), data1=FA, initial=0.0,
                                           op0=ALU.mult, op1=ALU.add), ["FA", one_c], ["NBr"])
        P.v(lambda e: e.scalar_tensor_tensor(out=IA, in0=IA, scalar=gb4[:, 0:1], in1=NBr, op0=ALU.add, op1=ALU.add),
            ["IA", gb4, "NBr"], ["IA"])
        P.v(lambda e: e.tensor_tensor_scan(out=Mr, data0=IA, data1=IA, initial=-1e30, op0=ALU.max, op1=ALU.max), ["IA"], ["Mr"])
        P.v(lambda e: e.tensor_scalar(out=FA, in0=Mr, scalar1=-1.0, scalar2=None, op0=ALU.mult), ["Mr", "FA"], ["FA"])
        P.dma("sync", rowsd[0], FA, ["FA"], ["rowsd"])
        P.v(lambda e: e.tensor_tensor(out=Mr, in0=NBr, in1=Mr, op=ALU.subtract), ["NBr", "Mr"], ["Mr"])
        P.act(Mr, Mr, AF.Exp, ["Mr"], ["Mr"])
        P.dma("sync", rowsd[1], Mr, ["Mr"], ["rowsd"])
        for b in range(NB):
            P.tr(pT[:, b * 4:(b + 1) * 4], IA[:, b * 128:(b + 1) * 128], ident_f[0:4, 0:4], ["IA", ident_f], [pT])
        P.v(lambda e: e.tensor_copy(out=aT_t[:], in_=pT[:, 0:128]), [pT], [aT_t])
        P.fence()
        chk('G')
        for cc in range(4):
            for tj in range(4):
                P.dma("sync", cw[:, cc, tj:tj + 1], I["ab_conv_w"][j][tj:tj + 1, cc * 128:(cc + 1) * 128].rearrange("o p -> p o"), ["cw"], [cw], **NC)
            P.dma("sync", cb[:, cc:cc + 1], I["ab_conv_b"][j:j + 1, cc * 128:(cc + 1) * 128].rearrange("o p -> p o"), ["cb"], [cb], **NC)
        QKc = R[1][:, :].rearrange("p (c t) -> p c t", c=4)
        for cch in range(4):
            for sg in range(4):
                a0 = sg * 1024
                acc = HM.next()
                P.v(lambda e, acc=acc, cch=cch, a0=a0: e.tensor_scalar(out=acc[:], in0=QKraw[:, cch, a0:a0 + 1024],
                                                                       scalar1=cw[:, cch, 3:4], scalar2=None, op0=ALU.mult),
                    ["QKraw", cw], [acc])
                for tj in (2, 1, 0):
                    s_ = 3 - tj
                    lo = max(a0, s_)
                    P.v(lambda e, acc=acc, cch=cch, a0=a0, lo=lo, s_=s_, tj=tj: e.scalar_tensor_tensor(
                        out=acc[:, lo - a0:1024], in0=QKraw[:, cch, lo - s_:a0 + 1024 - s_], scalar=cw[:, cch, tj:tj + 1],
                        in1=acc[:, lo - a0:1024], op0=ALU.mult, op1=ALU.add), ["QKraw", cw, acc], [acc])
                P.act(QKc[:, cch, a0:a0 + 1024], acc[:], AF.Silu, [acc, cb], ["QK"], bias=cb[:, cch:cch + 1])
        P.fence()
        chk('C')
        lin_attn(l, 0, QKc, 0, 2, VA, I["ab_hnorm_g"][j:j + 1, :])
        P.fence()
        chk('M')
        wvb = W[0][:, 0:8 * 388].rearrange("p (k n) -> p k n", k=8)
        for k in range(8):
            rows = slice(k * 128, (k + 1) * 128)
            P.dma("gpsimd", wvb[:, k, 0:256], win[rows, 2184:2440], ["wsrc"], [W[0]])
            P.dma("gpsimd", wvb[:, k, 256:320], win[rows, 2440:2504], ["wsrc"], [W[0]])
            P.dma("gpsimd", wvb[:, k, 320:384], win[rows, 2440:2504], ["wsrc"], [W[0]])
            P.dma("gpsimd", wvb[:, k, 384:388], win[rows, 2504:2508], ["wsrc"], [W[0]])
        QI = R[0][:, 0:8192].rearrange("p (c t) -> p c t", c=2)
        KI2 = R[0][:, 8192:12288]
        for g in range(NG):
            ht = HT.next()
            for b in range(4):
                norm_block(xsrc(l), 4 * g + b, ht, b)
            for cch in range(2):
                pg = PG.next()
                gemm_fm(pg, wvb, cch * 128, 128, ht, [W[0], ht])
                P.act(QI[:, cch, g * 512:(g + 1) * 512], pg[:, :], AF.Copy, [pg], ["QI"])
            pg = PG.next()
            gemm_fm(pg, wvb, 256, 128, ht, [W[0], ht])
            P.act(KI2[:, g * 512:(g + 1) * 512], pg[:, :], AF.Copy, [pg], ["KI2"])
            for b in range(4):
                pg = PG.next()
                gemm_tm(pg, ht, b, wvb, 384, 4, [W[0], ht])
                P.v(lambda e, pg=pg, b=b, g=g: e.tensor_scalar(out=wit[:, 4 * g + b, :], in0=pg[:, 0:4], scalar1=0.5, scalar2=None,
                                                               op0=ALU.mult), [pg], [wit])
        P.fence()
        chk('B1')
        S = W[0][:, 0:8192].bitcast(F32)
        nmb = W[1][:, 0:L]
        NMs = W[1][:, L:2 * L].rearrange("p (k t) -> p k t", k=NB)
        jkb = R[1][:, 0:L]
        pTb = pT[:, :].bitcast(BF16)
        for qb in range(NB):
            nk = (qb + 1) * 128
            for kc in range((nk + 511) // 512):
                w_ = min(512, nk - kc * 512)
                for h4 in range(4):
                    c, po = h4 // 2, (h4 % 2) * 64
                    pl = PG.next()
                    P.mm(pl[:, 0:w_], QI[po:po + 64, c, qb * 128:(qb + 1) * 128], KI2[po:po + 64, kc * 512:kc * 512 + w_], True, True,
                         ["QI", "KI2"], [pl])
                    sl = S[:, kc * 512:kc * 512 + w_]
                    if h4 == 0:
                        P.v(lambda e, sl=sl, pl=pl, w_=w_, qb=qb: e.tensor_scalar(out=sl, in0=pl[:, 0:w_], scalar1=0.0,
                                                                                 scalar2=wit[:, qb, 0:1], op0=ALU.max, op1=ALU.mult),
                            [pl, wit], ["S"])
                    else:
                        ft = FT.next()
                        P.act(ft[:, 0:w_], pl[:, 0:w_], AF.Relu, [pl], [ft])
                        P.v(lambda e, sl=sl, ft=ft, w_=w_, qb=qb, h4=h4: e.scalar_tensor_tensor(
                            out=sl, in0=ft[:, 0:w_], scalar=wit[:, qb, h4:h4 + 1], in1=sl, op0=ALU.mult, op1=ALU.add),
                            [ft, wit, "S"], ["S"])
            P.g(lambda e, qb=qb: e.tensor_tensor(out=S[:, qb * 128:(qb + 1) * 128], in0=S[:, qb * 128:(qb + 1) * 128], in1=cmT[:],
                                                 op=ALU.add), ["S", cmT], ["S"])
            if qb >= 2:
                lo = sm[:, 8:9]
                P.v(lambda e, nk=nk: e.tensor_reduce(out=sm[:, 9:10], in_=S[:, 0:nk], axis=AX.X, op=ALU.max), ["S"], ["bis"])
                P.v(lambda e: e.tensor_reduce(out=sm[:, 8:9], in_=S[:, 0:256], axis=AX.X, op=ALU.min), ["S", "bis"], ["bis"])
                P.v(lambda e: e.scalar_tensor_tensor(out=sm[:, 10:11], in0=sm[:, 9:10], scalar=1.0, in1=sm[:, 8:9], op0=ALU.add,
                                                     op1=ALU.subtract), ["bis"], ["bis"])
                P.v(lambda e: e.tensor_scalar(out=steps[:], in0=ckt[:], scalar1=sm[:, 10:11], scalar2=None, op0=ALU.mult),
                    ["bis", ckt], [steps])
                for it in range(NIT):
                    P.v(lambda e, it=it: e.tensor_tensor(out=sm[:, 11:12], in0=sm[:, 8:9], in1=steps[:, it:it + 1], op=ALU.add),
                        ["bis", steps], ["bis"])
                    P.v(lambda e, nk=nk: e.tensor_scalar(out=jkb[:, 0:nk], in0=S[:, 0:nk], scalar1=sm[:, 11:12], scalar2=0.0,
                                                         op0=ALU.is_ge, op1=ALU.add, accum_out=sm[:, 12:13]), ["S", "bis"], ["jkb", "bis"])
                    P.v(lambda e, it=it: e.tensor_scalar(out=sm[:, 13:14], in0=sm[:, 12:13], scalar1=TOPK - 0.5,
                                                         scalar2=steps[:, it:it + 1], op0=ALU.is_ge, op1=ALU.mult), ["bis", steps], ["bis"])
                    P.v(lambda e: e.tensor_tensor(out=sm[:, 8:9], in0=sm[:, 8:9], in1=sm[:, 13:14], op=ALU.add), ["bis"], ["bis"])
                thr = sm[:, 8:9]
            else:
                thr = thr_c[:, 0:1]
            P.v(lambda e, nk=nk, thr=thr: e.tensor_scalar(out=nmb[:, 0:nk], in0=S[:, 0:nk], scalar1=thr, scalar2=NEG, op0=ALU.is_lt,
                                                          op1=ALU.mult), ["S", "bis", thr_c], ["nmb"])
            for kb in range(qb + 1):
                P.tr(pTb[:, (kb % 4) * 128:(kb % 4 + 1) * 128], nmb[:, kb * 128:(kb + 1) * 128], ident_b[:, :], ["nmb", ident_b], [pT])
                if kb % 4 == 3 or kb == qb:
                    k0 = kb - kb % 4
                    n_ = kb - k0 + 1
                    P.act(NMs[:, k0:k0 + n_, :], pTb[:, 0:n_ * 128].rearrange("p (k t) -> p k t", k=n_), AF.Copy, [pT], ["NMs"])
            P.dma("sync", nmtd[0:qb + 1, :, qb * 128:(qb + 1) * 128].rearrange("k s t -> s k t"), NMs[:, 0:qb + 1, :], ["NMs"], ["nmtd"])
        P.fence()
        chk('IDX')
        wvc = W[0][:, 0:8 * 640].rearrange("p (k n) -> p k n", k=8)
        for k in range(8):
            P.dma("gpsimd", wvc[:, k, :], win[k * 128:(k + 1) * 128, 1544:2184], ["wsrc"], [W[0]])
        wuk = W[1][:, 0:512]
        wuv = W[1][:, 512:1024]
        P.dma("gpsimd", wuk, I["ab_w_uk"][j], ["wsrc"], [W[1]])
        P.dma("gpsimd", wuv, I["ab_w_uv"][j], ["wsrc"], [W[1]])
        ckvT = W[1][:, 1024:1024 + L]
        QB = R[0][:, :].rearrange("p (c t) -> p c t", c=4)
        KH = R[1][:, :].rearrange("p (c t) -> p c t", c=4)
        VH = R[2][:, :].rearrange("p (b n) -> p b n", b=NB)
        for g in range(NG):
            ht = HT.next()
            for b in range(4):
                norm_block(xsrc(l), 4 * g + b, ht, b)
            for cch in range(4):
                pg = PG.next()
                gemm_fm(pg, wvc, cch * 128, 128, ht, [W[0], ht])
                P.act(QB[:, cch, g * 512:(g + 1) * 512], pg[:, :], AF.Copy, [pg], ["QT"], scale=0.125)
            pg = PG.next()
            gemm_fm(pg, wvc, 512, 128, ht, [W[0], ht])
            P.act(ckvT[:, g * 512:(g + 1) * 512], pg[:, :], AF.Copy, [pg], ["ckvT"])
            for cch in range(4):
                pg = PG.next()
                P.mm(pg[:, :], wuk[:, cch * 128:(cch + 1) * 128], ckvT[:, g * 512:(g + 1) * 512], True, True, [W[1], "ckvT"], [pg])
                P.v(lambda e, pg=pg, cch=cch, g=g: e.tensor_copy(out=KH[:, cch, g * 512:(g + 1) * 512], in_=pg[:, :]), [pg], ["KT"])
            for b in range(4):
                pg = PG.next()
                P.mm(pg[:, :], ckvT[:, (4 * g + b) * 128:(4 * g + b + 1) * 128], wuv, True, True, [W[1], "ckvT"], [pg])
                P.v(lambda e, pg=pg, b=b, g=g: e.tensor_copy(out=VH[:, 4 * g + b, :], in_=pg[:, :]), [pg], ["VT"])
        P.fence()
        chk('B2')
        softmax_attn(0, QB, KH, VH, True, 4)
        P.fence()
        chk('ATT')
        prep_norm(l, False)
        out_proj_residual(l, I["ab_w_out"][j])
        P.fence()

    zt = PT.next()
    P.v(lambda e: e.memset(zt[:], 0.0), [], [zt])
    for kb in range(NB):
        r_ = kb % 4
        if r_:
            P.dma("sync", nmtd[kb][:, (kb - r_) * 128:kb * 128], zt[:, 0:r_ * 128], [zt], ["nmtd"])
    P.fence()

    def layer_cd(l):
        j = l // 2
        win = I["cd_w_in"][j]
        P.dma("gpsimd", maskneg[:], I["maskneg"], ["c_mk"], [maskneg])
        P.v(lambda e: e.tensor_scalar(out=mask01[:], in0=maskneg[:], scalar1=-1.0, scalar2=None, op0=ALU.is_ge), [maskneg], [maskneg])
        prep_norm(l, False)
        wv = wload(W[0], 1552, win[:, 0:1552])
        QKC = R[0][:, :].rearrange("p (c t) -> p c t", c=4)
        LS = R[1][:, :].bitcast(F32).rearrange("p (c t) -> p c t", c=2)
        VC = R[2][:, :].rearrange("p (b n) -> p b n", b=NB)
        wal = P_wal
        P.dma("sync", wal, I["cd_w_alpha"][j], ["wal"], ["wal"])
        for cc in range(2):
            P.dma("sync", cb[:, cc:cc + 1], I["cd_b_alpha"][j:j + 1, cc * 128:(cc + 1) * 128].rearrange("o p -> p o"), ["cb"], [cb], **NC)
        P.v(lambda e: e.tensor_scalar(out=cb[:, 2:4], in0=cb[:, 0:2], scalar1=-1.0, scalar2=None, op0=ALU.mult), [cb], [cb])
        for g in range(NG):
            ht = HT.next()
            for b in range(4):
                norm_block(xsrc(l), 4 * g + b, ht, b)
            for cch in range(4):
                pg = PG.next()
                gemm_fm(pg, wv, cch * 128, 128, ht, [W[0], ht])
                P.act(QKC[:, cch, g * 512:(g + 1) * 512], pg[:, :], AF.Copy, [pg], ["QK"])
            for cch in range(4):
                pg = PG.next()
                gemm_fm(pg, wv, 1040 + cch * 128, 128, ht, [W[0], ht])
                store_gated(pg, AF.Silu, cch, g)
            pg = PG.next()
            gemm_fm(pg, wv, 1024, 16, ht, [W[0], ht])
            gct = FT.next()
            P.v(lambda e, pg=pg, gct=gct: e.tensor_copy(out=gct[0:16, :], in_=pg[0:16, :]), [pg], [gct])
            for cch in range(2):
                pg = PG.next()
                P.mm(pg[:, :], wal[:, cch * 128:(cch + 1) * 128], gct[0:16, :], True, True, ["wal", gct], [pg])
                et = ET.next()
                P.act(et[:], pg[:, :], AF.Exp, [pg, cb], [et], scale=-1.0, bias=cb[:, 2 + cch:3 + cch])
                P.act(LS[:, cch, g * 512:(g + 1) * 512], et[:], AF.Ln, [et, one_c], ["LS"], bias=one_c[:, 0:1])
            for b in range(4):
                pg = PG.next()
                gemm_tm(pg, ht, b, wv, 512, 512, [W[0], ht])
                P.v(lambda e, pg=pg, b=b, g=g: e.tensor_copy(out=VC[:, 4 * g + b, :], in_=pg[:, :]), [pg], ["VT"])
        P.fence()
        chk('CA')
        nBs = [W[0][:, 0:8192].bitcast(F32), W[1][:, 0:8192].bitcast(F32)]
        for cch in range(2):
            P.v(lambda e, cch=cch: e.tensor_tensor_scan(out=nBs[cch], data0=one_c[:, 0:1].to_broadcast([128, L]), data1=LS[:, cch, :],
                                                        initial=0.0, op0=ALU.mult, op1=ALU.add), ["LS", one_c], [("nB", cch)])
        P.fence()
        KTg = R[1][:, 0:8192].rearrange("p (c t) -> p c t", c=2)
        Etmp = R[1][:, 8192:16384].bitcast(F32)
        QTg = W[1][:, 8192:9216].rearrange("p (c t) -> p c t", c=2)

        def gla(g):
            n = (g + 1) * 512
            for cch in range(2):
                if g == 0:
                    P.v(lambda e: e.memset(sm[:, 16:18], 0.0), [], ["bq"])
                    P.v(lambda e: e.memset(sm[:, 18:20], 0.0), ["bq"], ["bq"])
                else:
                    r_ = g * 512 - 1
                    P.v(lambda e, cch=cch, r_=r_: e.tensor_scalar(out=sm[:, 16 + cch:17 + cch], in0=nBs[cch][:, r_:r_ + 1], scalar1=1.0 / 16,
                                                                  scalar2=None, op0=ALU.mult), [("nB", cch)], ["bq"])
                    P.v(lambda e, cch=cch, r_=r_: e.tensor_scalar(out=sm[:, 18 + cch:19 + cch], in0=nBs[cch][:, r_:r_ + 1], scalar1=-1.0 / 16,
                                                                  scalar2=None, op0=ALU.mult), [("nB", cch), "bq"], ["bq"])
                et = ET.next()
                P.act(et[:], nBs[cch][:, g * 512:(g + 1) * 512], AF.Exp, [("nB", cch), "bq"], [et], scale=-1.0 / 16,
                      bias=sm[:, 16 + cch:17 + cch])
                P.v(lambda e, cch=cch, et=et, g=g: e.scalar_tensor_tensor(out=QTg[:, cch, :], in0=QKC[:, cch, g * 512:(g + 1) * 512],
                                                                          scalar=0.125, in1=et[:], op0=ALU.mult, op1=ALU.mult),
                    ["QK", et], ["QTg"])
                P.act(Etmp[:, 0:n], nBs[cch][:, 0:n], AF.Exp, [("nB", cch), "bq"], ["Etmp"], scale=1.0 / 16, bias=sm[:, 18 + cch:19 + cch])
                P.v(lambda e, cch=cch, n=n: e.tensor_tensor(out=KTg[:, cch, 0:n], in0=QKC[:, 2 + cch, 0:n], in1=Etmp[:, 0:n], op=ALU.mult),
                    ["QK", "Etmp"], ["KTg"])
            return QTg, KTg

        lin_attn(l, 1, None, 0, 0, VC, I["cd_hnorm_g"][j:j + 1, :], gla=gla)
        P.fence()
        chk('CG')
        wvd = wload(W[0], 1536, win[:, 1552:3088])
        QD = R[0][:, :].rearrange("p (c t) -> p c t", c=4)
        KD = R[1][:, :].rearrange("p (c t) -> p c t", c=4)
        VD = R[2][:, :].rearrange("p (b n) -> p b n", b=NB)
        for g in range(NG):
            ht = HT.next()
            for b in range(4):
                norm_block(xsrc(l), 4 * g + b, ht, b)
            for cch in range(4):
                pg = PG.next()
                gemm_fm(pg, wvd, cch * 128, 128, ht, [W[0], ht])
                P.act(QD[:, cch, g * 512:(g + 1) * 512], pg[:, :], AF.Copy, [pg], ["QT"], scale=0.125)
            for cch in range(4):
                pg = PG.next()
                gemm_fm(pg, wvd, 512 + cch * 128, 128, ht, [W[0], ht])
                P.v(lambda e, pg=pg, cch=cch, g=g: e.tensor_copy(out=KD[:, cch, g * 512:(g + 1) * 512], in_=pg[:, :]), [pg], ["KT"])
            for b in range(4):
                pg = PG.next()
                gemm_tm(pg, ht, b, wvd, 1024, 512, [W[0], ht])
                P.v(lambda e, pg=pg, b=b, g=g: e.tensor_copy(out=VD[:, 4 * g + b, :], in_=pg[:, :]), [pg], ["VT"])
        P.fence()
        chk('CD')
        softmax_attn(1, QD, KD, VD, False, 4)
        P.fence()
        chk('CS')
        prep_norm(l, False)
        out_proj_residual(l, I["cd_w_out"][j])
        P.fence()

    P_wal = W[1][0:16, 0:512].bitcast(F32)
    wr_t = P.sb("wr_t", [128, 8, 20], F32)

    rbias = P.sb("rbias", [128, 20], F32)
    R0f = R[0][:, :].bitcast(F32)
    LG = R0f[:, 4096:4176].rearrange("p (b n) -> p b n", b=4)
    GT = R0f[:, 4224:4480].rearrange("p (b n) -> p b n", b=16)
    rt = R0f[:, 4480:4736].rearrange("p (b n) -> p b n", b=4)

    def moe(l):
        prep_norm(l, True)
        wr = wr_t
        P.dma("sync", wr[:, :, 0:4], I["moe_w_coarse"][l].rearrange("(k p) n -> p k n", p=128), ["wr"], ["wr"], **NC)
        for gi in range(4):
            P.dma("sync", wr[:, :, 4 + gi * 4:8 + gi * 4], I["moe_w_fine"][l][gi].rearrange("(k p) n -> p k n", p=128), ["wr"], ["wr"], **NC)
        P.dma("sync", rbias[:, 0:4], bc(I["moe_b_coarse"][l:l + 1, :], 4), ["rb"], [rbias])
        P.dma("sync", rbias[:, 4:20], bc(I["moe_b_fine"][l:l + 1, :], 16), ["rb"], [rbias])
        P.fence()
        chk('R0')
        HTF = R[0][:, 0:8192].bitcast(F32).rearrange("p (k t) -> p k t", k=8)
        for half in range(2):
            HH = R[1][:, :].rearrange("p (k t) -> p k t", k=8)
            YA = R[2]
            for gg in range(4):
                g = half * 4 + gg
                ht = HT.next()
                for b in range(4):
                    norm_block(xs, 4 * g + b, ht, b, htf=(None if os.environ.get('DBG_NOHTF') else HTF))
                chk('R1a')
                P.g(lambda e, ht=ht, gg=gg: e.tensor_copy(out=HH[:, :, gg * 512:(gg + 1) * 512], in_=ht[:, :, :]), [ht], ["HH"])
                chk('R1b')
                for b in range(4):
                    pg = PG.next()
                    for k in range(8):
                        P.mm(pg[:, 0:20], HTF[:, k, b * 128:(b + 1) * 128], wr[:, k, :], k == 0, k == 7, ["htf", "wr"], [pg])
                    P.v(lambda e, pg=pg, b=b: e.tensor_tensor(out=LG[:, b, :], in0=pg[:, 0:20], in1=rbias[:], op=ALU.add), [pg, rbias], ["LG"])
                chk('R1')
                lc = LG[:, :, 0:4]
                lf = LG[:, :, 4:20]
                cmax = rt[:, :, 0:1]
                ec = rt[:, :, 1:5]
                csum = rt[:, :, 5:6]
                ohg = rt[:, :, 6:10]
                msk = rt[:, :, 10:26]
                v1 = rt[:, :, 26:27]
                oh1 = rt[:, :, 27:43]
                v2 = rt[:, :, 43:44]
                p1 = rt[:, :, 44:45]
                p2 = rt[:, :, 45:46]
                oh2 = rt[:, :, 46:62]
                RT = ["rt", "LG"]
                P.v(lambda e: e.tensor_reduce(out=cmax, in_=lc, axis=AX.X, op=ALU.max), RT, ["rt"])
                P.v(lambda e: e.tensor_tensor(out=ec, in0=lc, in1=cmax.to_broadcast([128, 4, 4]), op=ALU.subtract), RT, ["rt"])
                P.v(lambda e: e.tensor_scalar(out=ohg, in0=ec, scalar1=0.0, scalar2=None, op0=ALU.is_ge), RT, ["rt"])
                P.act(ec, ec, AF.Exp, RT, ["rt"])
                P.v(lambda e: e.tensor_reduce(out=csum, in_=ec, axis=AX.X, op=ALU.add), RT, ["rt"])
                P.v(lambda e: e.reciprocal(out=csum, in_=csum), RT, ["rt"])
                P.v(lambda e: e.tensor_scalar(out=ohg, in0=ohg, scalar1=-1.0, scalar2=1e30, op0=ALU.add, op1=ALU.mult), RT, ["rt"])
                P.v(lambda e: e.tensor_tensor(out=msk.rearrange("p b (g e) -> p b g e", e=4), in0=lf.rearrange("p b (g e) -> p b g e", e=4),
                                              in1=ohg.unsqueeze(3).to_broadcast([128, 4, 4, 4]), op=ALU.add), RT, ["rt"])
                P.v(lambda e: e.tensor_reduce(out=v1, in_=msk, axis=AX.X, op=ALU.max), RT, ["rt"])
                P.v(lambda e: e.tensor_tensor(out=oh1, in0=msk, in1=v1.to_broadcast([128, 4, 16]), op=ALU.is_ge), RT, ["rt"])
                P.v(lambda e: e.scalar_tensor_tensor(out=msk, in0=oh1, scalar=-1e30, in1=msk, op0=ALU.mult, op1=ALU.add), RT, ["rt"])
                P.v(lambda e: e.tensor_reduce(out=v2, in_=msk, axis=AX.X, op=ALU.max), RT, ["rt"])
                P.v(lambda e: e.tensor_tensor(out=oh2, in0=msk, in1=v2.to_broadcast([128, 4, 16]), op=ALU.is_ge), RT, ["rt"])
                P.v(lambda e: e.tensor_tensor(out=p1, in0=v1, in1=v2, op=ALU.subtract), RT, ["rt"])
                P.act(p1, p1, AF.Sigmoid, RT, ["rt"])
                P.v(lambda e: e.tensor_scalar(out=p2, in0=p1, scalar1=-1.0, scalar2=1.0, op0=ALU.mult, op1=ALU.add), RT, ["rt"])
                P.v(lambda e: e.tensor_tensor(out=p1, in0=p1, in1=csum, op=ALU.mult), RT, ["rt"])
                P.v(lambda e: e.tensor_tensor(out=p2, in0=p2, in1=csum, op=ALU.mult), RT, ["rt"])
                P.v(lambda e: e.tensor_tensor(out=oh1, in0=oh1, in1=p1.to_broadcast([128, 4, 16]), op=ALU.mult), RT, ["rt"])
                P.v(lambda e: e.tensor_tensor(out=oh2, in0=oh2, in1=p2.to_broadcast([128, 4, 16]), op=ALU.mult), RT, ["rt"])
                P.v(lambda e, gg=gg: e.tensor_tensor(out=GT[:, gg * 4:(gg + 1) * 4, :], in0=oh1, in1=oh2, op=ALU.add), RT, ["GT"])
            chk('R2')
            YAf = R[2][:, :].bitcast(F32)
            for q4 in range(2):
                for ex in range(int(os.environ.get('DBG_EX', 16))):
                    wt = W[ex % 2]
                    wg = wt[:, 0:4096].rearrange("p (k n) -> p k n", k=8)
                    wu = wt[:, 4096:8192].rearrange("p (k n) -> p k n", k=8)
                    wd = wt[:, 8192:12288].rearrange("p (k n) -> p k n", k=4)
                    for k in range(8):
                        P.dma("gpsimd", wg[:, k, :], I["moe_w_gate"][l][ex][k * 128:(k + 1) * 128, :], ["wsrc"], [wt])
                        P.dma("gpsimd", wu[:, k, :], I["moe_w_up"][l][ex][k * 128:(k + 1) * 128, :], ["wsrc"], [wt])
                    for k in range(4):
                        P.dma("gpsimd", wd[:, k, :], I["moe_w_down"][l][ex][k * 128:(k + 1) * 128, :], ["wsrc"], [wt])
                    for g2 in range(2):
                        t0 = q4 * 1024 + g2 * 512
                        aT = HT.next()
                        for fc in range(4):
                            pg = PG.next()
                            gemm_fm(pg, wg, fc * 128, 128, HH, [wt, "HH"], t0=t0)
                            pu = PA[fc % 2]
                            gemm_fm(pu, wu, fc * 128, 128, HH, [wt, "HH"], t0=t0)
                            sg_ = PT.next()
                            P.act(sg_[:], pg[:, :], AF.Silu, [pg], [sg_])
                            P.v(lambda e, aT=aT, fc=fc, sg_=sg_, pu=pu: e.tensor_tensor(out=aT[:, fc, :], in0=pu[:, :], in1=sg_[:], op=ALU.mult),
                                [pu, sg_], [aT])
                        for b in range(4):
                            bi = g2 * 4 + b
                            bh = q4 * 8 + bi
                            for hf in range(2):
                                py = PA[2 + (b * 2 + hf) % 2]
                                for fc in range(4):
                                    P.mm(py[:, :], aT[:, fc, b * 128:(b + 1) * 128], wd[:, fc, hf * 512:(hf + 1) * 512], fc == 0, fc == 3,
                                         [aT, wt], [py])
                                ysl = YAf[:, bi * 1024 + hf * 512:bi * 1024 + (hf + 1) * 512]
                                if ex == 0:
                                    P.v(lambda e, ysl=ysl, py=py, bh=bh, ex=ex: e.tensor_scalar(out=ysl, in0=py[:, :], scalar1=GT[:, bh, ex:ex + 1],
                                                                                                scalar2=None, op0=ALU.mult), [py, "GT"], [("YA", bi)])
                                else:
                                    P.v(lambda e, ysl=ysl, py=py, bh=bh, ex=ex: e.scalar_tensor_tensor(out=ysl, in0=py[:, :], scalar=GT[:, bh, ex:ex + 1],
                                                                                                       in1=ysl, op0=ALU.mult, op1=ALU.add),
                                        [py, "GT", ("YA", bi)], [("YA", bi)])
                for bi in range(8):
                    blk = half * 16 + q4 * 8 + bi
                    xb = XB.next()
                    P.dma("sync", xb[:], xs[blk * 128:(blk + 1) * 128, :], [("xs", blk)], [xb])
                    ysl = YAf[:, bi * 1024:(bi + 1) * 1024]
                    P.v(lambda e, ysl=ysl: e.tensor_tensor(out=ysl, in0=ysl, in1=gtB[:], op=ALU.mult), [("YA", bi), gtB], [("YA", bi)])
                    P.g(lambda e, xb=xb, ysl=ysl: e.tensor_tensor(out=xb[:], in0=xb[:], in1=ysl, op=ALU.add), [xb, ("YA", bi)], [xb])
                    P.dma("sync", xs[blk * 128:(blk + 1) * 128, :], xb[:], [xb], [("xs", blk)])
            P.fence()

    try:
        if only == "moe":
            for blk in range(NB):
                xb = XB.next()
                P.dma("sync", xb[:], I["x"][blk * 128:(blk + 1) * 128, :], ["xin"], [xb])
                P.dma("sync", xs[blk * 128:(blk + 1) * 128, :], xb[:], [xb], [("xs", blk)])
            P.fence()
            moe(0)
            raise _Stop()
        if only == "cd":
            for blk in range(NB):
                xb = XB.next()
                P.dma("sync", xb[:], I["x"][blk * 128:(blk + 1) * 128, :], ["xin"], [xb])
                P.dma("sync", xs[blk * 128:(blk + 1) * 128, :], xb[:], [xb], [("xs", blk)])
            P.fence()
            layer_cd(1)
            raise _Stop()
        for l in range(nlayers):
            if l % 2 == 0:
                layer_ab(l)
            else:
                layer_cd(l)
            chk('MIX')
            moe(l)
    except _Stop:
        P.fence()

    P.dma("sync", gsB[:], bc(I["g_final"][0:1, :]), ["g"], [gsB])
    for blk in range(NB):
        xb = XB.next()
        P.dma("sync", xb[:], (xs if nlayers > 0 else I["x"])[blk * 128:(blk + 1) * 128, :], [("xs", blk)], [xb])
        P.act(junk, xb[:], AF.Square, [xb], [ET.t[0], "ss"], accum_out=sm[:, 0:1])
        P.v(lambda e: e.tensor_scalar(out=sm[:, 1:2], in0=sm[:, 0:1], scalar1=1.0 / D, scalar2=1e-6, op0=ALU.mult, op1=ALU.add), ["ss"], ["rs"])
        P.act(sm[:, 1:2], sm[:, 1:2], AF.Sqrt, ["rs"], ["rs"])
        P.v(lambda e: e.reciprocal(out=sm[:, 1:2], in_=sm[:, 1:2]), ["rs"], ["rs"])
        hm = HM.next()
        P.v(lambda e, hm=hm, xb=xb: e.scalar_tensor_tensor(out=hm[:], in0=xb[:], scalar=sm[:, 1:2], in1=gsB[:], op0=ALU.mult, op1=ALU.mult),
            [xb, "rs", gsB], [hm])
        P.dma("sync", out[blk * 128:(blk + 1) * 128, :], hm[:], [hm], ["out"])
    P.finish()
    return nc


_CACHE = {}


def kernel(**inputs):
    f32 = lambda a: np.ascontiguousarray(np.asarray(a, dtype=np.float32))
    inp = {k: f32(v) for k, v in inputs.items()}
    consts = _host_consts()
    shared = {}
    for k in ("w_ada", "b_ada", "g_mix", "g_ffn", "rel_bias", "ab_w_in", "ab_conv_w", "ab_conv_b", "ab_hnorm_g", "ab_w_out",
              "cd_w_in", "cd_w_alpha", "cd_b_alpha", "cd_hnorm_g", "cd_w_out", "moe_w_coarse", "moe_b_coarse", "moe_w_fine"):
        shared[k] = inp[k]
    shared["g_final"] = inp["g_final"].reshape(1, D)
    shared["ab_gate_b"] = inp["ab_gate_b"].reshape(2, 8)
    shared["ab_w_uk"] = inp["ab_w_uk"].reshape(2, 128, 512)
    shared["ab_w_uv"] = inp["ab_w_uv"].reshape(2, 128, 512)
    shared["moe_b_fine"] = inp["moe_b_fine"].reshape(4, 16)
    shared["moe_w_gate"] = inp["moe_w_gate"].reshape(4, 16, D, 512)
    shared["moe_w_up"] = inp["moe_w_up"].reshape(4, 16, D, 512)
    shared["moe_w_down"] = inp["moe_w_down"].reshape(4, 16, 512, D)
    shared.update(consts)
    if "nc" not in _CACHE:
        _CACHE["nc"] = build_nc()
    nc = _CACHE["nc"]
    in_maps = []
    for b in range(8):
        m = dict(shared)
        m["x"] = inp["x"][b]
        m["c"] = inp["c"][b].reshape(8, 128)
        in_maps.append(m)
    res = run_bass_kernel_spmd(nc, in_maps, core_ids=list(range(8)))
    return np.stack([np.asarray(r["out"], dtype=np.float32) for r in res.results], axis=0)
```

```python
import math
import os
from contextlib import ExitStack
import numpy as np
import concourse.bass as bass
import concourse.mybir as mybir
from concourse.bass_utils import run_bass_kernel_spmd

F32 = mybir.dt.float32
BF16 = mybir.dt.bfloat16
ALU = mybir.AluOpType
AF = mybir.ActivationFunctionType
AX = mybir.AxisListType


class _Op:
    __slots__ = ("eng", "fn", "deps", "signal", "sidx", "is_dma", "dslot", "dval", "event")

    def __init__(self, eng, fn, is_dma):
        self.eng = eng
        self.fn = fn
        self.is_dma = is_dma
        self.deps = []
        self.signal = False
        self.sidx = 0
        self.dslot = 0
        self.dval = 0
        self.event = None


def _key(k):
    if isinstance(k, str):
        return k
    if isinstance(k, tuple):
        return _key(k[0]) + "#" + "#".join(str(i) for i in k[1:])
    return k.name


class Prog:
    ENGS = ("tensor", "vector", "scalar", "gpsimd", "sync")
    EPOCH = 16000
    NDSEM = 8

    def __init__(self, nc):
        self.nc = nc
        self.es = ExitStack()
        self.ops = {e: [] for e in self.ENGS}
        self.lastw = {}
        self.readers = {}
        self.ndma = {e: 0 for e in self.ENGS}
        self.lastop = {}
        self.lastdma = {}

    def sb(self, name, shape, dt):
        return self.es.enter_context(self.nc.sbuf_tensor(name, list(shape), dt))

    def ps(self, name, shape, dt):
        return self.es.enter_context(self.nc.psum_tensor(name, list(shape), dt))

    def dram(self, name, shape, dt):
        return self.nc.dram_tensor(name, list(shape), dt, kind="Internal").ap()

    def add(self, eng, fn, reads=(), writes=(), is_dma=False):
        op = _Op(eng, fn, is_dma)
        deps = {}
        rk = [_key(k) for k in reads]
        wk = [_key(k) for k in writes]
        for k in rk:
            w = self.lastw.get(k)
            if w is not None:
                deps[id(w)] = w
        for k in wk:
            w = self.lastw.get(k)
            if w is not None:
                deps[id(w)] = w
            for r in self.readers.get(k, {}).values():
                deps[id(r)] = r
        for d in deps.values():
            if d is op:
                continue
            if (not is_dma) and (not d.is_dma) and d.eng == eng == "tensor":
                continue
            op.deps.append(d)
            d.signal = True
        if is_dma:
            n = self.ndma[eng]
            self.ndma[eng] = n + 1
            op.dslot = n % self.NDSEM
            op.dval = 16 * (n // self.NDSEM + 1)
            self.lastdma[(eng, op.dslot)] = op
        else:
            self.lastop[eng] = op
        rkey = (eng, op.dslot) if is_dma else eng
        for k in rk:
            self.readers.setdefault(k, {})[rkey] = op
        for k in wk:
            self.lastw[k] = op
            self.readers[k] = {}
        self.ops[eng].append(op)
        return op

    def fence(self):
        allops = list(self.lastop.values()) + list(self.lastdma.values())
        for e in self.ENGS:
            op = _Op(e, None, False)
            for d in allops:
                if d.eng == e and not d.is_dma:
                    continue
                op.deps.append(d)
                d.signal = True
            self.ops[e].append(op)
        self.lastw = {}
        self.readers = {}

    def dma(self, q, out, in_, reads, writes, **kw):
        return self.add(q, lambda e: e.dma_start(out=out, in_=in_, **kw), reads, writes, is_dma=True)

    def act(self, out, in_, func, reads, writes, **kw):
        return self.add("scalar", lambda e: e.activation(out=out, in_=in_, func=func, **kw), reads, writes)

    def mm(self, out, lhsT, rhs, start, stop, reads, writes):
        return self.add("tensor", lambda e: e.matmul(out, lhsT=lhsT, rhs=rhs, start=start, stop=stop),
                        reads, writes)

    def tr(self, out, in_, ident, reads, writes):
        return self.add("tensor", lambda e: e.transpose(out, in_, ident), reads, writes)

    def v(self, fn, reads, writes):
        return self.add("vector", fn, reads, writes)

    def g(self, fn, reads, writes):
        return self.add("gpsimd", fn, reads, writes)

    def finish(self):
        nc = self.nc
        es = self.es
        esem = {}
        for e in self.ENGS:
            c = 0
            for op in self.ops[e]:
                if op.is_dma:
                    continue
                if op.signal:
                    c += 1
                    op.sidx = c
            nep = c // self.EPOCH + 1
            esem[e] = [es.enter_context(nc.semaphore(f"s_{e}_{i}")) for i in range(nep)]
        dsem = {}
        for e in self.ENGS:
            if self.ndma[e]:
                dsem[e] = [es.enter_context(nc.semaphore(f"d_{e}_{i}")) for i in range(self.NDSEM)]
        for e in self.ENGS:
            for op in self.ops[e]:
                if op.is_dma:
                    op.event = (dsem[e][op.dslot], op.dval)
                elif op.signal:
                    i = op.sidx - 1
                    op.event = (esem[e][i // self.EPOCH], i % self.EPOCH + 1)

        def emit(eng, e):
            waited = {}

            def wait(ev):
                sem, val = ev
                k = sem.name
                if waited.get(k, 0) < val:
                    eng.wait_ge(sem, val)
                    waited[k] = val

            for op in self.ops[e]:
                mx = {}
                for d in op.deps:
                    sem, val = d.event
                    k = sem.name
                    if k not in mx or mx[k][1] < val:
                        mx[k] = (sem, val)
                for ev in mx.values():
                    wait(ev)
                if op.fn is None:
                    continue
                if op.is_dma:
                    if op.dval > 16:
                        wait((dsem[e][op.dslot], op.dval - 16))
                    op.fn(eng).then_inc(dsem[e][op.dslot], 16)
                else:
                    ins = op.fn(eng)
                    if op.signal:
                        ins.then_inc(*((op.event[0], 1)))
            if e == "sync":
                for q in self.ENGS:
                    n = self.ndma[q]
                    for s in range(min(n, self.NDSEM)):
                        last = ((n - 1 - s) // self.NDSEM) * self.NDSEM + s
                        wait((dsem[q][s], 16 * (last // self.NDSEM + 1)))

        with nc.Block() as block:
            @block.tensor
            def _(eng):
                emit(eng, "tensor")

            @block.vector
            def _(eng):
                emit(eng, "vector")

            @block.scalar
            def _(eng):
                emit(eng, "scalar")

            @block.gpsimd
            def _(eng):
                emit(eng, "gpsimd")

            @block.sync
            def _(eng):
                emit(eng, "sync")
        es.close()


L = 4096
D = 1024
NB = 32
NG = 8
DEPTH = 4
W_AB = 2508
W_CD = 3088
NEG = -30000.0
NFR = 3072
SU = 2944
NIT = 18
TOPK = 256


def _rel_bucket_np(d):
    n = np.maximum(d, 0)
    nf = np.maximum(n, 1).astype(np.float32)
    large = 16 + (np.log(nf / np.float32(16)) / np.float32(math.log(2048 / 16)) * np.float32(16)).astype(np.int32)
    large = np.minimum(large, 31)
    return np.where(n < 16, n, large)


def _host_consts():
    c = {}
    c["ident"] = np.eye(128, dtype=np.float32)
    c["exch"] = np.eye(128, dtype=np.float32)[::-1].copy()
    mk = np.zeros((128, 4, 512), np.float32)
    p = np.arange(128)[:, None]
    u = np.arange(512)[None, :]
    for j in range(4):
        mk[:, j, :] = np.where(u - 128 * j - p >= 0, 0.0, NEG)
    c["maskneg"] = mk
    c["cmT"] = np.where(np.arange(128)[None, :] > np.arange(128)[:, None], -1e30, 0.0).astype(np.float32)
    i = np.arange(NFR)
    d = i - 511
    bk = _rel_bucket_np(d)
    oh = np.zeros((32, NFR), np.float32)
    oh[bk, i] = 1.0
    oh[:, d < 0] = 0.0
    c["oh"] = oh
    add = np.zeros((2, 8, NFR), np.float32)
    add[0, :, d < 0] = NEG
    cnt = ((d <= 128).astype(np.float32) + ((d % 4 == 0) & (d <= 512)) + ((d % 16 == 0) & (d <= 2048)))
    dil = np.where((d >= 0) & (cnt > 0), np.log(np.maximum(cnt, 1.0)), NEG).astype(np.float32)
    add[1, :, :] = dil[None, :]
    c["addrow"] = add
    c["ck"] = np.broadcast_to((0.5 ** (np.arange(NIT) + 1)).astype(np.float32)[None, :], (128, NIT)).copy()
    return c


class _Rot:
    def __init__(self, tiles):
        self.t = tiles
        self.i = 0

    def next(self):
        t = self.t[self.i % len(self.t)]
        self.i += 1
        return t


class _Stop(Exception):
    pass


def build_nc(nlayers=DEPTH, dbg=False, stop=None, only=None):
    def chk(name):
        if stop == name:
            raise _Stop()

    nc = bass.Bass("TRN2", target_bir_lowering=False)
    P = Prog(nc)
    I = {}

    def inp(name, shape):
        I[name] = nc.dram_tensor(name, list(shape), F32, kind="ExternalInput").ap()

    WL = max(1, nlayers) if dbg else 4
    for name, shape in [
        ("x", (L, D)), ("c", (8, 128)), ("w_ada", (WL, D, 6 * D)), ("b_ada", (4, 6 * D)), ("g_mix", (4, D)),
        ("g_ffn", (4, D)), ("g_final", (1, D)), ("rel_bias", (32, 8)), ("ab_w_in", (2, D, W_AB)),
        ("ab_conv_w", (2, 4, 512)), ("ab_conv_b", (2, 512)), ("ab_gate_b", (2, 8)), ("ab_hnorm_g", (2, 512)),
        ("ab_w_uk", (2, 128, 512)), ("ab_w_uv", (2, 128, 512)), ("ab_w_out", (2, D, D)),
        ("cd_w_in", (2, D, W_CD)), ("cd_w_alpha", (2, 16, 256)), ("cd_b_alpha", (2, 256)),
        ("cd_hnorm_g", (2, 512)), ("cd_w_out", (2, D, D)), ("moe_w_coarse", (4, D, 4)), ("moe_b_coarse", (4, 4)),
        ("moe_w_fine", (4, 4, D, 4)), ("moe_b_fine", (4, 16)), ("moe_w_gate", (WL, 16, D, 512)),
        ("moe_w_up", (WL, 16, D, 512)), ("moe_w_down", (WL, 16, 512, D)),
        ("ident", (128, 128)), ("exch", (128, 128)), ("maskneg", (128, 4, 512)), ("cmT", (128, 128)),
        ("oh", (32, NFR)), ("addrow", (2, 8, NFR)), ("ck", (128, NIT)),
    ]:
        inp(name, shape)
    out = nc.dram_tensor("out", [L, D], F32, kind="ExternalOutput").ap()
    okind = "ExternalOutput" if dbg else "Internal"
    xs = nc.dram_tensor("xs", [L, D], F32, kind=okind).ap()
    Yd = nc.dram_tensor("Yd", [8, 128, L], BF16, kind=okind).ap()
    modd = nc.dram_tensor("modd", [4, 6 * D], F32, kind=okind).ap()
    Frow = nc.dram_tensor("Frow", [2, 8, NFR], F32, kind="Internal").ap()
    gated = nc.dram_tensor("gated", [2, 128, L], BF16, kind="Internal").ap()
    gated = nc.dram_tensor("gated4", [4, 128, L], BF16, kind="Internal").ap()
    rowsd = nc.dram_tensor("rowsd", [2, 4, L], F32, kind="Internal").ap()
    nmtd = nc.dram_tensor("nmtd", [NB, 128, L], BF16, kind="Internal").ap()

    R = [P.sb(f"R{i}", [128, 16384], BF16) for i in range(3)]
    W = [P.sb(f"W{i}", [128, 12416], BF16) for i in range(2)]
    ident_f = P.sb("ident_f", [128, 128], F32)
    ident_b = P.sb("ident_b", [128, 128], BF16)
    exch_b = P.sb("exch_b", [128, 128], BF16)
    ones_b = P.sb("ones_b", [128, 128], BF16)
    ones_f = P.sb("ones_f", [128, 128], F32)
    maskneg = P.sb("maskneg_t", [128, 4, 512], BF16)
    mask01 = maskneg
    cmT = P.sb("cmT_t", [128, 128], F32)
    eps_t = P.sb("eps_t", [128, 1], F32)
    ckt = P.sb("ckt", [128, NIT], F32)
    gsB = P.sb("gsB", [128, D], F32)
    shB = P.sb("shB", [128, D], F32)
    gtB = P.sb("gtB", [128, D], F32)
    XB = _Rot([P.sb(f"xb{i}", [128, D], F32) for i in range(1)])
    HM = _Rot([P.sb(f"hm{i}", [128, D], F32) for i in range(1)])
    HT = _Rot([P.sb(f"hT{i}", [128, 8, 512], BF16) for i in range(2)])
    sm = P.sb("sm", [128, 64], F32)
    PT = _Rot([P.sb(f"pt{i}", [128, 512], BF16) for i in range(3)])
    ET = _Rot([P.sb(f"et{i}", [128, 512], F32) for i in range(2)])
    junk = ET.t[0][:, :].bitcast(BF16)
    FT = _Rot([P.sb(f"ft{i}", [128, 512], F32) for i in range(4)])
    ST = _Rot([P.sb(f"st{i}", [128, 512], BF16) for i in range(2)])
    pT = P.ps("pT", [128, 1024], F32)
    PG = _Rot([P.ps(f"pG{i}", [128, 512], F32) for i in range(2)])
    PA = [P.ps(f"pA{i}", [128, 512], F32) for i in range(4)]

    def bc(row_ap, n=D):
        return row_ap.to_broadcast([128, n])

    P.dma("sync", ident_f[:], I["ident"], ["c_ident"], [ident_f])
    P.v(lambda e: e.tensor_copy(out=ident_b[:], in_=ident_f[:]), [ident_f], [ident_b])
    P.dma("gpsimd", exch_b[:], I["exch"], ["c_exch"], [exch_b])
    P.v(lambda e: e.memset(ones_b[:], 1.0), [], [ones_b])
    P.v(lambda e: e.memset(ones_f[:], 1.0), [], [ones_f])
    P.v(lambda e: e.memset(eps_t[:], 1e-6), [], [eps_t])
    P.dma("sync", cmT[:], I["cmT"], ["c_cm"], [cmT])
    P.dma("sync", ckt[:], I["ck"], ["c_ck"], [ckt])

    relt = P.sb("relt", [32, 8], F32)
    P.dma("sync", relt[:], I["rel_bias"], ["c_rel"], [relt])
    oht = R[0][0:32, 0:2 * NFR].bitcast(F32)
    P.dma("sync", oht, I["oh"], ["c_oh"], ["oht"])
    for ty in range(2):
        addt = R[1][0:8, 0:2 * NFR].bitcast(F32)
        P.dma("sync", addt, I["addrow"][ty], ["c_add"], ["addt"])
        for j in range(NFR // 512):
            pg = PG.next()
            P.mm(pg[0:8, :], relt[:, :], oht[:, j * 512:(j + 1) * 512], True, True, [relt, "oht"], [pg])
            P.v(lambda e, pg=pg, j=j, addt=addt: e.tensor_tensor(out=addt[:, j * 512:(j + 1) * 512], in0=pg[0:8, :],
                                                                  in1=addt[:, j * 512:(j + 1) * 512], op=ALU.add),
                [pg, "addt"], ["addt"])
        P.dma("sync", Frow[ty], addt, ["addt"], [("Frow", ty)])
    P.fence()

    c8 = P.sb("c8", [8, 128], F32)
    cs = P.sb("cs", [128, 8], F32)
    P.dma("sync", c8[:], I["c"], ["c_c"], [c8])
    pg = PG.next()
    P.tr(pg[:, 0:8], c8[:, :], ident_f[0:8, 0:8], [c8, ident_f], [pg])
    P.act(cs[:], pg[:, 0:8], AF.Silu, [pg], [cs])
    R2f = R[2][:, :].bitcast(F32)
    brow = _Rot([R2f[0:1, i * 512:(i + 1) * 512] for i in range(2)])
    mrow = _Rot([R2f[0:1, (2 + i) * 512:(3 + i) * 512] for i in range(2)])
    wi_ = 0
    for l in range(nlayers):
        for j in range(12):
            wt = W[wi_ % 2]
            wi_ += 1
            wv = wt[:, 0:8192].bitcast(F32).rearrange("p (k n) -> p k n", k=8)
            P.dma("sync", wv, I["w_ada"][l][:, j * 512:(j + 1) * 512].rearrange("(k p) n -> p k n", p=128),
                  ["w_ada"], [wt])
            br = brow.next()
            brk = f"brow{j % 2}"
            mrk = f"mrow{j % 2}"
            P.dma("sync", br, I["b_ada"][l:l + 1, j * 512:(j + 1) * 512], ["b_ada"], [brk])
            pg = PG.next()
            for k in range(8):
                P.mm(pg[0:1, :], cs[:, k:k + 1], wv[:, k, :], k == 0, k == 7, [cs, wt], [pg])
            mr = mrow.next()
            P.v(lambda e, mr=mr, pg=pg, br=br: e.tensor_tensor(out=mr, in0=pg[0:1, :], in1=br, op=ALU.add),
                [pg, brk], [mrk])
            P.dma("sync", modd[l:l + 1, j * 512:(j + 1) * 512], mr, [mrk], [("modd", l)])
    P.fence()

    def modrow(l, j):
        return modd[l:l + 1, j * D:(j + 1) * D]

    def prep_norm(l, ffn):
        o = 3 if ffn else 0
        grow = (I["g_ffn"] if ffn else I["g_mix"])[l:l + 1, :]
        gB = HM.next()
        P.dma("sync", gB[:], bc(grow), ["g"], [gB])
        P.dma("sync", gsB[:], bc(modrow(l, o + 1)), [("modd", l)], [gsB])
        P.dma("sync", shB[:], bc(modrow(l, o + 0)), [("modd", l)], [shB])
        P.dma("sync", gtB[:], bc(modrow(l, o + 2)), [("modd", l)], [gtB])
        P.v(lambda e: e.scalar_tensor_tensor(out=gsB[:], in0=gsB[:], scalar=1.0, in1=gB[:], op0=ALU.add, op1=ALU.mult),
            [gsB, gB], [gsB])

    def xsrc(l):
        return I["x"] if l == 0 else xs

    def norm_block(xsrc_ap, blk, ht, b, htf=None):
        xb = XB.next()
        P.dma("sync", xb[:], xsrc_ap[blk * 128:(blk + 1) * 128, :], [("xs", blk)], [xb])
        ss = sm[:, 0:1]
        P.act(junk, xb[:], AF.Square, [xb], [ET.t[0], "ss"], accum_out=ss)
        P.v(lambda e: e.tensor_scalar(out=sm[:, 1:2], in0=ss, scalar1=1.0 / D, scalar2=1e-6, op0=ALU.mult, op1=ALU.add),
            ["ss"], ["rs"])
        P.act(sm[:, 1:2], sm[:, 1:2], AF.Sqrt, ["rs"], ["rs"])
        P.v(lambda e: e.reciprocal(out=sm[:, 1:2], in_=sm[:, 1:2]), ["rs"], ["rs"])
        hm = HM.next()
        P.v(lambda e, hm=hm, xb=xb: e.scalar_tensor_tensor(out=hm[:], in0=xb[:], scalar=sm[:, 1:2], in1=gsB[:],
                                                           op0=ALU.mult, op1=ALU.mult), [xb, "rs", gsB], [hm])
        P.g(lambda e, hm=hm: e.tensor_tensor(out=hm[:], in0=hm[:], in1=shB[:], op=ALU.add), [hm, shB], [hm])
        for k in range(8):
            P.tr(pT[:, k * 128:(k + 1) * 128], hm[:, k * 128:(k + 1) * 128], ident_f[:], [hm, ident_f], [pT])
        pv = pT[:, :].rearrange("p (k t) -> p k t", k=8)
        P.act(ht[:, :, b * 128:(b + 1) * 128], pv, AF.Copy, [pT], [ht])
        if htf is not None:
            P.act(htf[:, :, b * 128:(b + 1) * 128], pv, AF.Copy, [pT], ["htf"])

    def wload(wt, ncols, src2d):
        wv = wt[:, 0:8 * ncols].rearrange("p (k n) -> p k n", k=8)
        for k in range(8):
            P.dma("gpsimd", wv[:, k, :], src2d[k * 128:(k + 1) * 128, :], ["wsrc"], [wt])
        return wv

    def gemm_fm(pg, wv, c0, m, ht, reads, n=512, t0=0):
        for k in range(8):
            P.mm(pg[0:m, 0:n], wv[:, k, c0:c0 + m], ht[:, k, t0:t0 + n], k == 0, k == 7, reads, [pg])

    def gemm_tm(pg, ht, b, wv, c0, n, reads):
        for k in range(8):
            P.mm(pg[:, 0:n], ht[:, k, b * 128:(b + 1) * 128], wv[:, k, c0:c0 + n], k == 0, k == 7, reads, [pg])

    def head_norm_store(l, h, g, raw, hg_col, gate_chunk, ychunk):
        sq = FT.next()
        P.act(sq[:], raw[:], AF.Square, [raw], [sq])
        pg = PG.next()
        P.mm(pg[:, :], ones_f[:, :], sq[:], True, True, [ones_f, sq], [pg])
        rsd = FT.next()
        P.act(rsd[:], pg[:, :], AF.Sqrt, [pg, eps_t], [rsd], scale=1.0 / 128, bias=eps_t[:, 0:1])
        P.v(lambda e: e.reciprocal(out=rsd[:], in_=rsd[:]), [rsd], [rsd])
        P.v(lambda e: e.scalar_tensor_tensor(out=raw[:], in0=raw[:], scalar=hg_col, in1=rsd[:], op0=ALU.mult,
                                             op1=ALU.mult), [raw, rsd, "hgt"], [raw])
        gt_ = PT.next()
        P.dma("sync", gt_[:], gated[gate_chunk][:, g * 512:(g + 1) * 512], [("gated", gate_chunk)], [gt_])
        st = ST.next()
        P.v(lambda e: e.tensor_tensor(out=st[:], in0=raw[:], in1=gt_[:], op=ALU.mult), [raw, gt_], [st])
        P.dma("sync", Yd[ychunk][:, g * 512:(g + 1) * 512], st[:], [st], [("Yd", ychunk)])

    def load_strip(ty, h, stf, stb):
        src = bass.AP(Frow.tensor, (ty * 8 + h) * NFR, [[1, 128], [1, SU]])
        P.dma("sync", stf, src, [("Frow", ty)], ["stf"])
        P.act(stb, stf, AF.Copy, ["stf"], ["stb"])

    def softmax_attn(ty, QTv, KTv, VTv, use_mask, ybase):
        stf = W[0][:, 0:2 * SU].bitcast(F32)
        stb = W[1][:, 0:SU]
        NMT = _Rot([W[1][:, 3072 + i * 512:3072 + (i + 1) * 512] for i in range(4)])
        nmi = 0
        for h in range(8):
            load_strip(ty, h, stf, stb)
            c, po = h // 2, (h % 2) * 64
            for g in range(NG):
                acc_o = PA[(h * NG + g) % 2 * 2]
                acc_d = PA[(h * NG + g) % 2 * 2 + 1]
                kb_lo = 0 if ty == 0 else max(0, 4 * g - 16)
                kbs = list(range(kb_lo, 4 * g + 4))
                def qk(kb):
                    nonlocal nmi
                    delta = 512 * g - 128 * kb
                    col = min(delta, 1664 if ty == 0 else 2048) + 384
                    pl = PG.next()
                    P.mm(pl[:, :], KTv[po:po + 64, c, kb * 128:(kb + 1) * 128], QTv[po:po + 64, c, g * 512:(g + 1) * 512],
                         True, False, ["KT", "QT"], [pl])
                    P.mm(pl[:, :], exch_b[:, :], stb[:, col:col + 512], False, not use_mask, [exch_b, "stb"], [pl])
                    if use_mask:
                        nm = NMT.next()
                        nk = f"nmt{nmi % 4}"
                        nmi += 1
                        P.dma("sync", nm, nmtd[kb][:, g * 512:(g + 1) * 512], ["nmtd"], [nk])
                        P.mm(pl[:, :], ident_b[:, :], nm, False, True, [ident_b, nk], [pl])
                    return pl

                def pv(i, kb, pl):
                    pt = PT.next()
                    P.act(pt[:], pl[:, :], AF.Exp, [pl], [pt])
                    P.mm(acc_o[0:64, :], VTv[:, kb, h * 64:(h + 1) * 64], pt[:], i == 0, i == len(kbs) - 1, ["VT", pt], [acc_o])
                    P.mm(acc_d[0:64, :], ones_b[:, 0:64], pt[:], i == 0, i == len(kbs) - 1, [ones_b, pt], [acc_d])

                prev = None
                for i, kb in enumerate(kbs):
                    pl = qk(kb)
                    if prev is not None:
                        pv(*prev)
                    prev = (i, kb, pl)
                pv(*prev)
                rec = FT.next()
                P.v(lambda e, rec=rec, acc_d=acc_d: e.reciprocal(out=rec[0:64, :], in_=acc_d[0:64, :]), [acc_d], [rec])
                st = ST.next()
                P.v(lambda e, rec=rec, acc_o=acc_o, st=st: e.tensor_tensor(out=st[0:64, :], in0=acc_o[0:64, :],
                                                                          in1=rec[0:64, :], op=ALU.mult),
                    [acc_o, rec], [st])
                P.dma("sync", Yd[ybase + c][po:po + 64, g * 512:(g + 1) * 512], st[0:64, :], [st], [("Yd", ybase + c)])

    def out_proj_residual(l, wsrc):
        wv = wload(W[0], 1024, wsrc)
        for g in range(NG):
            yg = HT.next()
            for k in range(8):
                P.dma("sync", yg[:, k, :], Yd[k][:, g * 512:(g + 1) * 512], [("Yd", k)], [yg])
            for b in range(4):
                blk = 4 * g + b
                xb = XB.next()
                P.dma("sync", xb[:], xsrc(l)[blk * 128:(blk + 1) * 128, :], [("xs", blk)], [xb])
                hm = HM.next()
                for half in range(2):
                    pg = PG.next()
                    gemm_tm(pg, yg, b, wv, half * 512, 512, [yg, W[0]])
                    P.v(lambda e, hm=hm, pg=pg, half=half: e.tensor_tensor(out=hm[:, half * 512:(half + 1) * 512], in0=pg[:, :],
                                                                            in1=gtB[:, half * 512:(half + 1) * 512], op=ALU.mult),
                        [pg, gtB], [hm])
                P.g(lambda e, hm=hm, xb=xb: e.tensor_tensor(out=xb[:], in0=xb[:], in1=hm[:], op=ALU.add), [xb, hm], [xb])
                P.dma("sync", xs[blk * 128:(blk + 1) * 128, :], xb[:], [xb], [("xs", blk)])

    aT_t = P.sb("aT_t", [128, 128], F32)
    hgt = P.sb("hgt", [128, 4], F32)
    cw = P.sb("cw", [128, 4, 4], F32)
    cb = P.sb("cb", [128, 4], F32)
    gb4 = P.sb("gb4", [4, 4], F32)
    one_c = P.sb("one_c", [128, 1], F32)
    wit = P.sb("wit", [128, NB, 4], F32)
    thr_c = P.sb("thr_c", [128, 1], F32)
    steps = P.sb("steps", [128, NIT], F32)
    P.v(lambda e: e.memset(one_c[:], 1.0), [], [one_c])
    P.v(lambda e: e.memset(thr_c[:], -1e29), [], [thr_c])
    NC = dict(allow_slow_non_contiguous=True)

    def store_gated(pg, func, chunk, g):
        st = ST.next()
        P.act(st[:], pg[:, :], func, [pg], [st])
        P.dma("sync", gated[chunk][:, g * 512:(g + 1) * 512], st[:], [st], [("gated", chunk)])

    def lin_attn(l, kind, QKv, qc0, kc0, VTv, hn_src, gla=None):
        for hh in range(4):
            P.dma("sync", hgt[:, hh:hh + 1], hn_src[:, hh * 128:(hh + 1) * 128].rearrange("o p -> p o"), ["hn"], [hgt], **NC)
        for g in range(int(os.environ.get("DBG_G", NG))):
            if kind == 1:
                QTg, KTg = gla(g)
            for h in range(int(os.environ.get("DBG_H", 4))):
                c, po = h // 2, (h % 2) * 64
                acc_n = PA[(h * NG + g) % 2 * 2]
                acc_d = PA[(h * NG + g) % 2 * 2 + 1]
                if kind == 0:
                    negMB = FT.next()
                    P.dma("sync", negMB[:], bc(rowsd[0][h:h + 1, g * 512:(g + 1) * 512], 512), ["rowsd"], [negMB])
                    emB = FT.next()
                    P.dma("sync", emB[:], bc(rowsd[1][h:h + 1, g * 512:(g + 1) * 512], 512), ["rowsd"], [emB])
                nkb = 4 * g + 4

                def qk(kb):
                    pl = PG.next()
                    if kind == 0:
                        P.mm(pl[:, :], QKv[po:po + 64, kc0 + c, kb * 128:(kb + 1) * 128],
                             QKv[po:po + 64, qc0 + c, g * 512:(g + 1) * 512], True, True, ["QK"], [pl])
                    else:
                        P.mm(pl[:, :], KTg[po:po + 64, c, kb * 128:(kb + 1) * 128], QTg[po:po + 64, c, :], True, True,
                             ["KTg", "QTg"], [pl])
                    return pl

                def pv(kb, pl):
                    pt = PT.next()
                    j = kb - 4 * g
                    if kind == 0:
                        src_ = negMB
                        if j >= 0:
                            tmp = ET.next()
                            P.g(lambda e, tmp=tmp, negMB=negMB, j=j: e.tensor_tensor(out=tmp[:], in0=negMB[:], in1=maskneg[:, j, :],
                                                                                      op=ALU.add), [negMB, maskneg], [tmp])
                            src_ = tmp
                        E = ET.next()
                        P.act(E[:], src_[:], AF.Exp, [src_, aT_t], [E], bias=aT_t[:, kb * 4 + h:kb * 4 + h + 1])
                        P.v(lambda e, pt=pt, pl=pl, E=E: e.scalar_tensor_tensor(out=pt[:], in0=pl[:, :], scalar=0.125, in1=E[:],
                                                                                op0=ALU.mult, op1=ALU.mult), [pl, E], [pt])
                    else:
                        if j >= 0:
                            P.v(lambda e, pt=pt, pl=pl, j=j: e.tensor_tensor(out=pt[:], in0=pl[:, :], in1=mask01[:, j, :], op=ALU.mult),
                                [pl, mask01], [pt])
                        else:
                            P.act(pt[:], pl[:, :], AF.Copy, [pl], [pt])
                    P.mm(acc_n[:, :], VTv[:, kb, h * 128:(h + 1) * 128], pt[:], kb == 0, kb == nkb - 1, ["VT", pt], [acc_n])
                    if kind == 0:
                        P.mm(acc_d[:, :], ones_b[:, :], pt[:], kb == 0, kb == nkb - 1, [ones_b, pt], [acc_d])

                prev = None
                for kb in range(nkb):
                    pl = qk(kb)
                    if prev is not None:
                        pv(*prev)
                    prev = (kb, pl)
                pv(*prev)
                raw = FT.next()
                if kind == 0:
                    dn = FT.next()
                    P.act(dn[:], acc_d[:, :], AF.Abs, [acc_d], [dn])
                    P.v(lambda e, dn=dn, emB=emB: e.tensor_tensor(out=dn[:], in0=dn[:], in1=emB[:], op=ALU.max), [dn, emB], [dn])
                    P.v(lambda e, dn=dn: e.reciprocal(out=dn[:], in_=dn[:]), [dn], [dn])
                    P.v(lambda e, raw=raw, acc_n=acc_n, dn=dn: e.tensor_tensor(out=raw[:], in0=acc_n[:, :], in1=dn[:], op=ALU.mult),
                        [acc_n, dn], [raw])
                else:
                    P.act(raw[:], acc_n[:, :], AF.Copy, [acc_n], [raw])
                head_norm_store(l, h, g, raw, hgt[:, h:h + 1], h, h)

    def layer_ab(l):
        j = l // 2
        win = I["ab_w_in"][j]
        P.dma("gpsimd", maskneg[:], I["maskneg"], ["c_mk"], [maskneg])
        prep_norm(l, False)
        wv = wload(W[0], 1544, win[:, 0:1544])
        QKraw = R[0][:, :].rearrange("p (c t) -> p c t", c=4)
        R1f = R[1][:, :].bitcast(F32)
        IA = R1f[0:4, 0:L]
        FA = R1f[0:4, L:2 * L]
        VA = R[2][:, :].rearrange("p (b n) -> p b n", b=NB)
        for g in range(NG):
            ht = HT.next()
            for b in range(4):
                norm_block(xsrc(l), 4 * g + b, ht, b)
            for cch in range(4):
                pg = PG.next()
                gemm_fm(pg, wv, cch * 128, 128, ht, [W[0], ht])
                P.act(QKraw[:, cch, g * 512:(g + 1) * 512], pg[:, :], AF.Copy, [pg], ["QKraw"])
            for cch in range(4):
                pg = PG.next()
                gemm_fm(pg, wv, 1024 + cch * 128, 128, ht, [W[0], ht])
                store_gated(pg, AF.Sigmoid, cch, g)
            pg = PG.next()
            gemm_fm(pg, wv, 1536, 4, ht, [W[0], ht])
            P.v(lambda e, pg=pg, g=g: e.tensor_copy(out=IA[:, g * 512:(g + 1) * 512], in_=pg[0:4, :]), [pg], ["IA"])
            pg = PG.next()
            gemm_fm(pg, wv, 1540, 4, ht, [W[0], ht])
            P.v(lambda e, pg=pg, g=g: e.tensor_copy(out=FA[:, g * 512:(g + 1) * 512], in_=pg[0:4, :]), [pg], ["FA"])
            for b in range(4):
                pg = PG.next()
                gemm_tm(pg, ht, b, wv, 512, 512, [W[0], ht])
                P.v(lambda e, pg=pg, b=b, g=g: e.tensor_copy(out=VA[:, 4 * g + b, :], in_=pg[:, :]), [pg], ["VT"])
        P.fence()
        chk('A')
        W0f = W[0][:, :].bitcast(F32)
        W1f = W[1][:, :].bitcast(F32)
        NBr = W0f[0:4, 0:L]
        Mr = W1f[0:4, 0:L]
        for tt in range(2):
            P.dma("sync", gb4[:, tt:tt + 1], I["ab_gate_b"][j:j + 1, tt * 4:(tt + 1) * 4].rearrange("o p -> p o"), ["gb"], [gb4], **NC)
        P.v(lambda e: e.tensor_scalar(out=gb4[:, 2:3], in0=gb4[:, 1:2], scalar1=-1.0, scalar2=None, op0=ALU.mult), [gb4], [gb4])
        P.act(FA, FA, AF.Exp, ["FA", gb4], ["FA"], scale=-1.0, bias=gb4[:, 2:3])
        P.act(FA, FA, AF.Ln, ["FA", one_c], ["FA"], bias=one_c[0:4, 0:1])
        P.v(lambda e: e.tensor_tensor_scan(out=NBr, data0=one_c[0:4, 0:1].to_broadcast([4, L]), data1=FA, initial=0.0,
                                           op0=ALU.mult, op1=ALU.add), ["FA", one_c], ["NBr"])
        P.v(lambda e: e.scalar_tensor_tensor(out=IA, in0=IA, scalar=gb4[:, 0:1], in1=NBr, op0=ALU.add, op1=ALU.add),
            ["IA", gb4, "NBr"], ["IA"])
        P.v(lambda e: e.tensor_tensor_scan(out=Mr, data0=IA, data1=IA, initial=-1e30, op0=ALU.max, op1=ALU.max), ["IA"], ["Mr"])
        P.v(lambda e: e.tensor_scalar(out=FA, in0=Mr, scalar1=-1.0, scalar2=None, op0=ALU.mult), ["Mr", "FA"], ["FA"])
        P.dma("sync", rowsd[0], FA, ["FA"], ["rowsd"])
        P.v(lambda e: e.tensor_tensor(out=Mr, in0=NBr, in1=Mr, op=ALU.subtract), ["NBr", "Mr"], ["Mr"])
        P.act(Mr, Mr, AF.Exp, ["Mr"], ["Mr"])
        P.dma("sync", rowsd[1], Mr, ["Mr"], ["rowsd"])
        for b in range(NB):
            P.tr(pT[:, b * 4:(b + 1) * 4], IA[:, b * 128:(b + 1) * 128], ident_f[0:4, 0:4], ["IA", ident_f], [pT])
        P.v(lambda e: e.tensor_copy(out=aT_t[:], in_=pT[:, 0:128]), [pT], [aT_t])
        P.fence()
        chk('G')
        for cc in range(4):
            for tj in range(4):
                P.dma("sync", cw[:, cc, tj:tj + 1], I["ab_conv_w"][j][tj:tj + 1, cc * 128:(cc + 1) * 128].rearrange("o p -> p o"), ["cw"], [cw], **NC)
            P.dma("sync", cb[:, cc:cc + 1], I["ab_conv_b"][j:j + 1, cc * 128:(cc + 1) * 128].rearrange("o p -> p o"), ["cb"], [cb], **NC)
        QKc = R[1][:, :].rearrange("p (c t) -> p c t", c=4)
        for cch in range(4):
            for sg in range(4):
                a0 = sg * 1024
                acc = HM.next()
                P.v(lambda e, acc=acc, cch=cch, a0=a0: e.tensor_scalar(out=acc[:], in0=QKraw[:, cch, a0:a0 + 1024],
                                                                       scalar1=cw[:, cch, 3:4], scalar2=None, op0=ALU.mult),
                    ["QKraw", cw], [acc])
                for tj in (2, 1, 0):
                    s_ = 3 - tj
                    lo = max(a0, s_)
                    P.v(lambda e, acc=acc, cch=cch, a0=a0, lo=lo, s_=s_, tj=tj: e.scalar_tensor_tensor(
                        out=acc[:, lo - a0:1024], in0=QKraw[:, cch, lo - s_:a0 + 1024 - s_], scalar=cw[:, cch, tj:tj + 1],
                        in1=acc[:, lo - a0:1024], op0=ALU.mult, op1=ALU.add), ["QKraw", cw, acc], [acc])
                P.act(QKc[:, cch, a0:a0 + 1024], acc[:], AF.Silu, [acc, cb], ["QK"], bias=cb[:, cch:cch + 1])
        P.fence()
        chk('C')
        lin_attn(l, 0, QKc, 0, 2, VA, I["ab_hnorm_g"][j:j + 1, :])
        P.fence()
        chk('M')
        wvb = W[0][:, 0:8 * 388].rearrange("p (k n) -> p k n", k=8)
        for k in range(8):
            rows = slice(k * 128, (k + 1) * 128)
            P.dma("gpsimd", wvb[:, k, 0:256], win[rows, 2184:2440], ["wsrc"], [W[0]])
            P.dma("gpsimd", wvb[:, k, 256:320], win[rows, 2440:2504], ["wsrc"], [W[0]])
            P.dma("gpsimd", wvb[:, k, 320:384], win[rows, 2440:2504], ["wsrc"], [W[0]])
            P.dma("gpsimd", wvb[:, k, 384:388], win[rows, 2504:2508], ["wsrc"], [W[0]])
        QI = R[0][:, 0:8192].rearrange("p (c t) -> p c t", c=2)
        KI2 = R[0][:, 8192:12288]
        for g in range(NG):
            ht = HT.next()
            for b in range(4):
                norm_block(xsrc(l), 4 * g + b, ht, b)
            for cch in range(2):
                pg = PG.next()
                gemm_fm(pg, wvb, cch * 128, 128, ht, [W[0], ht])
                P.act(QI[:, cch, g * 512:(g + 1) * 512], pg[:, :], AF.Copy, [pg], ["QI"])
            pg = PG.next()
            gemm_fm(pg, wvb, 256, 128, ht, [W[0], ht])
            P.act(KI2[:, g * 512:(g + 1) * 512], pg[:, :], AF.Copy, [pg], ["KI2"])
            for b in range(4):
                pg = PG.next()
                gemm_tm(pg, ht, b, wvb, 384, 4, [W[0], ht])
                P.v(lambda e, pg=pg, b=b, g=g: e.tensor_scalar(out=wit[:, 4 * g + b, :], in0=pg[:, 0:4], scalar1=0.5, scalar2=None,
                                                               op0=ALU.mult), [pg], [wit])
        P.fence()
        chk('B1')
        S = W[0][:, 0:8192].bitcast(F32)
        nmb = W[1][:, 0:L]
        NMs = W[1][:, L:2 * L].rearrange("p (k t) -> p k t", k=NB)
        jkb = R[1][:, 0:L]
        pTb = pT[:, :].bitcast(BF16)
        for qb in range(NB):
            nk = (qb + 1) * 128
            for kc in range((nk + 511) // 512):
                w_ = min(512, nk - kc * 512)
                for h4 in range(4):
                    c, po = h4 // 2, (h4 % 2) * 64
                    pl = PG.next()
                    P.mm(pl[:, 0:w_], QI[po:po + 64, c, qb * 128:(qb + 1) * 128], KI2[po:po + 64, kc * 512:kc * 512 + w_], True, True,
                         ["QI", "KI2"], [pl])
                    sl = S[:, kc * 512:kc * 512 + w_]
                    if h4 == 0:
                        P.v(lambda e, sl=sl, pl=pl, w_=w_, qb=qb: e.tensor_scalar(out=sl, in0=pl[:, 0:w_], scalar1=0.0,
                                                                                 scalar2=wit[:, qb, 0:1], op0=ALU.max, op1=ALU.mult),
                            [pl, wit], ["S"])
                    else:
                        ft = FT.next()
                        P.act(ft[:, 0:w_], pl[:, 0:w_], AF.Relu, [pl], [ft])
                        P.v(lambda e, sl=sl, ft=ft, w_=w_, qb=qb, h4=h4: e.scalar_tensor_tensor(
                            out=sl, in0=ft[:, 0:w_], scalar=wit[:, qb, h4:h4 + 1], in1=sl, op0=ALU.mult, op1=ALU.add),
                            [ft, wit, "S"], ["S"])
            P.g(lambda e, qb=qb: e.tensor_tensor(out=S[:, qb * 128:(qb + 1) * 128], in0=S[:, qb * 128:(qb + 1) * 128], in1=cmT[:],
                                                 op=ALU.add), ["S", cmT], ["S"])
            if qb >= 2:
                lo = sm[:, 8:9]
                P.v(lambda e, nk=nk: e.tensor_reduce(out=sm[:, 9:10], in_=S[:, 0:nk], axis=AX.X, op=ALU.max), ["S"], ["bis"])
                P.v(lambda e: e.tensor_reduce(out=sm[:, 8:9], in_=S[:, 0:256], axis=AX.X, op=ALU.min), ["S", "bis"], ["bis"])
                P.v(lambda e: e.scalar_tensor_tensor(out=sm[:, 10:11], in0=sm[:, 9:10], scalar=1.0, in1=sm[:, 8:9], op0=ALU.add,
                                                     op1=ALU.subtract), ["bis"], ["bis"])
                P.v(lambda e: e.tensor_scalar(out=steps[:], in0=ckt[:], scalar1=sm[:, 10:11], scalar2=None, op0=ALU.mult),
                    ["bis", ckt], [steps])
                for it in range(NIT):
                    P.v(lambda e, it=it: e.tensor_tensor(out=sm[:, 11:12], in0=sm[:, 8:9], in1=steps[:, it:it + 1], op=ALU.add),
                        ["bis", steps], ["bis"])
                    P.v(lambda e, nk=nk: e.tensor_scalar(out=jkb[:, 0:nk], in0=S[:, 0:nk], scalar1=sm[:, 11:12], scalar2=0.0,
                                                         op0=ALU.is_ge, op1=ALU.add, accum_out=sm[:, 12:13]), ["S", "bis"], ["jkb", "bis"])
                    P.v(lambda e, it=it: e.tensor_scalar(out=sm[:, 13:14], in0=sm[:, 12:13], scalar1=TOPK - 0.5,
                                                         scalar2=steps[:, it:it + 1], op0=ALU.is_ge, op1=ALU.mult), ["bis", steps], ["bis"])
                    P.v(lambda e: e.tensor_tensor(out=sm[:, 8:9], in0=sm[:, 8:9], in1=sm[:, 13:14], op=ALU.add), ["bis"], ["bis"])
                thr = sm[:, 8:9]
            else:
                thr = thr_c[:, 0:1]
            P.v(lambda e, nk=nk, thr=thr: e.tensor_scalar(out=nmb[:, 0:nk], in0=S[:, 0:nk], scalar1=thr, scalar2=NEG, op0=ALU.is_lt,
                                                          op1=ALU.mult), ["S", "bis", thr_c], ["nmb"])
            for kb in range(qb + 1):
                P.tr(pTb[:, (kb % 4) * 128:(kb % 4 + 1) * 128], nmb[:, kb * 128:(kb + 1) * 128], ident_b[:, :], ["nmb", ident_b], [pT])
                if kb % 4 == 3 or kb == qb:
                    k0 = kb - kb % 4
                    n_ = kb - k0 + 1
                    P.act(NMs[:, k0:k0 + n_, :], pTb[:, 0:n_ * 128].rearrange("p (k t) -> p k t", k=n_), AF.Copy, [pT], ["NMs"])
            P.dma("sync", nmtd[0:qb + 1, :, qb * 128:(qb + 1) * 128].rearrange("k s t -> s k t"), NMs[:, 0:qb + 1, :], ["NMs"], ["nmtd"])
        P.fence()
        chk('IDX')
        wvc = W[0][:, 0:8 * 640].rearrange("p (k n) -> p k n", k=8)
        for k in range(8):
            P.dma("gpsimd", wvc[:, k, :], win[k * 128:(k + 1) * 128, 1544:2184], ["wsrc"], [W[0]])
        wuk = W[1][:, 0:512]
        wuv = W[1][:, 512:1024]
        P.dma("gpsimd", wuk, I["ab_w_uk"][j], ["wsrc"], [W[1]])
        P.dma("gpsimd", wuv, I["ab_w_uv"][j], ["wsrc"], [W[1]])
        ckvT = W[1][:, 1024:1024 + L]
        QB = R[0][:, :].rearrange("p (c t) -> p c t", c=4)
        KH = R[1][:, :].rearrange("p (c t) -> p c t", c=4)
        VH = R[2][:, :].rearrange("p (b n) -> p b n", b=NB)
        for g in range(NG):
            ht = HT.next()
            for b in range(4):
                norm_block(xsrc(l), 4 * g + b, ht, b)
            for cch in range(4):
                pg = PG.next()
                gemm_fm(pg, wvc, cch * 128, 128, ht, [W[0], ht])
                P.act(QB[:, cch, g * 512:(g + 1) * 512], pg[:, :], AF.Copy, [pg], ["QT"], scale=0.125)
            pg = PG.next()
            gemm_fm(pg, wvc, 512, 128, ht, [W[0], ht])
            P.act(ckvT[:, g * 512:(g + 1) * 512], pg[:, :], AF.Copy, [pg], ["ckvT"])
            for cch in range(4):
                pg = PG.next()
                P.mm(pg[:, :], wuk[:, cch * 128:(cch + 1) * 128], ckvT[:, g * 512:(g + 1) * 512], True, True, [W[1], "ckvT"], [pg])
                P.v(lambda e, pg=pg, cch=cch, g=g: e.tensor_copy(out=KH[:, cch, g * 512:(g + 1) * 512], in_=pg[:, :]), [pg], ["KT"])
            for b in range(4):
                pg = PG.next()
                P.mm(pg[:, :], ckvT[:, (4 * g + b) * 128:(4 * g + b + 1) * 128], wuv, True, True, [W[1], "ckvT"], [pg])
                P.v(lambda e, pg=pg, b=b, g=g: e.tensor_copy(out=VH[:, 4 * g + b, :], in_=pg[:, :]), [pg], ["VT"])
        P.fence()
        chk('B2')
        softmax_attn(0, QB, KH, VH, True, 4)
        P.fence()
        chk('ATT')
        prep_norm(l, False)
        out_proj_residual(l, I["ab_w_out"][j])
        P.fence()

    zt = PT.next()
    P.v(lambda e: e.memset(zt[:], 0.0), [], [zt])
    for kb in range(NB):
        r_ = kb % 4
        if r_:
            P.dma("sync", nmtd[kb][:, (kb - r_) * 128:kb * 128], zt[:, 0:r_ * 128], [zt], ["nmtd"])
    P.fence()

    def layer_cd(l):
        j = l // 2
        win = I["cd_w_in"][j]
        P.dma("gpsimd", maskneg[:], I["maskneg"], ["c_mk"], [maskneg])
        P.v(lambda e: e.tensor_scalar(out=mask01[:], in0=maskneg[:], scalar1=-1.0, scalar2=None, op0=ALU.is_ge), [maskneg], [maskneg])
        prep_norm(l, False)
        wv = wload(W[0], 1552, win[:, 0:1552])
        QKC = R[0][:, :].rearrange("p (c t) -> p c t", c=4)
        LS = R[1][:, :].bitcast(F32).rearrange("p (c t) -> p c t", c=2)
        VC = R[2][:, :].rearrange("p (b n) -> p b n", b=NB)
        wal = P_wal
        P.dma("sync", wal, I["cd_w_alpha"][j], ["wal"], ["wal"])
        for cc in range(2):
            P.dma("sync", cb[:, cc:cc + 1], I["cd_b_alpha"][j:j + 1, cc * 128:(cc + 1) * 128].rearrange("o p -> p o"), ["cb"], [cb], **NC)
        P.v(lambda e: e.tensor_scalar(out=cb[:, 2:4], in0=cb[:, 0:2], scalar1=-1.0, scalar2=None, op0=ALU.mult), [cb], [cb])
        for g in range(NG):
            ht = HT.next()
            for b in range(4):
                norm_block(xsrc(l), 4 * g + b, ht, b)
            for cch in range(4):
                pg = PG.next()
                gemm_fm(pg, wv, cch * 128, 128, ht, [W[0], ht])
                P.act(QKC[:, cch, g * 512:(g + 1) * 512], pg[:, :], AF.Copy, [pg], ["QK"])
            for cch in range(4):
                pg = PG.next()
                gemm_fm(pg, wv, 1040 + cch * 128, 128, ht, [W[0], ht])
                store_gated(pg, AF.Silu, cch, g)
            pg = PG.next()
            gemm_fm(pg, wv, 1024, 16, ht, [W[0], ht])
            gct = FT.next()
            P.v(lambda e, pg=pg, gct=gct: e.tensor_copy(out=gct[0:16, :], in_=pg[0:16, :]), [pg], [gct])
            for cch in range(2):
                pg = PG.next()
                P.mm(pg[:, :], wal[:, cch * 128:(cch + 1) * 128], gct[0:16, :], True, True, ["wal", gct], [pg])
                et = ET.next()
                P.act(et[:], pg[:, :], AF.Exp, [pg, cb], [et], scale=-1.0, bias=cb[:, 2 + cch:3 + cch])
                P.act(LS[:, cch, g * 512:(g + 1) * 512], et[:], AF.Ln, [et, one_c], ["LS"], bias=one_c[:, 0:1])
            for b in range(4):
                pg = PG.next()
                gemm_tm(pg, ht, b, wv, 512, 512, [W[0], ht])
                P.v(lambda e, pg=pg, b=b, g=g: e.tensor_copy(out=VC[:, 4 * g + b, :], in_=pg[:, :]), [pg], ["VT"])
        P.fence()
        chk('CA')
        nBs = [W[0][:, 0:8192].bitcast(F32), W[1][:, 0:8192].bitcast(F32)]
        for cch in range(2):
            P.v(lambda e, cch=cch: e.tensor_tensor_scan(out=nBs[cch], data0=one_c[:, 0:1].to_broadcast([128, L]), data1=LS[:, cch, :],
                                                        initial=0.0, op0=ALU.mult, op1=ALU.add), ["LS", one_c], [("nB", cch)])
        P.fence()
        KTg = R[1][:, 0:8192].rearrange("p (c t) -> p c t", c=2)
        Etmp = R[1][:, 8192:16384].bitcast(F32)
        QTg = W[1][:, 8192:9216].rearrange("p (c t) -> p c t", c=2)

        def gla(g):
            n = (g + 1) * 512
            for cch in range(2):
                if g == 0:
                    P.v(lambda e: e.memset(sm[:, 16:18], 0.0), [], ["bq"])
                    P.v(lambda e: e.memset(sm[:, 18:20], 0.0), ["bq"], ["bq"])
                else:
                    r_ = g * 512 - 1
                    P.v(lambda e, cch=cch, r_=r_: e.tensor_scalar(out=sm[:, 16 + cch:17 + cch], in0=nBs[cch][:, r_:r_ + 1], scalar1=1.0 / 16,
                                                                  scalar2=None, op0=ALU.mult), [("nB", cch)], ["bq"])
                    P.v(lambda e, cch=cch, r_=r_: e.tensor_scalar(out=sm[:, 18 + cch:19 + cch], in0=nBs[cch][:, r_:r_ + 1], scalar1=-1.0 / 16,
                                                                  scalar2=None, op0=ALU.mult), [("nB", cch), "bq"], ["bq"])
                et = ET.next()
                P.act(et[:], nBs[cch][:, g * 512:(g + 1) * 512], AF.Exp, [("nB", cch), "bq"], [et], scale=-1.0 / 16,
                      bias=sm[:, 16 + cch:17 + cch])
                P.v(lambda e, cch=cch, et=et, g=g: e.scalar_tensor_tensor(out=QTg[:, cch, :], in0=QKC[:, cch, g * 512:(g + 1) * 512],
                                                                          scalar=0.125, in1=et[:], op0=ALU.mult, op1=ALU.mult),
                    ["QK", et], ["QTg"])
                P.act(Etmp[:, 0:n], nBs[cch][:, 0:n], AF.Exp, [("nB", cch), "bq"], ["Etmp"], scale=1.0 / 16, bias=sm[:, 18 + cch:19 + cch])
                P.v(lambda e, cch=cch, n=n: e.tensor_tensor(out=KTg[:, cch, 0:n], in0=QKC[:, 2 + cch, 0:n], in1=Etmp[:, 0:n], op=ALU.mult),
                    ["QK", "Etmp"], ["KTg"])
            return QTg, KTg

        lin_attn(l, 1, None, 0, 0, VC, I["cd_hnorm_g"][j:j + 1, :], gla=gla)
        P.fence()
        chk('CG')
        wvd = wload(W[0], 1536, win[:, 1552:3088])
        QD = R[0][:, :].rearrange("p (c t) -> p c t", c=4)
        KD = R[1][:, :].rearrange("p (c t) -> p c t", c=4)
        VD = R[2][:, :].rearrange("p (b n) -> p b n", b=NB)
        for g in range(NG):
            ht = HT.next()
            for b in range(4):
                norm_block(xsrc(l), 4 * g + b, ht, b)
            for cch in range(4):
                pg = PG.next()
                gemm_fm(pg, wvd, cch * 128, 128, ht, [W[0], ht])
                P.act(QD[:, cch, g * 512:(g + 1) * 512], pg[:, :], AF.Copy, [pg], ["QT"], scale=0.125)
            for cch in range(4):
                pg = PG.next()
                gemm_fm(pg, wvd, 512 + cch * 128, 128, ht, [W[0], ht])
                P.v(lambda e, pg=pg, cch=cch, g=g: e.tensor_copy(out=KD[:, cch, g * 512:(g + 1) * 512], in_=pg[:, :]), [pg], ["KT"])
            for b in range(4):
                pg = PG.next()
                gemm_tm(pg, ht, b, wvd, 1024, 512, [W[0], ht])
                P.v(lambda e, pg=pg, b=b, g=g: e.tensor_copy(out=VD[:, 4 * g + b, :], in_=pg[:, :]), [pg], ["VT"])
        P.fence()
        chk('CD')
        softmax_attn(1, QD, KD, VD, False, 4)
        P.fence()
        chk('CS')
        prep_norm(l, False)
        out_proj_residual(l, I["cd_w_out"][j])
        P.fence()

    P_wal = W[1][0:16, 0:512].bitcast(F32)
    wr_t = P.sb("wr_t", [128, 8, 20], F32)

    rbias = P.sb("rbias", [128, 20], F32)
    R0f = R[0][:, :].bitcast(F32)
    R2f_ = R[2][:, :].bitcast(F32)
    GT = FT.t[0][:, 0:256].rearrange("p (b n) -> p b n", b=16)
    LG = FT.t[1][:, 0:80].rearrange("p (b n) -> p b n", b=4)
    rt = FT.t[1][:, 128:384].rearrange("p (b n) -> p b n", b=4)

    def moe(l):
        prep_norm(l, True)
        wr = wr_t
        P.dma("sync", wr[:, :, 0:4], I["moe_w_coarse"][l].rearrange("(k p) n -> p k n", p=128), ["wr"], ["wr"], **NC)
        for gi in range(4):
            P.dma("sync", wr[:, :, 4 + gi * 4:8 + gi * 4], I["moe_w_fine"][l][gi].rearrange("(k p) n -> p k n", p=128), ["wr"], ["wr"], **NC)
        P.dma("sync", rbias[:, 0:4], bc(I["moe_b_coarse"][l:l + 1, :], 4), ["rb"], [rbias])
        P.dma("sync", rbias[:, 4:20], bc(I["moe_b_fine"][l:l + 1, :], 16), ["rb"], [rbias])
        P.fence()
        chk('R0')
        HTF = R[0][:, 0:8192].bitcast(F32).rearrange("p (k t) -> p k t", k=8)
        for half in range(2):
            HH = R[1][:, :].rearrange("p (k t) -> p k t", k=8)
            YA = R[2]
            for gg in range(4):
                g = half * 4 + gg
                ht = HT.next()
                for b in range(4):
                    norm_block(xs, 4 * g + b, ht, b, htf=(None if os.environ.get('DBG_NOHTF') else HTF))
                chk('R1a')
                P.g(lambda e, ht=ht, gg=gg: e.tensor_copy(out=HH[:, :, gg * 512:(gg + 1) * 512], in_=ht[:, :, :]), [ht], ["HH"])
                chk('R1b')
                for b in range(4):
                    pg = PG.next()
                    for k in range(8):
                        P.mm(pg[:, 0:20], HTF[:, k, b * 128:(b + 1) * 128], wr[:, k, :], k == 0, k == 7, ["htf", "wr"], [pg])
                    P.v(lambda e, pg=pg, b=b: e.tensor_tensor(out=LG[:, b, :], in0=pg[:, 0:20], in1=rbias[:], op=ALU.add), [pg, rbias], ["LG"])
                chk('R1')
                lc = LG[:, :, 0:4]
                lf = LG[:, :, 4:20]
                cmax = rt[:, :, 0:1]
                ec = rt[:, :, 1:5]
                csum = rt[:, :, 5:6]
                ohg = rt[:, :, 6:10]
                msk = rt[:, :, 10:26]
                v1 = rt[:, :, 26:27]
                oh1 = rt[:, :, 27:43]
                v2 = rt[:, :, 43:44]
                p1 = rt[:, :, 44:45]
                p2 = rt[:, :, 45:46]
                oh2 = rt[:, :, 46:62]
                RT = ["rt", "LG"]
                P.v(lambda e: e.tensor_reduce(out=cmax, in_=lc, axis=AX.X, op=ALU.max), RT, ["rt"])
                P.v(lambda e: e.tensor_tensor(out=ec, in0=lc, in1=cmax.to_broadcast([128, 4, 4]), op=ALU.subtract), RT, ["rt"])
                P.v(lambda e: e.tensor_scalar(out=ohg, in0=ec, scalar1=0.0, scalar2=None, op0=ALU.is_ge), RT, ["rt"])
                P.act(ec, ec, AF.Exp, RT, ["rt"])
                P.v(lambda e: e.tensor_reduce(out=csum, in_=ec, axis=AX.X, op=ALU.add), RT, ["rt"])
                P.v(lambda e: e.reciprocal(out=csum, in_=csum), RT, ["rt"])
                P.v(lambda e: e.tensor_scalar(out=ohg, in0=ohg, scalar1=-1.0, scalar2=1e30, op0=ALU.add, op1=ALU.mult), RT, ["rt"])
                P.v(lambda e: e.tensor_tensor(out=msk.rearrange("p b (g e) -> p b g e", e=4), in0=lf.rearrange("p b (g e) -> p b g e", e=4),
                                              in1=ohg.unsqueeze(3).to_broadcast([128, 4, 4, 4]), op=ALU.add), RT, ["rt"])
                P.v(lambda e: e.tensor_reduce(out=v1, in_=msk, axis=AX.X, op=ALU.max), RT, ["rt"])
                P.v(lambda e: e.tensor_tensor(out=oh1, in0=msk, in1=v1.to_broadcast([128, 4, 16]), op=ALU.is_ge), RT, ["rt"])
                P.v(lambda e: e.scalar_tensor_tensor(out=msk, in0=oh1, scalar=-1e30, in1=msk, op0=ALU.mult, op1=ALU.add), RT, ["rt"])
                P.v(lambda e: e.tensor_reduce(out=v2, in_=msk, axis=AX.X, op=ALU.max), RT, ["rt"])
                P.v(lambda e: e.tensor_tensor(out=oh2, in0=msk, in1=v2.to_broadcast([128, 4, 16]), op=ALU.is_ge), RT, ["rt"])
                P.v(lambda e: e.tensor_tensor(out=p1, in0=v1, in1=v2, op=ALU.subtract), RT, ["rt"])
                P.act(p1, p1, AF.Sigmoid, RT, ["rt"])
                P.v(lambda e: e.tensor_scalar(out=p2, in0=p1, scalar1=-1.0, scalar2=1.0, op0=ALU.mult, op1=ALU.add), RT, ["rt"])
                P.v(lambda e: e.tensor_tensor(out=p1, in0=p1, in1=csum, op=ALU.mult), RT, ["rt"])
                P.v(lambda e: e.tensor_tensor(out=p2, in0=p2, in1=csum, op=ALU.mult), RT, ["rt"])
                P.v(lambda e: e.tensor_tensor(out=oh1, in0=oh1, in1=p1.to_broadcast([128, 4, 16]), op=ALU.mult), RT, ["rt"])
                P.v(lambda e: e.tensor_tensor(out=oh2, in0=oh2, in1=p2.to_broadcast([128, 4, 16]), op=ALU.mult), RT, ["rt"])
                P.v(lambda e, gg=gg: e.tensor_tensor(out=GT[:, gg * 4:(gg + 1) * 4, :], in0=oh1, in1=oh2, op=ALU.add), RT, ["GT"])
            chk('R2')
            P.fence()

            def YA(bi):
                return (R2f_ if bi < 8 else R0f)[:, (bi % 8) * 1024:(bi % 8 + 1) * 1024]

            NEX = int(os.environ.get('DBG_EX', 16))
            units = [(ex, g4) for ex in range(NEX) for g4 in range(4)]
            wviews = {}

            def wl(ex):
                wt = W[ex % 2]
                wg = wt[:, 0:4096].rearrange("p (k n) -> p k n", k=8)
                wu = wt[:, 4096:8192].rearrange("p (k n) -> p k n", k=8)
                wd = wt[:, 8192:12288].rearrange("p (k n) -> p k n", k=4)
                for k in range(8):
                    P.dma("gpsimd", wg[:, k, :], I["moe_w_gate"][l][ex][k * 128:(k + 1) * 128, :], ["wsrc"], [wt])
                    P.dma("gpsimd", wu[:, k, :], I["moe_w_up"][l][ex][k * 128:(k + 1) * 128, :], ["wsrc"], [wt])
                for k in range(4):
                    P.dma("gpsimd", wd[:, k, :], I["moe_w_down"][l][ex][k * 128:(k + 1) * 128, :], ["wsrc"], [wt])
                wviews[ex] = (wt, wg, wu, wd)

            def stageA(ex, g4):
                if g4 == 0:
                    wl(ex)
                wt, wg, wu, wd = wviews[ex]
                t0 = g4 * 512
                aT = HT.next()
                for fc in range(4):
                    pg = PG.next()
                    gemm_fm(pg, wg, fc * 128, 128, HH, [wt, "HH"], t0=t0)
                    pu = PA[fc % 2]
                    gemm_fm(pu, wu, fc * 128, 128, HH, [wt, "HH"], t0=t0)
                    sg_ = PT.next()
                    P.act(sg_[:], pg[:, :], AF.Silu, [pg], [sg_])
                    P.v(lambda e, aT=aT, fc=fc, sg_=sg_, pu=pu: e.tensor_tensor(out=aT[:, fc, :], in0=pu[:, :], in1=sg_[:], op=ALU.mult),
                        [pu, sg_], [aT])
                return aT

            def stageB(ex, g4, aT):
                wt, wg, wu, wd = wviews[ex]
                for b in range(4):
                    bi = g4 * 4 + b
                    for hf in range(2):
                        py = PA[2 + (b * 2 + hf) % 2]
                        for fc in range(4):
                            P.mm(py[:, :], aT[:, fc, b * 128:(b + 1) * 128], wd[:, fc, hf * 512:(hf + 1) * 512], fc == 0, fc == 3,
                                 [aT, wt], [py])
                        ysl = YA(bi)[:, hf * 512:(hf + 1) * 512]
                        if ex == 0:
                            P.v(lambda e, ysl=ysl, py=py, bi=bi, ex=ex: e.tensor_scalar(out=ysl, in0=py[:, :], scalar1=GT[:, bi, ex:ex + 1],
                                                                                        scalar2=None, op0=ALU.mult), [py, "GT"], [("YA", bi)])
                        else:
                            P.v(lambda e, ysl=ysl, py=py, bi=bi, ex=ex: e.scalar_tensor_tensor(out=ysl, in0=py[:, :], scalar=GT[:, bi, ex:ex + 1],
                                                                                               in1=ysl, op0=ALU.mult, op1=ALU.add),
                                [py, "GT", ("YA", bi)], [("YA", bi)])

            prevu = None
            for (ex, g4) in units:
                aT = stageA(ex, g4)
                if prevu is not None:
                    stageB(*prevu)
                prevu = (ex, g4, aT)
            stageB(*prevu)
            for bi in range(16):
                blk = half * 16 + bi
                xb = XB.next()
                P.dma("sync", xb[:], xs[blk * 128:(blk + 1) * 128, :], [("xs", blk)], [xb])
                ysl = YA(bi)
                P.v(lambda e, ysl=ysl: e.tensor_tensor(out=ysl, in0=ysl, in1=gtB[:], op=ALU.mult), [("YA", bi), gtB], [("YA", bi)])
                P.g(lambda e, xb=xb, ysl=ysl: e.tensor_tensor(out=xb[:], in0=xb[:], in1=ysl, op=ALU.add), [xb, ("YA", bi)], [xb])
                P.dma("sync", xs[blk * 128:(blk + 1) * 128, :], xb[:], [xb], [("xs", blk)])
            P.fence()

    try:
        if only == "moe":
            for blk in range(NB):
                xb = XB.next()
                P.dma("sync", xb[:], I["x"][blk * 128:(blk + 1) * 128, :], ["xin"], [xb])
                P.dma("sync", xs[blk * 128:(blk + 1) * 128, :], xb[:], [xb], [("xs", blk)])
            P.fence()
            moe(0)
            raise _Stop()
        if only == "cd":
            for blk in range(NB):
                xb = XB.next()
                P.dma("sync", xb[:], I["x"][blk * 128:(blk + 1) * 128, :], ["xin"], [xb])
                P.dma("sync", xs[blk * 128:(blk + 1) * 128, :], xb[:], [xb], [("xs", blk)])
            P.fence()
            layer_cd(1)
            raise _Stop()
        for l in range(nlayers):
            if l % 2 == 0:
                layer_ab(l)
            else:
                layer_cd(l)
            chk('MIX')
            moe(l)
    except _Stop:
        P.fence()

    P.dma("sync", gsB[:], bc(I["g_final"][0:1, :]), ["g"], [gsB])
    for blk in range(NB):
        xb = XB.next()
        P.dma("sync", xb[:], (xs if nlayers > 0 else I["x"])[blk * 128:(blk + 1) * 128, :], [("xs", blk)], [xb])
        P.act(junk, xb[:], AF.Square, [xb], [ET.t[0], "ss"], accum_out=sm[:, 0:1])
        P.v(lambda e: e.tensor_scalar(out=sm[:, 1:2], in0=sm[:, 0:1], scalar1=1.0 / D, scalar2=1e-6, op0=ALU.mult, op1=ALU.add), ["ss"], ["rs"])
        P.act(sm[:, 1:2], sm[:, 1:2], AF.Sqrt, ["rs"], ["rs"])
        P.v(lambda e: e.reciprocal(out=sm[:, 1:2], in_=sm[:, 1:2]), ["rs"], ["rs"])
        hm = HM.next()
        P.v(lambda e, hm=hm, xb=xb: e.scalar_tensor_tensor(out=hm[:], in0=xb[:], scalar=sm[:, 1:2], in1=gsB[:], op0=ALU.mult, op1=ALU.mult),
            [xb, "rs", gsB], [hm])
        P.dma("sync", out[blk * 128:(blk + 1) * 128, :], hm[:], [hm], ["out"])
    P.finish()
    return nc


_CACHE = {}


def kernel(**inputs):
    f32 = lambda a: np.ascontiguousarray(np.asarray(a, dtype=np.float32))
    inp = {k: f32(v) for k, v in inputs.items()}
    consts = _host_consts()
    shared = {}
    for k in ("w_ada", "b_ada", "g_mix", "g_ffn", "rel_bias", "ab_w_in", "ab_conv_w", "ab_conv_b", "ab_hnorm_g", "ab_w_out",
              "cd_w_in", "cd_w_alpha", "cd_b_alpha", "cd_hnorm_g", "cd_w_out", "moe_w_coarse", "moe_b_coarse", "moe_w_fine"):
        shared[k] = inp[k]
    shared["g_final"] = inp["g_final"].reshape(1, D)
    shared["ab_gate_b"] = inp["ab_gate_b"].reshape(2, 8)
    shared["ab_w_uk"] = inp["ab_w_uk"].reshape(2, 128, 512)
    shared["ab_w_uv"] = inp["ab_w_uv"].reshape(2, 128, 512)
    shared["moe_b_fine"] = inp["moe_b_fine"].reshape(4, 16)
    shared["moe_w_gate"] = inp["moe_w_gate"].reshape(4, 16, D, 512)
    shared["moe_w_up"] = inp["moe_w_up"].reshape(4, 16, D, 512)
    shared["moe_w_down"] = inp["moe_w_down"].reshape(4, 16, 512, D)
    shared.update(consts)
    if "nc" not in _CACHE:
        _CACHE["nc"] = build_nc()
    nc = _CACHE["nc"]
    in_maps = []
    for b in range(8):
        m = dict(shared)
        m["x"] = inp["x"][b]
        m["c"] = inp["c"][b].reshape(8, 128)
        in_maps.append(m)
    res = run_bass_kernel_spmd(nc, in_maps, core_ids=list(range(8)))
    return np.stack([np.asarray(r["out"], dtype=np.float32) for r in res.results], axis=0)
```

```python
import math
import os
from contextlib import ExitStack
import numpy as np
import concourse.bass as bass
import concourse.mybir as mybir
from concourse.bass_utils import run_bass_kernel_spmd

F32 = mybir.dt.float32
BF16 = mybir.dt.bfloat16
ALU = mybir.AluOpType
AF = mybir.ActivationFunctionType
AX = mybir.AxisListType


class _Op:
    __slots__ = ("eng", "fn", "deps", "signal", "sidx", "is_dma", "dslot", "dval", "event")

    def __init__(self, eng, fn, is_dma):
        self.eng = eng
        self.fn = fn
        self.is_dma = is_dma
        self.deps = []
        self.signal = False
        self.sidx = 0
        self.dslot = 0
        self.dval = 0
        self.event = None


def _key(k):
    if isinstance(k, str):
        return k
    if isinstance(k, tuple):
        return _key(k[0]) + "#" + "#".join(str(i) for i in k[1:])
    return k.name


class Prog:
    ENGS = ("tensor", "vector", "scalar", "gpsimd", "sync")
    EPOCH = 16000
    NDSEM = 8

    def __init__(self, nc):
        self.nc = nc
        self.es = ExitStack()
        self.ops = {e: [] for e in self.ENGS}
        self.lastw = {}
        self.readers = {}
        self.ndma = {e: 0 for e in self.ENGS}
        self.lastop = {}
        self.lastdma = {}

    def sb(self, name, shape, dt):
        return self.es.enter_context(self.nc.sbuf_tensor(name, list(shape), dt))

    def ps(self, name, shape, dt):
        return self.es.enter_context(self.nc.psum_tensor(name, list(shape), dt))

    def dram(self, name, shape, dt):
        return self.nc.dram_tensor(name, list(shape), dt, kind="Internal").ap()

    def add(self, eng, fn, reads=(), writes=(), is_dma=False):
        op = _Op(eng, fn, is_dma)
        deps = {}
        rk = [_key(k) for k in reads]
        wk = [_key(k) for k in writes]
        for k in rk:
            w = self.lastw.get(k)
            if w is not None:
                deps[id(w)] = w
        for k in wk:
            w = self.lastw.get(k)
            if w is not None:
                deps[id(w)] = w
            for r in self.readers.get(k, {}).values():
                deps[id(r)] = r
        for d in deps.values():
            if d is op:
                continue
            if (not is_dma) and (not d.is_dma) and d.eng == eng == "tensor":
                continue
            op.deps.append(d)
            d.signal = True
        if is_dma:
            n = self.ndma[eng]
            self.ndma[eng] = n + 1
            op.dslot = n % self.NDSEM
            op.dval = 16 * (n // self.NDSEM + 1)
            self.lastdma[(eng, op.dslot)] = op
        else:
            self.lastop[eng] = op
        rkey = (eng, op.dslot) if is_dma else eng
        for k in rk:
            self.readers.setdefault(k, {})[rkey] = op
        for k in wk:
            self.lastw[k] = op
            self.readers[k] = {}
        self.ops[eng].append(op)
        return op

    def fence(self):
        allops = list(self.lastop.values()) + list(self.lastdma.values())
        for e in self.ENGS:
            op = _Op(e, None, False)
            for d in allops:
                if d.eng == e and not d.is_dma:
                    continue
                op.deps.append(d)
                d.signal = True
            self.ops[e].append(op)
        self.lastw = {}
        self.readers = {}

    def dma(self, q, out, in_, reads, writes, **kw):
        return self.add(q, lambda e: e.dma_start(out=out, in_=in_, **kw), reads, writes, is_dma=True)

    def act(self, out, in_, func, reads, writes, **kw):
        return self.add("scalar", lambda e: e.activation(out=out, in_=in_, func=func, **kw), reads, writes)

    def mm(self, out, lhsT, rhs, start, stop, reads, writes):
        return self.add("tensor", lambda e: e.matmul(out, lhsT=lhsT, rhs=rhs, start=start, stop=stop),
                        reads, writes)

    def tr(self, out, in_, ident, reads, writes):
        return self.add("tensor", lambda e: e.transpose(out, in_, ident), reads, writes)

    def v(self, fn, reads, writes):
        return self.add("vector", fn, reads, writes)

    def g(self, fn, reads, writes):
        return self.add("gpsimd", fn, reads, writes)

    def finish(self):
        nc = self.nc
        es = self.es
        esem = {}
        for e in self.ENGS:
            c = 0
            for op in self.ops[e]:
                if op.is_dma:
                    continue
                if op.signal:
                    c += 1
                    op.sidx = c
            nep = c // self.EPOCH + 1
            esem[e] = [es.enter_context(nc.semaphore(f"s_{e}_{i}")) for i in range(nep)]
        dsem = {}
        for e in self.ENGS:
            if self.ndma[e]:
                dsem[e] = [es.enter_context(nc.semaphore(f"d_{e}_{i}")) for i in range(self.NDSEM)]
        for e in self.ENGS:
            for op in self.ops[e]:
                if op.is_dma:
                    op.event = (dsem[e][op.dslot], op.dval)
                elif op.signal:
                    i = op.sidx - 1
                    op.event = (esem[e][i // self.EPOCH], i % self.EPOCH + 1)

        def emit(eng, e):
            waited = {}

            def wait(ev):
                sem, val = ev
                k = sem.name
                if waited.get(k, 0) < val:
                    eng.wait_ge(sem, val)
                    waited[k] = val

            for op in self.ops[e]:
                mx = {}
                for d in op.deps:
                    sem, val = d.event
                    k = sem.name
                    if k not in mx or mx[k][1] < val:
                        mx[k] = (sem, val)
                for ev in mx.values():
                    wait(ev)
                if op.fn is None:
                    continue
                if op.is_dma:
                    if op.dval > 16:
                        wait((dsem[e][op.dslot], op.dval - 16))
                    op.fn(eng).then_inc(dsem[e][op.dslot], 16)
                else:
                    ins = op.fn(eng)
                    if op.signal:
                        ins.then_inc(*((op.event[0], 1)))
            if e == "sync":
                for q in self.ENGS:
                    n = self.ndma[q]
                    for s in range(min(n, self.NDSEM)):
                        last = ((n - 1 - s) // self.NDSEM) * self.NDSEM + s
                        wait((dsem[q][s], 16 * (last // self.NDSEM + 1)))

        with nc.Block() as block:
            @block.tensor
            def _(eng):
                emit(eng, "tensor")

            @block.vector
            def _(eng):
                emit(eng, "vector")

            @block.scalar
            def _(eng):
                emit(eng, "scalar")

            @block.gpsimd
            def _(eng):
                emit(eng, "gpsimd")

            @block.sync
            def _(eng):
                emit(eng, "sync")
        es.close()


L = 4096
D = 1024
NB = 32
NG = 8
DEPTH = 4
W_AB = 2508
W_CD = 3088
NEG = -30000.0
NFR = 3072
SU = 2944
NIT = 18
TOPK = 256


def _rel_bucket_np(d):
    n = np.maximum(d, 0)
    nf = np.maximum(n, 1).astype(np.float32)
    large = 16 + (np.log(nf / np.float32(16)) / np.float32(math.log(2048 / 16)) * np.float32(16)).astype(np.int32)
    large = np.minimum(large, 31)
    return np.where(n < 16, n, large)


def _host_consts():
    c = {}
    c["ident"] = np.eye(128, dtype=np.float32)
    c["exch"] = np.eye(128, dtype=np.float32)[::-1].copy()
    mk = np.zeros((128, 4, 512), np.float32)
    p = np.arange(128)[:, None]
    u = np.arange(512)[None, :]
    for j in range(4):
        mk[:, j, :] = np.where(u - 128 * j - p >= 0, 0.0, NEG)
    c["maskneg"] = mk
    c["cmT"] = np.where(np.arange(128)[None, :] > np.arange(128)[:, None], -1e30, 0.0).astype(np.float32)
    i = np.arange(NFR)
    d = i - 511
    bk = _rel_bucket_np(d)
    oh = np.zeros((32, NFR), np.float32)
    oh[bk, i] = 1.0
    oh[:, d < 0] = 0.0
    c["oh"] = oh
    add = np.zeros((2, 8, NFR), np.float32)
    add[0, :, d < 0] = NEG
    cnt = ((d <= 128).astype(np.float32) + ((d % 4 == 0) & (d <= 512)) + ((d % 16 == 0) & (d <= 2048)))
    dil = np.where((d >= 0) & (cnt > 0), np.log(np.maximum(cnt, 1.0)), NEG).astype(np.float32)
    add[1, :, :] = dil[None, :]
    c["addrow"] = add
    c["ck"] = np.broadcast_to((0.5 ** (np.arange(NIT) + 1)).astype(np.float32)[None, :], (128, NIT)).copy()
    return c


class _Rot:
    def __init__(self, tiles):
        self.t = tiles
        self.i = 0

    def next(self):
        t = self.t[self.i % len(self.t)]
        self.i += 1
        return t


class _Stop(Exception):
    pass


def build_nc(nlayers=DEPTH, dbg=False, stop=None, only=None):
    def chk(name):
        if stop == name:
            raise _Stop()

    nc = bass.Bass("TRN2", target_bir_lowering=False)
    P = Prog(nc)
    I = {}

    def inp(name, shape):
        I[name] = nc.dram_tensor(name, list(shape), F32, kind="ExternalInput").ap()

    WL = max(1, nlayers) if dbg else 4
    for name, shape in [
        ("x", (L, D)), ("c", (8, 128)), ("w_ada", (WL, D, 6 * D)), ("b_ada", (4, 6 * D)), ("g_mix", (4, D)),
        ("g_ffn", (4, D)), ("g_final", (1, D)), ("rel_bias", (32, 8)), ("ab_w_in", (2, D, W_AB)),
        ("ab_conv_w", (2, 4, 512)), ("ab_conv_b", (2, 512)), ("ab_gate_b", (2, 8)), ("ab_hnorm_g", (2, 512)),
        ("ab_w_uk", (2, 128, 512)), ("ab_w_uv", (2, 128, 512)), ("ab_w_out", (2, D, D)),
        ("cd_w_in", (2, D, W_CD)), ("cd_w_alpha", (2, 16, 256)), ("cd_b_alpha", (2, 256)),
        ("cd_hnorm_g", (2, 512)), ("cd_w_out", (2, D, D)), ("moe_w_coarse", (4, D, 4)), ("moe_b_coarse", (4, 4)),
        ("moe_w_fine", (4, 4, D, 4)), ("moe_b_fine", (4, 16)), ("moe_w_gate", (WL, 16, D, 512)),
        ("moe_w_up", (WL, 16, D, 512)), ("moe_w_down", (WL, 16, 512, D)),
        ("ident", (128, 128)), ("exch", (128, 128)), ("maskneg", (128, 4, 512)), ("cmT", (128, 128)),
        ("oh", (32, NFR)), ("addrow", (2, 8, NFR)), ("ck", (128, NIT)),
    ]:
        inp(name, shape)
    out = nc.dram_tensor("out", [L, D], F32, kind="ExternalOutput").ap()
    okind = "ExternalOutput" if dbg else "Internal"
    xs = nc.dram_tensor("xs", [L, D], F32, kind=okind).ap()
    Yd = nc.dram_tensor("Yd", [8, 128, L], BF16, kind=okind).ap()
    modd = nc.dram_tensor("modd", [4, 6 * D], F32, kind=okind).ap()
    Frow = nc.dram_tensor("Frow", [2, 8, NFR], F32, kind="Internal").ap()
    gated = nc.dram_tensor("gated", [2, 128, L], BF16, kind="Internal").ap()
    gated = nc.dram_tensor("gated4", [4, 128, L], BF16, kind="Internal").ap()
    rowsd = nc.dram_tensor("rowsd", [2, 4, L], F32, kind="Internal").ap()
    nmtd = nc.dram_tensor("nmtd", [NB, 128, L], BF16, kind="Internal").ap()
    hTd = nc.dram_tensor("hTd", [8, 128, L], BF16, kind="Internal").ap()

    R = [P.sb(f"R{i}", [128, 16384], BF16) for i in range(3)]
    W = [P.sb(f"W{i}", [128, 12416], BF16) for i in range(2)]
    ident_f = P.sb("ident_f", [128, 128], F32)
    ident_b = P.sb("ident_b", [128, 128], BF16)
    exch_b = P.sb("exch_b", [128, 128], BF16)
    ones_b = P.sb("ones_b", [128, 128], BF16)
    ones_f = P.sb("ones_f", [128, 128], F32)
    maskneg = P.sb("maskneg_t", [128, 4, 512], BF16)
    mask01 = maskneg
    cmT = P.sb("cmT_t", [128, 128], F32)
    eps_t = P.sb("eps_t", [128, 1], F32)
    ckt = P.sb("ckt", [128, NIT], F32)
    gsB = P.sb("gsB", [128, D], F32)
    shB = P.sb("shB", [128, D], F32)
    gtB = P.sb("gtB", [128, D], F32)
    XB = _Rot([P.sb(f"xb{i}", [128, D], F32) for i in range(1)])
    HM = _Rot([P.sb(f"hm{i}", [128, D], F32) for i in range(1)])
    HT = _Rot([P.sb(f"hT{i}", [128, 8, 512], BF16) for i in range(2)])
    sm = P.sb("sm", [128, 64], F32)
    PT = _Rot([P.sb(f"pt{i}", [128, 512], BF16) for i in range(3)])
    ET = _Rot([P.sb(f"et{i}", [128, 512], F32) for i in range(2)])
    junk = ET.t[0][:, :].bitcast(BF16)
    FT = _Rot([P.sb(f"ft{i}", [128, 512], F32) for i in range(4)])
    ST = _Rot([P.sb(f"st{i}", [128, 512], BF16) for i in range(2)])
    pT = P.ps("pT", [128, 1024], F32)
    PG = _Rot([P.ps(f"pG{i}", [128, 512], F32) for i in range(2)])
    PA = [P.ps(f"pA{i}", [128, 512], F32) for i in range(4)]

    def bc(row_ap, n=D):
        return row_ap.to_broadcast([128, n])

    P.dma("sync", ident_f[:], I["ident"], ["c_ident"], [ident_f])
    P.v(lambda e: e.tensor_copy(out=ident_b[:], in_=ident_f[:]), [ident_f], [ident_b])
    P.dma("gpsimd", exch_b[:], I["exch"], ["c_exch"], [exch_b])
    P.v(lambda e: e.memset(ones_b[:], 1.0), [], [ones_b])
    P.v(lambda e: e.memset(ones_f[:], 1.0), [], [ones_f])
    P.v(lambda e: e.memset(eps_t[:], 1e-6), [], [eps_t])
    P.dma("sync", cmT[:], I["cmT"], ["c_cm"], [cmT])
    P.dma("sync", ckt[:], I["ck"], ["c_ck"], [ckt])

    relt = P.sb("relt", [32, 8], F32)
    P.dma("sync", relt[:], I["rel_bias"], ["c_rel"], [relt])
    oht = R[0][0:32, 0:2 * NFR].bitcast(F32)
    P.dma("sync", oht, I["oh"], ["c_oh"], ["oht"])
    for ty in range(2):
        addt = R[1][0:8, 0:2 * NFR].bitcast(F32)
        P.dma("sync", addt, I["addrow"][ty], ["c_add"], ["addt"])
        for j in range(NFR // 512):
            pg = PG.next()
            P.mm(pg[0:8, :], relt[:, :], oht[:, j * 512:(j + 1) * 512], True, True, [relt, "oht"], [pg])
            P.v(lambda e, pg=pg, j=j, addt=addt: e.tensor_tensor(out=addt[:, j * 512:(j + 1) * 512], in0=pg[0:8, :],
                                                                  in1=addt[:, j * 512:(j + 1) * 512], op=ALU.add),
                [pg, "addt"], ["addt"])
        P.dma("sync", Frow[ty], addt, ["addt"], [("Frow", ty)])
    P.fence()

    c8 = P.sb("c8", [8, 128], F32)
    cs = P.sb("cs", [128, 8], F32)
    P.dma("sync", c8[:], I["c"], ["c_c"], [c8])
    pg = PG.next()
    P.tr(pg[:, 0:8], c8[:, :], ident_f[0:8, 0:8], [c8, ident_f], [pg])
    P.act(cs[:], pg[:, 0:8], AF.Silu, [pg], [cs])
    R2f = R[2][:, :].bitcast(F32)
    brow = _Rot([R2f[0:1, i * 512:(i + 1) * 512] for i in range(2)])
    mrow = _Rot([R2f[0:1, (2 + i) * 512:(3 + i) * 512] for i in range(2)])
    wi_ = 0
    for l in range(nlayers):
        for j in range(12):
            wt = W[wi_ % 2]
            wi_ += 1
            wv = wt[:, 0:8192].bitcast(F32).rearrange("p (k n) -> p k n", k=8)
            P.dma("sync", wv, I["w_ada"][l][:, j * 512:(j + 1) * 512].rearrange("(k p) n -> p k n", p=128),
                  ["w_ada"], [wt])
            br = brow.next()
            brk = f"brow{j % 2}"
            mrk = f"mrow{j % 2}"
            P.dma("sync", br, I["b_ada"][l:l + 1, j * 512:(j + 1) * 512], ["b_ada"], [brk])
            pg = PG.next()
            for k in range(8):
                P.mm(pg[0:1, :], cs[:, k:k + 1], wv[:, k, :], k == 0, k == 7, [cs, wt], [pg])
            mr = mrow.next()
            P.v(lambda e, mr=mr, pg=pg, br=br: e.tensor_tensor(out=mr, in0=pg[0:1, :], in1=br, op=ALU.add),
                [pg, brk], [mrk])
            P.dma("sync", modd[l:l + 1, j * 512:(j + 1) * 512], mr, [mrk], [("modd", l)])
    P.fence()

    def modrow(l, j):
        return modd[l:l + 1, j * D:(j + 1) * D]

    def prep_norm(l, ffn):
        o = 3 if ffn else 0
        grow = (I["g_ffn"] if ffn else I["g_mix"])[l:l + 1, :]
        gB = HM.next()
        P.dma("sync", gB[:], bc(grow), ["g"], [gB])
        P.dma("sync", gsB[:], bc(modrow(l, o + 1)), [("modd", l)], [gsB])
        P.dma("sync", shB[:], bc(modrow(l, o + 0)), [("modd", l)], [shB])
        P.dma("sync", gtB[:], bc(modrow(l, o + 2)), [("modd", l)], [gtB])
        P.v(lambda e: e.scalar_tensor_tensor(out=gsB[:], in0=gsB[:], scalar=1.0, in1=gB[:], op0=ALU.add, op1=ALU.mult),
            [gsB, gB], [gsB])

    def xsrc(l):
        return I["x"] if l == 0 else xs

    def norm_block(xsrc_ap, blk, ht, b, htf=None):
        xb = XB.next()
        P.dma("sync", xb[:], xsrc_ap[blk * 128:(blk + 1) * 128, :], [("xs", blk)], [xb])
        ss = sm[:, 0:1]
        P.act(junk, xb[:], AF.Square, [xb], [ET.t[0], "ss"], accum_out=ss)
        P.v(lambda e: e.tensor_scalar(out=sm[:, 1:2], in0=ss, scalar1=1.0 / D, scalar2=1e-6, op0=ALU.mult, op1=ALU.add),
            ["ss"], ["rs"])
        P.act(sm[:, 1:2], sm[:, 1:2], AF.Sqrt, ["rs"], ["rs"])
        P.v(lambda e: e.reciprocal(out=sm[:, 1:2], in_=sm[:, 1:2]), ["rs"], ["rs"])
        hm = HM.next()
        P.v(lambda e, hm=hm, xb=xb: e.scalar_tensor_tensor(out=hm[:], in0=xb[:], scalar=sm[:, 1:2], in1=gsB[:],
                                                           op0=ALU.mult, op1=ALU.mult), [xb, "rs", gsB], [hm])
        P.g(lambda e, hm=hm: e.tensor_tensor(out=hm[:], in0=hm[:], in1=shB[:], op=ALU.add), [hm, shB], [hm])
        for k in range(8):
            P.tr(pT[:, k * 128:(k + 1) * 128], hm[:, k * 128:(k + 1) * 128], ident_f[:], [hm, ident_f], [pT])
        pv = pT[:, :].rearrange("p (k t) -> p k t", k=8)
        P.act(ht[:, :, b * 128:(b + 1) * 128], pv, AF.Copy, [pT], [ht])
        if htf is not None:
            P.act(htf[:, :, b * 128:(b + 1) * 128], pv, AF.Copy, [pT], ["htf"])

    def ht_store(ht, g):
        for k in range(8):
            P.dma("sync", hTd[k][:, g * 512:(g + 1) * 512], ht[:, k, :], [ht], [("hTd", g)])

    def ht_load(g):
        ht = HT.next()
        for k in range(8):
            P.dma("sync", ht[:, k, :], hTd[k][:, g * 512:(g + 1) * 512], [("hTd", g)], [ht])
        return ht

    def wload(wt, ncols, src2d):
        wv = wt[:, 0:8 * ncols].rearrange("p (k n) -> p k n", k=8)
        for k in range(8):
            P.dma("gpsimd", wv[:, k, :], src2d[k * 128:(k + 1) * 128, :], ["wsrc"], [wt])
        return wv

    def gemm_fm(pg, wv, c0, m, ht, reads, n=512, t0=0):
        for k in range(8):
            P.mm(pg[0:m, 0:n], wv[:, k, c0:c0 + m], ht[:, k, t0:t0 + n], k == 0, k == 7, reads, [pg])

    def gemm_tm(pg, ht, b, wv, c0, n, reads):
        for k in range(8):
            P.mm(pg[:, 0:n], ht[:, k, b * 128:(b + 1) * 128], wv[:, k, c0:c0 + n], k == 0, k == 7, reads, [pg])

    def head_norm_store(l, h, g, raw, hg_col, gate_chunk, ychunk):
        sq = FT.next()
        P.act(sq[:], raw[:], AF.Square, [raw], [sq])
        pg = PG.next()
        P.mm(pg[:, :], ones_f[:, :], sq[:], True, True, [ones_f, sq], [pg])
        rsd = FT.next()
        P.act(rsd[:], pg[:, :], AF.Sqrt, [pg, eps_t], [rsd], scale=1.0 / 128, bias=eps_t[:, 0:1])
        P.v(lambda e: e.reciprocal(out=rsd[:], in_=rsd[:]), [rsd], [rsd])
        P.v(lambda e: e.scalar_tensor_tensor(out=raw[:], in0=raw[:], scalar=hg_col, in1=rsd[:], op0=ALU.mult,
                                             op1=ALU.mult), [raw, rsd, "hgt"], [raw])
        gt_ = PT.next()
        P.dma("sync", gt_[:], gated[gate_chunk][:, g * 512:(g + 1) * 512], [("gated", gate_chunk)], [gt_])
        st = ST.next()
        P.v(lambda e: e.tensor_tensor(out=st[:], in0=raw[:], in1=gt_[:], op=ALU.mult), [raw, gt_], [st])
        P.dma("sync", Yd[ychunk][:, g * 512:(g + 1) * 512], st[:], [st], [("Yd", ychunk)])

    def load_strip(ty, h, stf, stb):
        src = bass.AP(Frow.tensor, (ty * 8 + h) * NFR, [[1, 128], [1, SU]])
        P.dma("sync", stf, src, [("Frow", ty)], ["stf"])
        P.act(stb, stf, AF.Copy, ["stf"], ["stb"])

    def softmax_attn(ty, QTv, KTv, VTv, use_mask, ybase):
        stf = W[0][:, 0:2 * SU].bitcast(F32)
        stb = W[1][:, 0:SU]
        NMT = _Rot([W[1][:, 3072 + i * 512:3072 + (i + 1) * 512] for i in range(4)])
        nmi = 0
        for h in range(8):
            load_strip(ty, h, stf, stb)
            c, po = h // 2, (h % 2) * 64
            for g in range(NG):
                acc_o = PA[(h * NG + g) % 2 * 2]
                acc_d = PA[(h * NG + g) % 2 * 2 + 1]
                kb_lo = 0 if ty == 0 else max(0, 4 * g - 16)
                kbs = list(range(kb_lo, 4 * g + 4))
                def qk(kb):
                    nonlocal nmi
                    delta = 512 * g - 128 * kb
                    col = min(delta, 1664 if ty == 0 else 2048) + 384
                    pl = PG.next()
                    P.mm(pl[:, :], KTv[po:po + 64, c, kb * 128:(kb + 1) * 128], QTv[po:po + 64, c, g * 512:(g + 1) * 512],
                         True, False, ["KT", "QT"], [pl])
                    P.mm(pl[:, :], exch_b[:, :], stb[:, col:col + 512], False, not use_mask, [exch_b, "stb"], [pl])
                    if use_mask:
                        nm = NMT.next()
                        nk = f"nmt{nmi % 4}"
                        nmi += 1
                        P.dma("sync", nm, nmtd[kb][:, g * 512:(g + 1) * 512], ["nmtd"], [nk])
                        P.mm(pl[:, :], ident_b[:, :], nm, False, True, [ident_b, nk], [pl])
                    return pl

                def pv(i, kb, pl):
                    pt = PT.next()
                    P.act(pt[:], pl[:, :], AF.Exp, [pl], [pt])
                    P.mm(acc_o[0:64, :], VTv[:, kb, h * 64:(h + 1) * 64], pt[:], i == 0, i == len(kbs) - 1, ["VT", pt], [acc_o])
                    P.mm(acc_d[0:64, :], ones_b[:, 0:64], pt[:], i == 0, i == len(kbs) - 1, [ones_b, pt], [acc_d])

                prev = None
                for i, kb in enumerate(kbs):
                    pl = qk(kb)
                    if prev is not None:
                        pv(*prev)
                    prev = (i, kb, pl)
                pv(*prev)
                rec = FT.next()
                P.v(lambda e, rec=rec, acc_d=acc_d: e.reciprocal(out=rec[0:64, :], in_=acc_d[0:64, :]), [acc_d], [rec])
                st = ST.next()
                P.v(lambda e, rec=rec, acc_o=acc_o, st=st: e.tensor_tensor(out=st[0:64, :], in0=acc_o[0:64, :],
                                                                          in1=rec[0:64, :], op=ALU.mult),
                    [acc_o, rec], [st])
                P.dma("sync", Yd[ybase + c][po:po + 64, g * 512:(g + 1) * 512], st[0:64, :], [st], [("Yd", ybase + c)])

    def out_proj_residual(l, wsrc):
        wv = wload(W[0], 1024, wsrc)
        for g in range(NG):
            yg = HT.next()
            for k in range(8):
                P.dma("sync", yg[:, k, :], Yd[k][:, g * 512:(g + 1) * 512], [("Yd", k)], [yg])
            for b in range(4):
                blk = 4 * g + b
                xb = XB.next()
                P.dma("sync", xb[:], xsrc(l)[blk * 128:(blk + 1) * 128, :], [("xs", blk)], [xb])
                hm = HM.next()
                for half in range(2):
                    pg = PG.next()
                    gemm_tm(pg, yg, b, wv, half * 512, 512, [yg, W[0]])
                    P.v(lambda e, hm=hm, pg=pg, half=half: e.tensor_tensor(out=hm[:, half * 512:(half + 1) * 512], in0=pg[:, :],
                                                                            in1=gtB[:, half * 512:(half + 1) * 512], op=ALU.mult),
                        [pg, gtB], [hm])
                P.g(lambda e, hm=hm, xb=xb: e.tensor_tensor(out=xb[:], in0=xb[:], in1=hm[:], op=ALU.add), [xb, hm], [xb])
                P.dma("sync", xs[blk * 128:(blk + 1) * 128, :], xb[:], [xb], [("xs", blk)])

    aT_t = P.sb("aT_t", [128, 128], F32)
    hgt = P.sb("hgt", [128, 4], F32)
    cw = P.sb("cw", [128, 4, 4], F32)
    cb = P.sb("cb", [128, 4], F32)
    gb4 = P.sb("gb4", [4, 4], F32)
    one_c = P.sb("one_c", [128, 1], F32)
    wit = P.sb("wit", [128, NB, 4], F32)
    thr_c = P.sb("thr_c", [128, 1], F32)
    steps = P.sb("steps", [128, NIT], F32)
    P.v(lambda e: e.memset(one_c[:], 1.0), [], [one_c])
    P.v(lambda e: e.memset(thr_c[:], -1e29), [], [thr_c])
    NC = dict(allow_slow_non_contiguous=True)

    def store_gated(pg, func, chunk, g):
        st = ST.next()
        P.act(st[:], pg[:, :], func, [pg], [st])
        P.dma("sync", gated[chunk][:, g * 512:(g + 1) * 512], st[:], [st], [("gated", chunk)])

    def lin_attn(l, kind, QKv, qc0, kc0, VTv, hn_src, gla=None):
        for hh in range(4):
            P.dma("sync", hgt[:, hh:hh + 1], hn_src[:, hh * 128:(hh + 1) * 128].rearrange("o p -> p o"), ["hn"], [hgt], **NC)
        for g in range(int(os.environ.get("DBG_G", NG))):
            if kind == 1:
                QTg, KTg = gla(g)
            for h in range(int(os.environ.get("DBG_H", 4))):
                c, po = h // 2, (h % 2) * 64
                acc_n = PA[(h + g * 4) % 2 * 2]
                acc_d = PA[(h + g * 4) % 2 * 2 + 1]
                if kind == 0:
                    negMB = FT.next()
                    P.dma("sync", negMB[:], bc(rowsd[0][h:h + 1, g * 512:(g + 1) * 512], 512), ["rowsd"], [negMB])
                    emB = FT.next()
                    P.dma("sync", emB[:], bc(rowsd[1][h:h + 1, g * 512:(g + 1) * 512], 512), ["rowsd"], [emB])
                nkb = 4 * g + 4

                def qk(kb):
                    pl = PG.next()
                    if kind == 0:
                        P.mm(pl[:, :], QKv[po:po + 64, kc0 + c, kb * 128:(kb + 1) * 128],
                             QKv[po:po + 64, qc0 + c, g * 512:(g + 1) * 512], True, True, ["QK"], [pl])
                    else:
                        P.mm(pl[:, :], KTg[po:po + 64, c, kb * 128:(kb + 1) * 128], QTg[po:po + 64, c, :], True, True,
                             ["KTg", "QTg"], [pl])
                    return pl

                def pv(kb, pl):
                    pt = PT.next()
                    j = kb - 4 * g
                    if kind == 0:
                        src_ = negMB
                        if j >= 0:
                            tmp = ET.next()
                            P.g(lambda e, tmp=tmp, negMB=negMB, j=j: e.tensor_tensor(out=tmp[:], in0=negMB[:], in1=maskneg[:, j, :],
                                                                                      op=ALU.add), [negMB, maskneg], [tmp])
                            src_ = tmp
                        E = ET.next()
                        P.act(E[:], src_[:], AF.Exp, [src_, aT_t], [E], bias=aT_t[:, kb * 4 + h:kb * 4 + h + 1])
                        P.v(lambda e, pt=pt, pl=pl, E=E: e.scalar_tensor_tensor(out=pt[:], in0=pl[:, :], scalar=0.125, in1=E[:],
                                                                                op0=ALU.mult, op1=ALU.mult), [pl, E], [pt])
                    else:
                        if j >= 0:
                            P.v(lambda e, pt=pt, pl=pl, j=j: e.tensor_tensor(out=pt[:], in0=pl[:, :], in1=mask01[:, j, :], op=ALU.mult),
                                [pl, mask01], [pt])
                        else:
                            P.act(pt[:], pl[:, :], AF.Copy, [pl], [pt])
                    P.mm(acc_n[:, :], VTv[:, kb, h * 128:(h + 1) * 128], pt[:], kb == 0, kb == nkb - 1, ["VT", pt], [acc_n])
                    if kind == 0:
                        P.mm(acc_d[:, :], ones_b[:, :], pt[:], kb == 0, kb == nkb - 1, [ones_b, pt], [acc_d])

                prev = None
                for kb in range(nkb):
                    pl = qk(kb)
                    if prev is not None:
                        pv(*prev)
                    prev = (kb, pl)
                pv(*prev)
                raw = FT.next()
                if kind == 0:
                    dn = FT.next()
                    P.act(dn[:], acc_d[:, :], AF.Abs, [acc_d], [dn])
                    P.v(lambda e, dn=dn, emB=emB: e.tensor_tensor(out=dn[:], in0=dn[:], in1=emB[:], op=ALU.max), [dn, emB], [dn])
                    P.v(lambda e, dn=dn: e.reciprocal(out=dn[:], in_=dn[:]), [dn], [dn])
                    P.v(lambda e, raw=raw, acc_n=acc_n, dn=dn: e.tensor_tensor(out=raw[:], in0=acc_n[:, :], in1=dn[:], op=ALU.mult),
                        [acc_n, dn], [raw])
                else:
                    P.act(raw[:], acc_n[:, :], AF.Copy, [acc_n], [raw])
                head_norm_store(l, h, g, raw, hgt[:, h:h + 1], h, h)

    def layer_ab(l):
        j = l // 2
        win = I["ab_w_in"][j]
        P.dma("gpsimd", maskneg[:], I["maskneg"], ["c_mk"], [maskneg])
        prep_norm(l, False)
        wv = wload(W[0], 1544, win[:, 0:1544])
        QKraw = R[0][:, :].rearrange("p (c t) -> p c t", c=4)
        R1f = R[1][:, :].bitcast(F32)
        IA = R1f[0:4, 0:L]
        FA = R1f[0:4, L:2 * L]
        VA = R[2][:, :].rearrange("p (b n) -> p b n", b=NB)
        for g in range(NG):
            ht = HT.next()
            for b in range(4):
                norm_block(xsrc(l), 4 * g + b, ht, b)
            ht_store(ht, g)
            for cch in range(4):
                pg = PG.next()
                gemm_fm(pg, wv, cch * 128, 128, ht, [W[0], ht])
                P.act(QKraw[:, cch, g * 512:(g + 1) * 512], pg[:, :], AF.Copy, [pg], ["QKraw"])
            for cch in range(4):
                pg = PG.next()
                gemm_fm(pg, wv, 1024 + cch * 128, 128, ht, [W[0], ht])
                store_gated(pg, AF.Sigmoid, cch, g)
            pg = PG.next()
            gemm_fm(pg, wv, 1536, 4, ht, [W[0], ht])
            P.v(lambda e, pg=pg, g=g: e.tensor_copy(out=IA[:, g * 512:(g + 1) * 512], in_=pg[0:4, :]), [pg], ["IA"])
            pg = PG.next()
            gemm_fm(pg, wv, 1540, 4, ht, [W[0], ht])
            P.v(lambda e, pg=pg, g=g: e.tensor_copy(out=FA[:, g * 512:(g + 1) * 512], in_=pg[0:4, :]), [pg], ["FA"])
            for b in range(4):
                pg = PG.next()
                gemm_tm(pg, ht, b, wv, 512, 512, [W[0], ht])
                P.v(lambda e, pg=pg, b=b, g=g: e.tensor_copy(out=VA[:, 4 * g + b, :], in_=pg[:, :]), [pg], ["VT"])
        P.fence()
        chk('A')
        W0f = W[0][:, :].bitcast(F32)
        W1f = W[1][:, :].bitcast(F32)
        NBr = W0f[0:4, 0:L]
        Mr = W1f[0:4, 0:L]
        for tt in range(2):
            P.dma("sync", gb4[:, tt:tt + 1], I["ab_gate_b"][j:j + 1, tt * 4:(tt + 1) * 4].rearrange("o p -> p o"), ["gb"], [gb4], **NC)
        P.v(lambda e: e.tensor_scalar(out=gb4[:, 2:3], in0=gb4[:, 1:2], scalar1=-1.0, scalar2=None, op0=ALU.mult), [gb4], [gb4])
        P.act(FA, FA, AF.Exp, ["FA", gb4], ["FA"], scale=-1.0, bias=gb4[:, 2:3])
        P.act(FA, FA, AF.Ln, ["FA", one_c], ["FA"], bias=one_c[0:4, 0:1])
        P.v(lambda e: e.tensor_tensor_scan(out=NBr, data0=one_c[0:4, 0:1].to_broadcast([4, L]), data1=FA, initial=0.0,
                                           op0=ALU.mult, op1=ALU.add), ["FA", one_c], ["NBr"])
        P.v(lambda e: e.scalar_tensor_tensor(out=IA, in0=IA, scalar=gb4[:, 0:1], in1=NBr, op0=ALU.add, op1=ALU.add),
            ["IA", gb4, "NBr"], ["IA"])
        P.v(lambda e: e.tensor_tensor_scan(out=Mr, data0=IA, data1=IA, initial=-1e30, op0=ALU.max, op1=ALU.max), ["IA"], ["Mr"])
        P.v(lambda e: e.tensor_scalar(out=FA, in0=Mr, scalar1=-1.0, scalar2=None, op0=ALU.mult), ["Mr", "FA"], ["FA"])
        P.dma("sync", rowsd[0], FA, ["FA"], ["rowsd"])
        P.v(lambda e: e.tensor_tensor(out=Mr, in0=NBr, in1=Mr, op=ALU.subtract), ["NBr", "Mr"], ["Mr"])
        P.act(Mr, Mr, AF.Exp, ["Mr"], ["Mr"])
        P.dma("sync", rowsd[1], Mr, ["Mr"], ["rowsd"])
        for b in range(NB):
            P.tr(pT[:, b * 4:(b + 1) * 4], IA[:, b * 128:(b + 1) * 128], ident_f[0:4, 0:4], ["IA", ident_f], [pT])
        P.v(lambda e: e.tensor_copy(out=aT_t[:], in_=pT[:, 0:128]), [pT], [aT_t])
        P.fence()
        chk('G')
        for cc in range(4):
            for tj in range(4):
                P.dma("sync", cw[:, cc, tj:tj + 1], I["ab_conv_w"][j][tj:tj + 1, cc * 128:(cc + 1) * 128].rearrange("o p -> p o"), ["cw"], [cw], **NC)
            P.dma("sync", cb[:, cc:cc + 1], I["ab_conv_b"][j:j + 1, cc * 128:(cc + 1) * 128].rearrange("o p -> p o"), ["cb"], [cb], **NC)
        QKc = R[1][:, :].rearrange("p (c t) -> p c t", c=4)
        for cch in range(4):
            for sg in range(4):
                a0 = sg * 1024
                acc = HM.next()
                P.v(lambda e, acc=acc, cch=cch, a0=a0: e.tensor_scalar(out=acc[:], in0=QKraw[:, cch, a0:a0 + 1024],
                                                                       scalar1=cw[:, cch, 3:4], scalar2=None, op0=ALU.mult),
                    ["QKraw", cw], [acc])
                for tj in (2, 1, 0):
                    s_ = 3 - tj
                    lo = max(a0, s_)
                    P.v(lambda e, acc=acc, cch=cch, a0=a0, lo=lo, s_=s_, tj=tj: e.scalar_tensor_tensor(
                        out=acc[:, lo - a0:1024], in0=QKraw[:, cch, lo - s_:a0 + 1024 - s_], scalar=cw[:, cch, tj:tj + 1],
                        in1=acc[:, lo - a0:1024], op0=ALU.mult, op1=ALU.add), ["QKraw", cw, acc], [acc])
                P.act(QKc[:, cch, a0:a0 + 1024], acc[:], AF.Silu, [acc, cb], ["QK"], bias=cb[:, cch:cch + 1])
        P.fence()
        chk('C')
        lin_attn(l, 0, QKc, 0, 2, VA, I["ab_hnorm_g"][j:j + 1, :])
        P.fence()
        chk('M')
        wvb = W[0][:, 0:8 * 388].rearrange("p (k n) -> p k n", k=8)
        for k in range(8):
            rows = slice(k * 128, (k + 1) * 128)
            P.dma("gpsimd", wvb[:, k, 0:256], win[rows, 2184:2440], ["wsrc"], [W[0]])
            P.dma("gpsimd", wvb[:, k, 256:320], win[rows, 2440:2504], ["wsrc"], [W[0]])
            P.dma("gpsimd", wvb[:, k, 320:384], win[rows, 2440:2504], ["wsrc"], [W[0]])
            P.dma("gpsimd", wvb[:, k, 384:388], win[rows, 2504:2508], ["wsrc"], [W[0]])
        QI = R[0][:, 0:8192].rearrange("p (c t) -> p c t", c=2)
        KI2 = R[0][:, 8192:12288]
        for g in range(NG):
            ht = ht_load(g)
            for cch in range(2):
                pg = PG.next()
                gemm_fm(pg, wvb, cch * 128, 128, ht, [W[0], ht])
                P.act(QI[:, cch, g * 512:(g + 1) * 512], pg[:, :], AF.Copy, [pg], ["QI"])
            pg = PG.next()
            gemm_fm(pg, wvb, 256, 128, ht, [W[0], ht])
            P.act(KI2[:, g * 512:(g + 1) * 512], pg[:, :], AF.Copy, [pg], ["KI2"])
            for b in range(4):
                pg = PG.next()
                gemm_tm(pg, ht, b, wvb, 384, 4, [W[0], ht])
                P.v(lambda e, pg=pg, b=b, g=g: e.tensor_scalar(out=wit[:, 4 * g + b, :], in0=pg[:, 0:4], scalar1=0.5, scalar2=None,
                                                               op0=ALU.mult), [pg], [wit])
        P.fence()
        chk('B1')
        pTb = pT[:, :].bitcast(BF16)
        CH = [
            dict(S=W[0][:, 0:8192].bitcast(F32), nmb=W[1][:, 0:L], NMs=W[1][:, L:2 * L].rearrange("p (k t) -> p k t", k=NB),
                 jkb=R[1][:, 0:L], sb=8, steps=steps[:, :], k="a"),
            dict(S=R[2][:, 0:8192].bitcast(F32), nmb=R[1][:, 8192:12288], NMs=R[1][:, 12288:16384].rearrange("p (k t) -> p k t", k=NB),
                 jkb=R[1][:, L:2 * L], sb=16, steps=sm[:, 32:32 + NIT], k="b"),
        ]

        def idx_gen(qb, C):
            S, nmb, NMs, jkb, sb, stp, ck_ = C["S"], C["nmb"], C["NMs"], C["jkb"], C["sb"], C["steps"], C["k"]
            kS, kB, kJ, kN, kM, kT = "S" + ck_, "bis" + ck_, "jkb" + ck_, "nmb" + ck_, "NMs" + ck_, "stp" + ck_
            lo_, hi_, dl_, mid_, cnt_, t_ = (sm[:, sb + i:sb + i + 1] for i in range(6))
            nk = (qb + 1) * 128
            for kc in range((nk + 511) // 512):
                w_ = min(512, nk - kc * 512)
                for h4 in range(4):
                    c, po = h4 // 2, (h4 % 2) * 64
                    pl = PG.next()
                    P.mm(pl[:, 0:w_], QI[po:po + 64, c, qb * 128:(qb + 1) * 128], KI2[po:po + 64, kc * 512:kc * 512 + w_], True, True,
                         ["QI", "KI2"], [pl])
                    sl = S[:, kc * 512:kc * 512 + w_]
                    if h4 == 0:
                        P.v(lambda e, sl=sl, pl=pl, w_=w_: e.tensor_scalar(out=sl, in0=pl[:, 0:w_], scalar1=0.0,
                                                                          scalar2=wit[:, qb, 0:1], op0=ALU.max, op1=ALU.mult),
                            [pl, wit], [kS])
                    else:
                        ft = FT.next()
                        P.act(ft[:, 0:w_], pl[:, 0:w_], AF.Relu, [pl], [ft])
                        P.v(lambda e, sl=sl, ft=ft, w_=w_, h4=h4: e.scalar_tensor_tensor(
                            out=sl, in0=ft[:, 0:w_], scalar=wit[:, qb, h4:h4 + 1], in1=sl, op0=ALU.mult, op1=ALU.add),
                            [ft, wit, kS], [kS])
            P.g(lambda e: e.tensor_tensor(out=S[:, qb * 128:(qb + 1) * 128], in0=S[:, qb * 128:(qb + 1) * 128], in1=cmT[:],
                                          op=ALU.add), [kS, cmT], [kS])
            yield
            if qb >= 2:
                P.v(lambda e: e.tensor_reduce(out=hi_, in_=S[:, 0:nk], axis=AX.X, op=ALU.max), [kS], [kB])
                P.v(lambda e: e.tensor_reduce(out=lo_, in_=S[:, 0:256], axis=AX.X, op=ALU.min), [kS, kB], [kB])
                P.v(lambda e: e.scalar_tensor_tensor(out=dl_, in0=hi_, scalar=1.0, in1=lo_, op0=ALU.add, op1=ALU.subtract), [kB], [kB])
                P.v(lambda e: e.tensor_scalar(out=stp, in0=ckt[:], scalar1=dl_, scalar2=None, op0=ALU.mult), [kB, ckt], [kT])
                yield
                for it in range(NIT):
                    P.v(lambda e, it=it: e.tensor_tensor(out=mid_, in0=lo_, in1=stp[:, it:it + 1], op=ALU.add), [kB, kT], [kB])
                    P.v(lambda e: e.tensor_scalar(out=jkb[:, 0:nk], in0=S[:, 0:nk], scalar1=mid_, scalar2=0.0,
                                                  op0=ALU.is_ge, op1=ALU.add, accum_out=cnt_), [kS, kB], [kJ, kB])
                    P.v(lambda e, it=it: e.tensor_scalar(out=t_, in0=cnt_, scalar1=TOPK - 0.5, scalar2=stp[:, it:it + 1],
                                                         op0=ALU.is_ge, op1=ALU.mult), [kB, kT], [kB])
                    P.v(lambda e: e.tensor_tensor(out=lo_, in0=lo_, in1=t_, op=ALU.add), [kB], [kB])
                    yield
                thr = lo_
            else:
                thr = thr_c[:, 0:1]
            P.v(lambda e: e.tensor_scalar(out=nmb[:, 0:nk], in0=S[:, 0:nk], scalar1=thr, scalar2=NEG, op0=ALU.is_lt, op1=ALU.mult),
                [kS, kB, thr_c], [kN])
            for kb in range(qb + 1):
                P.tr(pTb[:, (kb % 4) * 128:(kb % 4 + 1) * 128], nmb[:, kb * 128:(kb + 1) * 128], ident_b[:, :], [kN, ident_b], [pT])
                if kb % 4 == 3 or kb == qb:
                    k0 = kb - kb % 4
                    n_ = kb - k0 + 1
                    P.act(NMs[:, k0:k0 + n_, :], pTb[:, 0:n_ * 128].rearrange("p (k t) -> p k t", k=n_), AF.Copy, [pT], [kM])
            P.dma("sync", nmtd[0:qb + 1, :, qb * 128:(qb + 1) * 128].rearrange("k s t -> s k t"), NMs[:, 0:qb + 1, :], [kM], ["nmtd"])

        for qb0 in range(0, NB, 2):
            gens = [idx_gen(qb0, CH[0]), idx_gen(qb0 + 1, CH[1])]
            while gens:
                for gen_ in list(gens):
                    try:
                        next(gen_)
                    except StopIteration:
                        gens.remove(gen_)
        P.fence()
        chk('IDX')
        wvc = W[0][:, 0:8 * 640].rearrange("p (k n) -> p k n", k=8)
        for k in range(8):
            P.dma("gpsimd", wvc[:, k, :], win[k * 128:(k + 1) * 128, 1544:2184], ["wsrc"], [W[0]])
        wuk = W[1][:, 0:512]
        wuv = W[1][:, 512:1024]
        P.dma("gpsimd", wuk, I["ab_w_uk"][j], ["wsrc"], [W[1]])
        P.dma("gpsimd", wuv, I["ab_w_uv"][j], ["wsrc"], [W[1]])
        ckvT = W[1][:, 1024:1024 + L]
        QB = R[0][:, :].rearrange("p (c t) -> p c t", c=4)
        KH = R[1][:, :].rearrange("p (c t) -> p c t", c=4)
        VH = R[2][:, :].rearrange("p (b n) -> p b n", b=NB)
        for g in range(NG):
            ht = ht_load(g)
            for cch in range(4):
                pg = PG.next()
                gemm_fm(pg, wvc, cch * 128, 128, ht, [W[0], ht])
                P.act(QB[:, cch, g * 512:(g + 1) * 512], pg[:, :], AF.Copy, [pg], ["QT"], scale=0.125)
            pg = PG.next()
            gemm_fm(pg, wvc, 512, 128, ht, [W[0], ht])
            P.act(ckvT[:, g * 512:(g + 1) * 512], pg[:, :], AF.Copy, [pg], ["ckvT"])
            for cch in range(4):
                pg = PG.next()
                P.mm(pg[:, :], wuk[:, cch * 128:(cch + 1) * 128], ckvT[:, g * 512:(g + 1) * 512], True, True, [W[1], "ckvT"], [pg])
                P.v(lambda e, pg=pg, cch=cch, g=g: e.tensor_copy(out=KH[:, cch, g * 512:(g + 1) * 512], in_=pg[:, :]), [pg], ["KT"])
            for b in range(4):
                pg = PG.next()
                P.mm(pg[:, :], ckvT[:, (4 * g + b) * 128:(4 * g + b + 1) * 128], wuv, True, True, [W[1], "ckvT"], [pg])
                P.v(lambda e, pg=pg, b=b, g=g: e.tensor_copy(out=VH[:, 4 * g + b, :], in_=pg[:, :]), [pg], ["VT"])
        P.fence()
        chk('B2')
        softmax_attn(0, QB, KH, VH, True, 4)
        P.fence()
        chk('ATT')
        prep_norm(l, False)
        out_proj_residual(l, I["ab_w_out"][j])
        P.fence()

    zt = PT.next()
    P.v(lambda e: e.memset(zt[:], 0.0), [], [zt])
    for kb in range(NB):
        r_ = kb % 4
        if r_:
            P.dma("sync", nmtd[kb][:, (kb - r_) * 128:kb * 128], zt[:, 0:r_ * 128], [zt], ["nmtd"])
    P.fence()

    def layer_cd(l):
        j = l // 2
        win = I["cd_w_in"][j]
        P.dma("gpsimd", maskneg[:], I["maskneg"], ["c_mk"], [maskneg])
        P.v(lambda e: e.tensor_scalar(out=mask01[:], in0=maskneg[:], scalar1=-1.0, scalar2=None, op0=ALU.is_ge), [maskneg], [maskneg])
        prep_norm(l, False)
        wv = wload(W[0], 1552, win[:, 0:1552])
        QKC = R[0][:, :].rearrange("p (c t) -> p c t", c=4)
        LS = R[1][:, :].bitcast(F32).rearrange("p (c t) -> p c t", c=2)
        VC = R[2][:, :].rearrange("p (b n) -> p b n", b=NB)
        wal = P_wal
        P.dma("sync", wal, I["cd_w_alpha"][j], ["wal"], ["wal"])
        for cc in range(2):
            P.dma("sync", cb[:, cc:cc + 1], I["cd_b_alpha"][j:j + 1, cc * 128:(cc + 1) * 128].rearrange("o p -> p o"), ["cb"], [cb], **NC)
        P.v(lambda e: e.tensor_scalar(out=cb[:, 2:4], in0=cb[:, 0:2], scalar1=-1.0, scalar2=None, op0=ALU.mult), [cb], [cb])
        for g in range(NG):
            ht = HT.next()
            for b in range(4):
                norm_block(xsrc(l), 4 * g + b, ht, b)
            ht_store(ht, g)
            for cch in range(4):
                pg = PG.next()
                gemm_fm(pg, wv, cch * 128, 128, ht, [W[0], ht])
                P.act(QKC[:, cch, g * 512:(g + 1) * 512], pg[:, :], AF.Copy, [pg], ["QK"])
            for cch in range(4):
                pg = PG.next()
                gemm_fm(pg, wv, 1040 + cch * 128, 128, ht, [W[0], ht])
                store_gated(pg, AF.Silu, cch, g)
            pg = PG.next()
            gemm_fm(pg, wv, 1024, 16, ht, [W[0], ht])
            gct = FT.next()
            P.v(lambda e, pg=pg, gct=gct: e.tensor_copy(out=gct[0:16, :], in_=pg[0:16, :]), [pg], [gct])
            for cch in range(2):
                pg = PG.next()
                P.mm(pg[:, :], wal[:, cch * 128:(cch + 1) * 128], gct[0:16, :], True, True, ["wal", gct], [pg])
                et = ET.next()
                P.act(et[:], pg[:, :], AF.Exp, [pg, cb], [et], scale=-1.0, bias=cb[:, 2 + cch:3 + cch])
                P.act(LS[:, cch, g * 512:(g + 1) * 512], et[:], AF.Ln, [et, one_c], ["LS"], bias=one_c[:, 0:1])
            for b in range(4):
                pg = PG.next()
                gemm_tm(pg, ht, b, wv, 512, 512, [W[0], ht])
                P.v(lambda e, pg=pg, b=b, g=g: e.tensor_copy(out=VC[:, 4 * g + b, :], in_=pg[:, :]), [pg], ["VT"])
        P.fence()
        chk('CA')
        nBs = [W[0][:, 0:8192].bitcast(F32), W[1][:, 0:8192].bitcast(F32)]
        for cch in range(2):
            P.v(lambda e, cch=cch: e.tensor_tensor_scan(out=nBs[cch], data0=one_c[:, 0:1].to_broadcast([128, L]), data1=LS[:, cch, :],
                                                        initial=0.0, op0=ALU.mult, op1=ALU.add), ["LS", one_c], [("nB", cch)])
        P.fence()
        KTg = R[1][:, 0:8192].rearrange("p (c t) -> p c t", c=2)
        Etmp = R[1][:, 8192:16384].bitcast(F32)
        QTg = W[1][:, 8192:9216].rearrange("p (c t) -> p c t", c=2)

        def gla(g):
            n = (g + 1) * 512
            for cch in range(2):
                if g == 0:
                    P.v(lambda e: e.memset(sm[:, 16:18], 0.0), [], ["bq"])
                    P.v(lambda e: e.memset(sm[:, 18:20], 0.0), ["bq"], ["bq"])
                else:
                    r_ = g * 512 - 1
                    P.v(lambda e, cch=cch, r_=r_: e.tensor_scalar(out=sm[:, 16 + cch:17 + cch], in0=nBs[cch][:, r_:r_ + 1], scalar1=1.0 / 16,
                                                                  scalar2=None, op0=ALU.mult), [("nB", cch)], ["bq"])
                    P.v(lambda e, cch=cch, r_=r_: e.tensor_scalar(out=sm[:, 18 + cch:19 + cch], in0=nBs[cch][:, r_:r_ + 1], scalar1=-1.0 / 16,
                                                                  scalar2=None, op0=ALU.mult), [("nB", cch), "bq"], ["bq"])
                et = ET.next()
                P.act(et[:], nBs[cch][:, g * 512:(g + 1) * 512], AF.Exp, [("nB", cch), "bq"], [et], scale=-1.0 / 16,
                      bias=sm[:, 16 + cch:17 + cch])
                P.v(lambda e, cch=cch, et=et, g=g: e.scalar_tensor_tensor(out=QTg[:, cch, :], in0=QKC[:, cch, g * 512:(g + 1) * 512],
                                                                          scalar=0.125, in1=et[:], op0=ALU.mult, op1=ALU.mult),
                    ["QK", et], ["QTg"])
                P.act(Etmp[:, 0:n], nBs[cch][:, 0:n], AF.Exp, [("nB", cch), "bq"], ["Etmp"], scale=1.0 / 16, bias=sm[:, 18 + cch:19 + cch])
                P.v(lambda e, cch=cch, n=n: e.tensor_tensor(out=KTg[:, cch, 0:n], in0=QKC[:, 2 + cch, 0:n], in1=Etmp[:, 0:n], op=ALU.mult),
                    ["QK", "Etmp"], ["KTg"])
            return QTg, KTg

        lin_attn(l, 1, None, 0, 0, VC, I["cd_hnorm_g"][j:j + 1, :], gla=gla)
        P.fence()
        chk('CG')
        wvd = wload(W[0], 1536, win[:, 1552:3088])
        QD = R[0][:, :].rearrange("p (c t) -> p c t", c=4)
        KD = R[1][:, :].rearrange("p (c t) -> p c t", c=4)
        VD = R[2][:, :].rearrange("p (b n) -> p b n", b=NB)
        for g in range(NG):
            ht = ht_load(g)
            for cch in range(4):
                pg = PG.next()
                gemm_fm(pg, wvd, cch * 128, 128, ht, [W[0], ht])
                P.act(QD[:, cch, g * 512:(g + 1) * 512], pg[:, :], AF.Copy, [pg], ["QT"], scale=0.125)
            for cch in range(4):
                pg = PG.next()
                gemm_fm(pg, wvd, 512 + cch * 128, 128, ht, [W[0], ht])
                P.v(lambda e, pg=pg, cch=cch, g=g: e.tensor_copy(out=KD[:, cch, g * 512:(g + 1) * 512], in_=pg[:, :]), [pg], ["KT"])
            for b in range(4):
                pg = PG.next()
                gemm_tm(pg, ht, b, wvd, 1024, 512, [W[0], ht])
                P.v(lambda e, pg=pg, b=b, g=g: e.tensor_copy(out=VD[:, 4 * g + b, :], in_=pg[:, :]), [pg], ["VT"])
        P.fence()
        chk('CD')
        softmax_attn(1, QD, KD, VD, False, 4)
        P.fence()
        chk('CS')
        prep_norm(l, False)
        out_proj_residual(l, I["cd_w_out"][j])
        P.fence()

    P_wal = W[1][0:16, 0:512].bitcast(F32)
    wr_t = P.sb("wr_t", [128, 8, 20], F32)

    rbias = P.sb("rbias", [128, 20], F32)
    R0f = R[0][:, :].bitcast(F32)
    R2f_ = R[2][:, :].bitcast(F32)
    GT = FT.t[0][:, 0:256].rearrange("p (b n) -> p b n", b=16)
    LG = FT.t[1][:, 0:80].rearrange("p (b n) -> p b n", b=4)
    rt = FT.t[1][:, 128:384].rearrange("p (b n) -> p b n", b=4)

    def moe(l):
        prep_norm(l, True)
        wr = wr_t
        P.dma("sync", wr[:, :, 0:4], I["moe_w_coarse"][l].rearrange("(k p) n -> p k n", p=128), ["wr"], ["wr"], **NC)
        for gi in range(4):
            P.dma("sync", wr[:, :, 4 + gi * 4:8 + gi * 4], I["moe_w_fine"][l][gi].rearrange("(k p) n -> p k n", p=128), ["wr"], ["wr"], **NC)
        P.dma("sync", rbias[:, 0:4], bc(I["moe_b_coarse"][l:l + 1, :], 4), ["rb"], [rbias])
        P.dma("sync", rbias[:, 4:20], bc(I["moe_b_fine"][l:l + 1, :], 16), ["rb"], [rbias])
        P.fence()
        chk('R0')
        HTF = R[0][:, 0:8192].bitcast(F32).rearrange("p (k t) -> p k t", k=8)
        for half in range(2):
            HH = R[1][:, :].rearrange("p (k t) -> p k t", k=8)
            YA = R[2]
            for gg in range(4):
                g = half * 4 + gg
                ht = HT.next()
                for b in range(4):
                    norm_block(xs, 4 * g + b, ht, b, htf=(None if os.environ.get('DBG_NOHTF') else HTF))
                chk('R1a')
                P.g(lambda e, ht=ht, gg=gg: e.tensor_copy(out=HH[:, :, gg * 512:(gg + 1) * 512], in_=ht[:, :, :]), [ht], ["HH"])
                chk('R1b')
                for b in range(4):
                    pg = PG.next()
                    for k in range(8):
                        P.mm(pg[:, 0:20], HTF[:, k, b * 128:(b + 1) * 128], wr[:, k, :], k == 0, k == 7, ["htf", "wr"], [pg])
                    P.v(lambda e, pg=pg, b=b: e.tensor_tensor(out=LG[:, b, :], in0=pg[:, 0:20], in1=rbias[:], op=ALU.add), [pg, rbias], ["LG"])
                chk('R1')
                lc = LG[:, :, 0:4]
                lf = LG[:, :, 4:20]
                cmax = rt[:, :, 0:1]
                ec = rt[:, :, 1:5]
                csum = rt[:, :, 5:6]
                ohg = rt[:, :, 6:10]
                msk = rt[:, :, 10:26]
                v1 = rt[:, :, 26:27]
                oh1 = rt[:, :, 27:43]
                v2 = rt[:, :, 43:44]
                p1 = rt[:, :, 44:45]
                p2 = rt[:, :, 45:46]
                oh2 = rt[:, :, 46:62]
                RT = ["rt", "LG"]
                P.v(lambda e: e.tensor_reduce(out=cmax, in_=lc, axis=AX.X, op=ALU.max), RT, ["rt"])
                P.v(lambda e: e.tensor_tensor(out=ec, in0=lc, in1=cmax.to_broadcast([128, 4, 4]), op=ALU.subtract), RT, ["rt"])
                P.v(lambda e: e.tensor_scalar(out=ohg, in0=ec, scalar1=0.0, scalar2=None, op0=ALU.is_ge), RT, ["rt"])
                P.act(ec, ec, AF.Exp, RT, ["rt"])
                P.v(lambda e: e.tensor_reduce(out=csum, in_=ec, axis=AX.X, op=ALU.add), RT, ["rt"])
                P.v(lambda e: e.reciprocal(out=csum, in_=csum), RT, ["rt"])
                P.v(lambda e: e.tensor_scalar(out=ohg, in0=ohg, scalar1=-1.0, scalar2=1e30, op0=ALU.add, op1=ALU.mult), RT, ["rt"])
                P.v(lambda e: e.tensor_tensor(out=msk.rearrange("p b (g e) -> p b g e", e=4), in0=lf.rearrange("p b (g e) -> p b g e", e=4),
                                              in1=ohg.unsqueeze(3).to_broadcast([128, 4, 4, 4]), op=ALU.add), RT, ["rt"])
                P.v(lambda e: e.tensor_reduce(out=v1, in_=msk, axis=AX.X, op=ALU.max), RT, ["rt"])
                P.v(lambda e: e.tensor_tensor(out=oh1, in0=msk, in1=v1.to_broadcast([128, 4, 16]), op=ALU.is_ge), RT, ["rt"])
                P.v(lambda e: e.scalar_tensor_tensor(out=msk, in0=oh1, scalar=-1e30, in1=msk, op0=ALU.mult, op1=ALU.add), RT, ["rt"])
                P.v(lambda e: e.tensor_reduce(out=v2, in_=msk, axis=AX.X, op=ALU.max), RT, ["rt"])
                P.v(lambda e: e.tensor_tensor(out=oh2, in0=msk, in1=v2.to_broadcast([128, 4, 16]), op=ALU.is_ge), RT, ["rt"])
                P.v(lambda e: e.tensor_tensor(out=p1, in0=v1, in1=v2, op=ALU.subtract), RT, ["rt"])
                P.act(p1, p1, AF.Sigmoid, RT, ["rt"])
                P.v(lambda e: e.tensor_scalar(out=p2, in0=p1, scalar1=-1.0, scalar2=1.0, op0=ALU.mult, op1=ALU.add), RT, ["rt"])
                P.v(lambda e: e.tensor_tensor(out=p1, in0=p1, in1=csum, op=ALU.mult), RT, ["rt"])
                P.v(lambda e: e.tensor_tensor(out=p2, in0=p2, in1=csum, op=ALU.mult), RT, ["rt"])
                P.v(lambda e: e.tensor_tensor(out=oh1, in0=oh1, in1=p1.to_broadcast([128, 4, 16]), op=ALU.mult), RT, ["rt"])
                P.v(lambda e: e.tensor_tensor(out=oh2, in0=oh2, in1=p2.to_broadcast([128, 4, 16]), op=ALU.mult), RT, ["rt"])
                P.v(lambda e, gg=gg: e.tensor_tensor(out=GT[:, gg * 4:(gg + 1) * 4, :], in0=oh1, in1=oh2, op=ALU.add), RT, ["GT"])
            chk('R2')
            P.fence()

            def YA(bi):
                return (R2f_ if bi < 8 else R0f)[:, (bi % 8) * 1024:(bi % 8 + 1) * 1024]

            NEX = int(os.environ.get('DBG_EX', 16))
            units = [(ex, g4) for ex in range(NEX) for g4 in range(4)]
            wviews = {}

            def wl(ex):
                wt = W[ex % 2]
                wg = wt[:, 0:4096].rearrange("p (k n) -> p k n", k=8)
                wu = wt[:, 4096:8192].rearrange("p (k n) -> p k n", k=8)
                wd = wt[:, 8192:12288].rearrange("p (k n) -> p k n", k=4)
                for k in range(8):
                    P.dma("gpsimd", wg[:, k, :], I["moe_w_gate"][l][ex][k * 128:(k + 1) * 128, :], ["wsrc"], [wt])
                    P.dma("gpsimd", wu[:, k, :], I["moe_w_up"][l][ex][k * 128:(k + 1) * 128, :], ["wsrc"], [wt])
                for k in range(4):
                    P.dma("gpsimd", wd[:, k, :], I["moe_w_down"][l][ex][k * 128:(k + 1) * 128, :], ["wsrc"], [wt])
                wviews[ex] = (wt, wg, wu, wd)

            def stageA(ex, g4):
                if g4 == 0:
                    wl(ex)
                wt, wg, wu, wd = wviews[ex]
                t0 = g4 * 512
                aT = HT.next()
                for fc in range(4):
                    pg = PG.next()
                    gemm_fm(pg, wg, fc * 128, 128, HH, [wt, "HH"], t0=t0)
                    pu = PA[fc % 2]
                    gemm_fm(pu, wu, fc * 128, 128, HH, [wt, "HH"], t0=t0)
                    sg_ = PT.next()
                    P.act(sg_[:], pg[:, :], AF.Silu, [pg], [sg_])
                    P.v(lambda e, aT=aT, fc=fc, sg_=sg_, pu=pu: e.tensor_tensor(out=aT[:, fc, :], in0=pu[:, :], in1=sg_[:], op=ALU.mult),
                        [pu, sg_], [aT])
                return aT

            def stageB(ex, g4, aT):
                wt, wg, wu, wd = wviews[ex]
                for b in range(4):
                    bi = g4 * 4 + b
                    for hf in range(2):
                        py = PA[2 + (b * 2 + hf) % 2]
                        for fc in range(4):
                            P.mm(py[:, :], aT[:, fc, b * 128:(b + 1) * 128], wd[:, fc, hf * 512:(hf + 1) * 512], fc == 0, fc == 3,
                                 [aT, wt], [py])
                        ysl = YA(bi)[:, hf * 512:(hf + 1) * 512]
                        if ex == 0:
                            P.v(lambda e, ysl=ysl, py=py, bi=bi, ex=ex: e.tensor_scalar(out=ysl, in0=py[:, :], scalar1=GT[:, bi, ex:ex + 1],
                                                                                        scalar2=None, op0=ALU.mult), [py, "GT"], [("YA", bi)])
                        else:
                            P.v(lambda e, ysl=ysl, py=py, bi=bi, ex=ex: e.scalar_tensor_tensor(out=ysl, in0=py[:, :], scalar=GT[:, bi, ex:ex + 1],
                                                                                               in1=ysl, op0=ALU.mult, op1=ALU.add),
                                [py, "GT", ("YA", bi)], [("YA", bi)])

            prevu = None
            for (ex, g4) in units:
                aT = stageA(ex, g4)
                if prevu is not None:
                    stageB(*prevu)
                prevu = (ex, g4, aT)
            stageB(*prevu)
            for bi in range(16):
                blk = half * 16 + bi
                xb = XB.next()
                P.dma("sync", xb[:], xs[blk * 128:(blk + 1) * 128, :], [("xs", blk)], [xb])
                ysl = YA(bi)
                P.v(lambda e, ysl=ysl: e.tensor_tensor(out=ysl, in0=ysl, in1=gtB[:], op=ALU.mult), [("YA", bi), gtB], [("YA", bi)])
                P.g(lambda e, xb=xb, ysl=ysl: e.tensor_tensor(out=xb[:], in0=xb[:], in1=ysl, op=ALU.add), [xb, ("YA", bi)], [xb])
                P.dma("sync", xs[blk * 128:(blk + 1) * 128, :], xb[:], [xb], [("xs", blk)])
            P.fence()

    try:
        if only == "moe":
            for blk in range(NB):
                xb = XB.next()
                P.dma("sync", xb[:], I["x"][blk * 128:(blk + 1) * 128, :], ["xin"], [xb])
                P.dma("sync", xs[blk * 128:(blk + 1) * 128, :], xb[:], [xb], [("xs", blk)])
            P.fence()
            moe(0)
            raise _Stop()
        if only == "cd":
            for blk in range(NB):
                xb = XB.next()
                P.dma("sync", xb[:], I["x"][blk * 128:(blk + 1) * 128, :], ["xin"], [xb])
                P.dma("sync", xs[blk * 128:(blk + 1) * 128, :], xb[:], [xb], [("xs", blk)])
            P.fence()
            layer_cd(1)
            raise _Stop()
        for l in range(nlayers):
            if l % 2 == 0:
                layer_ab(l)
            else:
                layer_cd(l)
            chk('MIX')
            moe(l)
    except _Stop:
        P.fence()

    P.dma("sync", gsB[:], bc(I["g_final"][0:1, :]), ["g"], [gsB])
    for blk in range(NB):
        xb = XB.next()
        P.dma("sync", xb[:], (xs if nlayers > 0 else I["x"])[blk * 128:(blk + 1) * 128, :], [("xs", blk)], [xb])
        P.act(junk, xb[:], AF.Square, [xb], [ET.t[0], "ss"], accum_out=sm[:, 0:1])
        P.v(lambda e: e.tensor_scalar(out=sm[:, 1:2], in0=sm[:, 0:1], scalar1=1.0 / D, scalar2=1e-6, op0=ALU.mult, op1=ALU.add), ["ss"], ["rs"])
        P.act(sm[:, 1:2], sm[:, 1:2], AF.Sqrt, ["rs"], ["rs"])
        P.v(lambda e: e.reciprocal(out=sm[:, 1:2], in_=sm[:, 1:2]), ["rs"], ["rs"])
        hm = HM.next()
        P.v(lambda e, hm=hm, xb=xb: e.scalar_tensor_tensor(out=hm[:], in0=xb[:], scalar=sm[:, 1:2], in1=gsB[:], op0=ALU.mult, op1=ALU.mult),
            [xb, "rs", gsB], [hm])
        P.dma("sync", out[blk * 128:(blk + 1) * 128, :], hm[:], [hm], ["out"])
    P.finish()
    return nc


_CACHE = {}


def kernel(**inputs):
    f32 = lambda a: np.ascontiguousarray(np.asarray(a, dtype=np.float32))
    inp = {k: f32(v) for k, v in inputs.items()}
    consts = _host_consts()
    shared = {}
    for k in ("w_ada", "b_ada", "g_mix", "g_ffn", "rel_bias", "ab_w_in", "ab_conv_w", "ab_conv_b", "ab_hnorm_g", "ab_w_out",
              "cd_w_in", "cd_w_alpha", "cd_b_alpha", "cd_hnorm_g", "cd_w_out", "moe_w_coarse", "moe_b_coarse", "moe_w_fine"):
        shared[k] = inp[k]
    shared["g_final"] = inp["g_final"].reshape(1, D)
    shared["ab_gate_b"] = inp["ab_gate_b"].reshape(2, 8)
    shared["ab_w_uk"] = inp["ab_w_uk"].reshape(2, 128, 512)
    shared["ab_w_uv"] = inp["ab_w_uv"].reshape(2, 128, 512)
    shared["moe_b_fine"] = inp["moe_b_fine"].reshape(4, 16)
    shared["moe_w_gate"] = inp["moe_w_gate"].reshape(4, 16, D, 512)
    shared["moe_w_up"] = inp["moe_w_up"].reshape(4, 16, D, 512)
    shared["moe_w_down"] = inp["moe_w_down"].reshape(4, 16, 512, D)
    shared.update(consts)
    if "nc" not in _CACHE:
        _CACHE["nc"] = build_nc()
    nc = _CACHE["nc"]
    in_maps = []
    for b in range(8):
        m = dict(shared)
        m["x"] = inp["x"][b]
        m["c"] = inp["c"][b].reshape(8, 128)
        in_maps.append(m)
    res = run_bass_kernel_spmd(nc, in_maps, core_ids=list(range(8)))
    return np.stack([np.asarray(r["out"], dtype=np.float32) for r in res.results], axis=0)
```
